# Optimizing a Trainium2 kernel written in Bass

```python
import math
import jax, jax.numpy as jnp
from jax import lax
import numpy as np

D_MODEL = 1024
BATCH = 8
SEQ = 4096
DEPTH = 1

M_HEADS = 4
M_WIDTH = D_MODEL
M_HEAD_DIM = M_WIDTH // M_HEADS
M_CONV = 4
M_CHUNK = 64
S_WIDTH = D_MODEL // 2
S_GROUP = 16
S_GROUPS = S_WIDTH // S_GROUP
S_STATE = 64
FFN_DIM = 2816
FFN_CONV = 3
EPS = 1e-6
IN_SIZES = (M_WIDTH, M_WIDTH, M_HEADS, M_HEADS, S_WIDTH, D_MODEL, D_MODEL)
N_IN = sum(IN_SIZES)

kernel_name = "hybrid_mlstm_s5_gated_merge_convffn"


def rmsnorm(x, g):
    x32 = x.astype(jnp.float32)
    y = x32 * lax.rsqrt(jnp.mean(x32 * x32, axis=-1, keepdims=True) + EPS)
    return (y * g.astype(jnp.float32)).astype(x.dtype)


def causal_dwconv(x, w, b):
    width = w.shape[0]
    L = x.shape[1]
    xp = jnp.pad(x, ((0, 0), (width - 1, 0), (0, 0)))
    return sum(xp[:, k:k + L] * w[k] for k in range(width)) + b


def mlstm_chunkwise(q, k, v, ig, fg):
    Bsz, L, H, Dh = q.shape
    nc = L // M_CHUNK

    def chunks4(t):
        return t.reshape(Bsz, nc, M_CHUNK, H, Dh).transpose(1, 0, 3, 2, 4)

    def chunks3(t):
        return t.reshape(Bsz, nc, M_CHUNK, H).transpose(1, 0, 3, 2)

    li = ig
    lf = jax.nn.log_sigmoid(fg)
    causal = jnp.tril(jnp.ones((M_CHUNK, M_CHUNK), dtype=bool))

    def step(carry, inp):
        C, n, m = carry
        qc, kc, vc, lic, lfc = inp
        b = jnp.cumsum(lfc, axis=-1)
        dmat = jnp.where(causal, b[..., :, None] - b[..., None, :] + lic[..., None, :], -jnp.inf)
        inter = b + m[..., None]
        m_row = jnp.maximum(inter, jnp.max(dmat, axis=-1))
        w_intra = jnp.exp(dmat - m_row[..., None])
        w_inter = jnp.exp(inter - m_row)
        s = jnp.einsum('bhqd,bhkd->bhqk', qc, kc) * w_intra
        num = (w_inter[..., None] * jnp.einsum('bhvd,bhqd->bhqv', C, qc)
               + jnp.einsum('bhqk,bhkv->bhqv', s, vc))
        den = w_inter * jnp.einsum('bhd,bhqd->bhq', n, qc) + jnp.sum(s, axis=-1)
        h = num / jnp.maximum(jnp.abs(den), jnp.exp(-m_row))[..., None]
        b_last = b[..., -1]
        g = b_last[..., None] - b + lic
        m_new = jnp.maximum(b_last + m, jnp.max(g, axis=-1))
        decay = jnp.exp(b_last + m - m_new)
        wk = jnp.exp(g - m_new[..., None])
        C_new = decay[..., None, None] * C + jnp.einsum('bhc,bhcv,bhcd->bhvd', wk, vc, kc)
        n_new = decay[..., None] * n + jnp.einsum('bhc,bhcd->bhd', wk, kc)
        return (C_new, n_new, m_new), h

    init = (jnp.zeros((Bsz, H, Dh, Dh), jnp.float32),
            jnp.zeros((Bsz, H, Dh), jnp.float32),
            jnp.zeros((Bsz, H), jnp.float32))
    _, hs = lax.scan(step, init, (chunks4(q), chunks4(k), chunks4(v), chunks3(li), chunks3(lf)))
    return hs.transpose(1, 0, 3, 2, 4).reshape(Bsz, L, H, Dh)


def mlstm_mixer(xm, om, ig, fg, conv_w, conv_b, wq, wk, wv, head_g, skip):
    Bsz, L, _ = xm.shape
    xc = jax.nn.silu(causal_dwconv(xm, conv_w, conv_b))
    xc_h = xc.reshape(Bsz, L, M_HEADS, M_HEAD_DIM)
    xm_h = xm.reshape(Bsz, L, M_HEADS, M_HEAD_DIM)
    q = jnp.einsum('blhd,hde->blhe', xc_h, wq).astype(jnp.float32)
    k = (jnp.einsum('blhd,hde->blhe', xc_h, wk) * (M_HEAD_DIM ** -0.5)).astype(jnp.float32)
    v = jnp.einsum('blhd,hde->blhe', xm_h, wv).astype(jnp.float32)
    hc = mlstm_chunkwise(q, k, v, ig.astype(jnp.float32), fg.astype(jnp.float32))
    mu = jnp.mean(hc, axis=-1, keepdims=True)
    var = jnp.mean(jnp.square(hc - mu), axis=-1, keepdims=True)
    hn = ((hc - mu) * lax.rsqrt(var + EPS)).reshape(Bsz, L, M_WIDTH) * head_g.astype(jnp.float32)
    out = jax.nn.sigmoid(om.astype(jnp.float32)) * hn + skip.astype(jnp.float32) * xc.astype(jnp.float32)
    return out.astype(xm.dtype)


def _complex_affine_combine(e1, e2):
    a1r, a1i, b1r, b1i = e1
    a2r, a2i, b2r, b2i = e2
    ar = a1r * a2r - a1i * a2i
    ai = a1r * a2i + a1i * a2r
    br = a2r * b1r - a2i * b1i + b2r
    bi = a2r * b1i + a2i * b1r + b2i
    return (ar, ai, br, bi)


def s5_mixer(u, a_re, a_im, log_dt, b_re, b_im, c_re, c_im, d_skip, w_glu, b_glu):
    Bsz, L, _ = u.shape
    f32 = jnp.float32
    u32 = u.astype(f32).reshape(Bsz, L, S_GROUPS, S_GROUP)
    ar, ai = a_re.astype(f32), a_im.astype(f32)
    dt = jnp.exp(log_dt.astype(f32))[:, None]
    mag = jnp.exp(dt * ar)
    abar_re, abar_im = mag * jnp.cos(dt * ai), mag * jnp.sin(dt * ai)
    den = ar * ar + ai * ai
    xr, xi = abar_re - 1.0, abar_im
    r_re = (xr * ar + xi * ai) / den
    r_im = (xi * ar - xr * ai) / den
    br_, bi_ = b_re.astype(f32), b_im.astype(f32)
    bbar_re = r_re[..., None] * br_ - r_im[..., None] * bi_
    bbar_im = r_re[..., None] * bi_ + r_im[..., None] * br_
    bu_re = jnp.einsum('blgc,gpc->blgp', u32, bbar_re)
    bu_im = jnp.einsum('blgc,gpc->blgp', u32, bbar_im)
    a_seq_re = jnp.broadcast_to(abar_re, (1, L, S_GROUPS, S_STATE))
    a_seq_im = jnp.broadcast_to(abar_im, (1, L, S_GROUPS, S_STATE))
    _, _, s_re, s_im = lax.associative_scan(_complex_affine_combine,
                                            (a_seq_re, a_seq_im, bu_re, bu_im), axis=1)
    y = (jnp.einsum('blgp,gcp->blgc', s_re, c_re.astype(f32))
         - jnp.einsum('blgp,gcp->blgc', s_im, c_im.astype(f32))
         + d_skip.astype(f32) * u32)
    y = jax.nn.gelu(y.reshape(Bsz, L, S_WIDTH))
    y = y * jax.nn.sigmoid(y @ w_glu.astype(f32) + b_glu.astype(f32))
    return y.astype(u.dtype)


def setup_inputs(seed: int = 0) -> dict:
    key = jax.random.key(seed)
    ks = jax.random.split(key, 40)
    nrm = lambda k, s, sc: jax.random.normal(k, s, jnp.float32) * sc
    Ld = DEPTH
    x = jax.random.normal(ks[0], (BATCH, SEQ, D_MODEL), jnp.float32)
    mix_norm_g = 1.0 + nrm(ks[1], (Ld, D_MODEL), 0.02)
    w_in = nrm(ks[2], (Ld, D_MODEL, N_IN), D_MODEL ** -0.5)
    b_in = jnp.concatenate([
        nrm(ks[3], (Ld, 2 * M_WIDTH), 0.02),
        nrm(ks[4], (Ld, M_HEADS), 0.1),
        jnp.linspace(3.0, 6.0, M_HEADS)[None, :] + nrm(ks[5], (Ld, M_HEADS), 0.1),
        nrm(ks[6], (Ld, S_WIDTH + 2 * D_MODEL), 0.02)], axis=-1)
    m_conv_w = nrm(ks[7], (Ld, M_CONV, M_WIDTH), M_CONV ** -0.5)
    m_conv_b = nrm(ks[8], (Ld, M_WIDTH), 0.02)
    m_wq = nrm(ks[9], (Ld, M_HEADS, M_HEAD_DIM, M_HEAD_DIM), M_HEAD_DIM ** -0.5)
    m_wk = nrm(ks[10], (Ld, M_HEADS, M_HEAD_DIM, M_HEAD_DIM), M_HEAD_DIM ** -0.5)
    m_wv = nrm(ks[11], (Ld, M_HEADS, M_HEAD_DIM, M_HEAD_DIM), M_HEAD_DIM ** -0.5)
    m_head_g = 1.0 + nrm(ks[12], (Ld, M_WIDTH), 0.02)
    m_skip = 1.0 + nrm(ks[13], (Ld, M_WIDTH), 0.02)
    s_a_re = -0.5 + nrm(ks[14], (Ld, S_GROUPS, S_STATE), 0.01)
    s_a_im = (math.pi * jnp.arange(S_STATE, dtype=jnp.float32))[None, None, :] + nrm(ks[15], (Ld, S_GROUPS, S_STATE), 0.01)
    s_log_dt = jax.random.uniform(ks[16], (Ld, S_GROUPS), jnp.float32, math.log(1e-3), math.log(1e-1))
    s_b_re = nrm(ks[17], (Ld, S_GROUPS, S_STATE, S_GROUP), (2 * S_GROUP) ** -0.5)
    s_b_im = nrm(ks[18], (Ld, S_GROUPS, S_STATE, S_GROUP), (2 * S_GROUP) ** -0.5)
    s_c_re = nrm(ks[19], (Ld, S_GROUPS, S_GROUP, S_STATE), S_STATE ** -0.5)
    s_c_im = nrm(ks[20], (Ld, S_GROUPS, S_GROUP, S_STATE), S_STATE ** -0.5)
    s_d = nrm(ks[21], (Ld, S_GROUPS, S_GROUP), 1.0)
    s_w_glu = nrm(ks[22], (Ld, S_WIDTH, S_WIDTH), S_WIDTH ** -0.5)
    s_b_glu = nrm(ks[23], (Ld, S_WIDTH), 0.02)
    w_branch_a = nrm(ks[24], (Ld, M_WIDTH, D_MODEL), M_WIDTH ** -0.5)
    w_branch_b = nrm(ks[25], (Ld, S_WIDTH, D_MODEL), S_WIDTH ** -0.5)
    w_out = nrm(ks[26], (Ld, D_MODEL, D_MODEL), D_MODEL ** -0.5)
    ffn_norm_g = 1.0 + nrm(ks[27], (Ld, D_MODEL), 0.02)
    w_up = nrm(ks[28], (Ld, D_MODEL, 2 * FFN_DIM), D_MODEL ** -0.5)
    ffn_conv_w = nrm(ks[29], (Ld, FFN_CONV, 2 * FFN_DIM), FFN_CONV ** -0.5)
    ffn_conv_b = nrm(ks[30], (Ld, 2 * FFN_DIM), 0.02)
    w_down = nrm(ks[31], (Ld, FFN_DIM, D_MODEL), FFN_DIM ** -0.5)
    final_norm_g = 1.0 + nrm(ks[32], (D_MODEL,), 0.02)
    return {"x": x, "mix_norm_g": mix_norm_g, "w_in": w_in, "b_in": b_in,
            "m_conv_w": m_conv_w, "m_conv_b": m_conv_b, "m_wq": m_wq, "m_wk": m_wk, "m_wv": m_wv,
            "m_head_g": m_head_g, "m_skip": m_skip,
            "s_a_re": s_a_re, "s_a_im": s_a_im, "s_log_dt": s_log_dt, "s_b_re": s_b_re, "s_b_im": s_b_im,
            "s_c_re": s_c_re, "s_c_im": s_c_im, "s_d": s_d, "s_w_glu": s_w_glu, "s_b_glu": s_b_glu,
            "w_branch_a": w_branch_a, "w_branch_b": w_branch_b, "w_out": w_out,
            "ffn_norm_g": ffn_norm_g, "w_up": w_up, "ffn_conv_w": ffn_conv_w, "ffn_conv_b": ffn_conv_b,
            "w_down": w_down, "final_norm_g": final_norm_g}


def reference(x, mix_norm_g, w_in, b_in, m_conv_w, m_conv_b, m_wq, m_wk, m_wv, m_head_g, m_skip,
              s_a_re, s_a_im, s_log_dt, s_b_re, s_b_im, s_c_re, s_c_im, s_d, s_w_glu, s_b_glu,
              w_branch_a, w_branch_b, w_out, ffn_norm_g, w_up, ffn_conv_w, ffn_conv_b, w_down,
              final_norm_g):
    splits = [int(s) for s in np.cumsum(IN_SIZES)[:-1]]
    for l in range(DEPTH):
        h = rmsnorm(x, mix_norm_g[l])
        proj = h @ w_in[l] + b_in[l]
        xm, om, ig, fg, us, ga, gb = jnp.split(proj, splits, axis=-1)
        a_out = mlstm_mixer(xm, om, ig, fg, m_conv_w[l], m_conv_b[l], m_wq[l], m_wk[l], m_wv[l],
                            m_head_g[l], m_skip[l])
        b_out = s5_mixer(us, s_a_re[l], s_a_im[l], s_log_dt[l], s_b_re[l], s_b_im[l],
                         s_c_re[l], s_c_im[l], s_d[l], s_w_glu[l], s_b_glu[l])
        merged = (jax.nn.sigmoid(ga) * (a_out @ w_branch_a[l])
                  + jax.nn.sigmoid(gb) * (b_out @ w_branch_b[l]))
        x = x + merged @ w_out[l]
        hf = rmsnorm(x, ffn_norm_g[l])
        up = causal_dwconv(hf @ w_up[l], ffn_conv_w[l], ffn_conv_b[l])
        val, gate = jnp.split(up, 2, axis=-1)
        x = x + (jax.nn.silu(gate) * val) @ w_down[l]
    return rmsnorm(x, final_norm_g)
```

```python
import contextlib
import math
import numpy as np
import concourse.bass as bass
import concourse.mybir as mybir
from concourse.bass_utils import run_bass_kernel_spmd

F32 = mybir.dt.float32
BF16 = mybir.dt.bfloat16
ALU = mybir.AluOpType
AF = mybir.ActivationFunctionType

SEQ = 4096
DM = 1024
TT = 512
NST = SEQ // TT
NS = 67
RING = 4
EPS = 1e-6


class _Op:
    __slots__ = ("eng", "fn", "is_dma", "deps", "inc", "sem", "semval")

    def __init__(self, eng, fn, is_dma):
        self.eng = eng
        self.fn = fn
        self.is_dma = is_dma
        self.deps = []
        self.inc = False
        self.sem = None
        self.semval = 0


class Sched:
    ENGS = ("pe", "act", "dve", "pool", "sp")
    N_DMA_SEMS = 24

    _uid = [0]

    @staticmethod
    def make_sems(nc, st):
        csem = {e: st.enter_context(nc.semaphore("cs_" + e)) for e in ("pe", "act", "dve", "pool")}
        dsem = {e: [st.enter_context(nc.semaphore("ds_%s%d" % (e, i))) for i in range(Sched.N_DMA_SEMS)]
                for e in ("sp", "pool")}
        return dict(csem=csem, dsem=dsem, ccount={e: 0 for e in csem}, dcount={e: [0] * Sched.N_DMA_SEMS for e in dsem})

    def __init__(self, nc, sems=None):
        self.nc = nc
        self.sems = sems
        Sched._uid[0] += 1
        self.uid = Sched._uid[0]
        self.ops = {e: [] for e in self.ENGS}
        self.last_w = {}
        self.readers = {}

    def _add(self, eng, fn, is_dma, reads, writes):
        op = _Op(eng, fn, is_dma)
        deps = []
        raw = set()
        for k in reads:
            w = self.last_w.get(k)
            if w is not None:
                deps.append(w)
                raw.add(id(w))
        for k in writes:
            w = self.last_w.get(k)
            if w is not None:
                deps.append(w)
            deps.extend(self.readers.get(k, ()))
        for k in writes:
            self.last_w[k] = op
            self.readers[k] = []
        for k in reads:
            if k not in writes:
                self.readers.setdefault(k, []).append(op)
        seen = set()
        for d in deps:
            if d is op or id(d) in seen:
                continue
            seen.add(id(d))
            if d.eng == eng and not d.is_dma and not is_dma:
                if eng == "pe" or id(d) not in raw:
                    continue
            op.deps.append(d)
            d.inc = True
        self.ops[eng].append(op)
        return op

    def op(self, eng, fn, reads=(), writes=()):
        return self._add(eng, fn, False, tuple(reads), tuple(writes))

    def dma(self, eng, out, in_, reads=(), writes=()):
        return self._add(eng, lambda e: e.dma_start(out=out, in_=in_), True, tuple(reads), tuple(writes))

    def emit(self):
        nc = self.nc
        with contextlib.ExitStack() as st:
            pool_ = self.sems
            csem, dsem, ccount, dcount = pool_["csem"], pool_["dsem"], pool_["ccount"], pool_["dcount"]
            for e in self.ENGS:
                nd = 0
                for op in self.ops[e]:
                    if op.is_dma:
                        i = nd % self.N_DMA_SEMS
                        nd += 1
                        dcount[e][i] += 16
                        op.sem = dsem[e][i]
                        op.semval = dcount[e][i]
                    elif op.inc:
                        ccount[e] += 1
                        op.sem = csem[e]
                        op.semval = ccount[e]
            block = st.enter_context(nc.Block())

            def run(ename, eng):
                waited = {}
                last = {}
                for op in self.ops[ename]:
                    need = {}
                    for d in op.deps:
                        key = id(d.sem)
                        if waited.get(key, 0) >= d.semval:
                            continue
                        if key not in need or need[key][1] < d.semval:
                            need[key] = (d.sem, d.semval)
                    for key, (sem, val) in need.items():
                        eng.wait_ge(sem, val)
                        waited[key] = val
                    ins = op.fn(eng)
                    if op.is_dma:
                        ins.then_inc(op.sem, 16)
                        last[id(op.sem)] = (op.sem, op.semval)
                    elif op.inc:
                        ins.then_inc(op.sem, 1)
                for key, (sem, val) in last.items():
                    if waited.get(key, 0) < val:
                        eng.wait_ge(sem, val)

            block.tensor(lambda e: run("pe", e))
            block.scalar(lambda e: run("act", e))
            block.vector(lambda e: run("dve", e))
            block.gpsimd(lambda e: run("pool", e))
            block.sync(lambda e: run("sp", e))


C_BIN, C_G1, C_G2, C_MCW, C_MCB, C_HG, C_SK, C_FCW, C_FCB, C_BGLU, C_SD = 0, 36, 44, 52, 84, 92, 100, 108, 240, 284, 288
NCOL = 292
IN_PERM = np.concatenate([np.arange(0, 2048), np.arange(2056, 4616)])
SL_A, SL_B, SL_KT, SL_VT, SL_GLU, SL_BRA, SL_BRB, SL_WO, SL_UP, SL_DN = 0, 18, 20, 21, 22, 23, 27, 29, 33, 55
ORDER = (list(range(0, 10)) + [18, 19, 20, 21, 22] + list(range(10, 18)) + list(range(23, 67)))


def _prep(inp):
    f = lambda a: np.ascontiguousarray(np.asarray(a, dtype=np.float32))
    w_in = f(inp["w_in"][0])
    w_in_p = w_in[:, IN_PERM]
    b_in = f(inp["b_in"][0])
    slabs = np.zeros((NS, 128, 2048), np.float32)

    def put(s, col, blk):
        slabs[s, :, col:col + blk.shape[1]] = blk

    for mc in range(36):
        for kt in range(8):
            put(SL_A + mc // 2, ((mc % 2) * 8 + kt) * 128, w_in_p[kt * 128:(kt + 1) * 128, mc * 128:(mc + 1) * 128])
    wq, wk, wv = f(inp["m_wq"][0]), f(inp["m_wk"][0]), f(inp["m_wv"][0])
    for h in range(4):
        for qk, W in enumerate((wq, wk)):
            for ec in range(2):
                for kt in range(2):
                    col = ((((h % 2) * 2 + qk) * 2 + ec) * 2 + kt) * 128
                    put(SL_B + h // 2, col, W[h, kt * 128:(kt + 1) * 128, ec * 128:(ec + 1) * 128])
        for kt in range(2):
            put(SL_KT, (h * 2 + kt) * 256, wk[h, kt * 128:(kt + 1) * 128, :])
            put(SL_VT, (h * 2 + kt) * 256, wv[h, kt * 128:(kt + 1) * 128, :])
    wglu = f(inp["s_w_glu"][0])
    for jc in range(4):
        for kt in range(4):
            put(SL_GLU, (jc * 4 + kt) * 128, wglu[kt * 128:(kt + 1) * 128, jc * 128:(jc + 1) * 128])
    wa, wb, wo = f(inp["w_branch_a"][0]), f(inp["w_branch_b"][0]), f(inp["w_out"][0])
    for mc in range(8):
        for kt in range(8):
            put(SL_BRA + mc // 2, ((mc % 2) * 8 + kt) * 128, wa[kt * 128:(kt + 1) * 128, mc * 128:(mc + 1) * 128])
        for kt in range(4):
            put(SL_BRB + mc // 4, ((mc % 4) * 4 + kt) * 128, wb[kt * 128:(kt + 1) * 128, mc * 128:(mc + 1) * 128])
    for nh in range(2):
        for kt in range(8):
            put(SL_WO + nh * 2 + kt // 4, (kt % 4) * 512, wo[kt * 128:(kt + 1) * 128, nh * 512:(nh + 1) * 512])
    wup, wdn = f(inp["w_up"][0]), f(inp["w_down"][0])
    for i in range(22):
        for vg in range(2):
            mc = i + 22 * vg
            for kt in range(8):
                put(SL_UP + i, (vg * 8 + kt) * 128, wup[kt * 128:(kt + 1) * 128, mc * 128:(mc + 1) * 128])
    for nh in range(2):
        for kt in range(22):
            put(SL_DN + nh * 6 + kt // 4, (kt % 4) * 512, wdn[kt * 128:(kt + 1) * 128, nh * 512:(nh + 1) * 512])

    ccol = np.zeros((128, NCOL), np.float32)
    col = lambda v, n: f(v).reshape(n, 128).T
    ccol[:, C_BIN:C_BIN + 36] = col(b_in[IN_PERM], 36)
    ccol[:, C_G1:C_G1 + 8] = col(inp["mix_norm_g"][0], 8)
    ccol[:, C_G2:C_G2 + 8] = col(inp["ffn_norm_g"][0], 8)
    mcw = f(inp["m_conv_w"][0])
    ccol[:, C_MCW:C_MCW + 32] = mcw.T.reshape(8, 128, 4).transpose(1, 0, 2).reshape(128, 32)
    ccol[:, C_MCB:C_MCB + 8] = col(inp["m_conv_b"][0], 8)
    ccol[:, C_HG:C_HG + 8] = col(inp["m_head_g"][0], 8)
    ccol[:, C_SK:C_SK + 8] = col(inp["m_skip"][0], 8)
    fcw = f(inp["ffn_conv_w"][0])
    ccol[:, C_FCW:C_FCW + 132] = fcw.T.reshape(44, 128, 3).transpose(1, 0, 2).reshape(128, 132)
    ccol[:, C_FCB:C_FCB + 44] = col(inp["ffn_conv_b"][0], 44)
    ccol[:, C_BGLU:C_BGLU + 4] = col(inp["s_b_glu"][0], 4)
    ccol[:, C_SD:C_SD + 4] = col(f(inp["s_d"][0]).reshape(-1), 4)
    crow = np.zeros((128, 1032), np.float32)
    crow[:, 0:1024] = f(inp["final_norm_g"])[None, :]
    crow[:, 1024:1032] = b_in[2048:2056][None, :]
    wg = np.ascontiguousarray(w_in[:, 2048:2056].reshape(8, 128, 8).transpose(1, 0, 2).reshape(128, 64))

    are, aim, ldt = f(inp["s_a_re"][0]), f(inp["s_a_im"][0]), f(inp["s_log_dt"][0])
    bre, bim = f(inp["s_b_re"][0]), f(inp["s_b_im"][0])
    cre, cim = f(inp["s_c_re"][0]), f(inp["s_c_im"][0])
    toL = lambda a: a.reshape(16, 2, 64).transpose(1, 2, 0).reshape(128, 16)
    rep = lambda a: np.broadcast_to(toL(a)[:, :, None], (128, 16, 32)).reshape(128, 512)
    s5X = np.stack([rep(are), rep(aim), rep(np.broadcast_to(ldt[:, None], (32, 64)))], axis=1)

    def toT(a):
        t = a.reshape(4, 4, 2, 64)
        t = np.broadcast_to(t[:, :, None, None, :, :], (4, 4, 2, 16, 2, 64))
        return t.transpose(1, 2, 3, 0, 4, 5).reshape(128, 4, 128)

    s5T = np.stack([toT(are), toT(aim), toT(np.broadcast_to(ldt[:, None], (32, 64)))], axis=1)

    def bT(b):
        t = b.reshape(4, 4, 2, 64, 16)
        o = np.zeros((4, 2, 16, 4, 2, 64), np.float32)
        for g2 in range(2):
            o[:, g2, :, :, g2, :] = t[:, :, g2].transpose(1, 3, 0, 2)
        return o.reshape(128, 4, 128)

    def bX(b):
        t = b.reshape(16, 2, 64, 16)
        o = np.zeros((2, 64, 16, 2, 16), np.float32)
        for g2 in range(2):
            o[g2, :, :, g2, :] = t[:, g2].transpose(1, 0, 2)
        return o.reshape(128, 16, 32)

    BtD = np.stack([bT(bre), bT(bim)], axis=1)
    BxD = np.stack([bX(bre), bX(bim)], axis=1)
    CxD = np.stack([bX(cre.transpose(0, 2, 1)), bX(cim.transpose(0, 2, 1))], axis=1)
    return dict(wall=slabs, ccol=ccol, crow=crow, wg=wg,
                s5X=np.ascontiguousarray(s5X), s5T=np.ascontiguousarray(s5T.reshape(128, 3, 512)),
                BtD=np.ascontiguousarray(BtD.reshape(128, 2, 512)), BxD=np.ascontiguousarray(BxD.reshape(128, 2, 512)),
                CxD=np.ascontiguousarray(CxD.reshape(128, 2, 512)))


C1G = math.sqrt(2.0 / math.pi)


def _cmul(dve, o_re, o_im, a_re, a_im, b_re, b_im, t1, t2):
    dve(lambda e: e.tensor_tensor(t1, a_re, b_re, ALU.mult))
    dve(lambda e: e.tensor_tensor(t2, a_im, b_im, ALU.mult))
    dve(lambda e: e.tensor_tensor(t1, t1, t2, ALU.subtract))
    dve(lambda e: e.tensor_tensor(t2, a_re, b_im, ALU.mult))
    dve(lambda e: e.tensor_tensor(o_im, a_im, b_re, ALU.mult))
    dve(lambda e: e.tensor_tensor(o_im, o_im, t2, ALU.add))
    dve(lambda e: e.tensor_copy(o_re, t1))


def _s5_params(S, par, W):
    k = ["s5"]
    dve = lambda fn: S.op("dve", fn, k, k)
    act = lambda fn: S.op("act", fn, k, k)
    are, aim, ldt = par[:, 0, :], par[:, 1, :], par[:, 2, :]
    dt, dre, dim, mag, c, s, t1, t2 = (W["w%d" % i][:] for i in range(8))
    act(lambda e: e.activation(dt, ldt, AF.Exp))
    dve(lambda e: e.tensor_tensor(dre, dt, are, ALU.mult))
    dve(lambda e: e.tensor_tensor(dim, dt, aim, ALU.mult))
    act(lambda e: e.activation(mag, dre, AF.Exp))
    act(lambda e: e.activation(W["R8"][:], dre, AF.Exp, scale=8.0))
    act(lambda e: e.activation(s, dim, AF.Sin, scale=1.0 / 16.0))
    act(lambda e: e.activation(c, dim, AF.Sin, bias=W["hpi"], scale=1.0 / 16.0))

    def square():
        dve(lambda e: e.tensor_tensor(t1, c, c, ALU.mult))
        dve(lambda e: e.tensor_tensor(t2, s, s, ALU.mult))
        dve(lambda e: e.scalar_tensor_tensor(s, c, 2.0, s, ALU.mult, ALU.mult))
        dve(lambda e: e.tensor_tensor(c, t1, t2, ALU.subtract))

    for _ in range(4):
        square()
    dve(lambda e: e.tensor_tensor(W["ab_re"][:], mag, c, ALU.mult))
    dve(lambda e: e.tensor_tensor(W["ab_im"][:], mag, s, ALU.mult))
    for _ in range(3):
        square()
    dve(lambda e: e.tensor_copy(W["c8"][:], c))
    dve(lambda e: e.tensor_copy(W["s8"][:], s))
    xr, den = dt, dre
    dve(lambda e: e.tensor_scalar_add(xr, W["ab_re"][:], -1.0))
    dve(lambda e: e.tensor_tensor(t1, are, are, ALU.mult))
    dve(lambda e: e.tensor_tensor(t2, aim, aim, ALU.mult))
    dve(lambda e: e.tensor_tensor(den, t1, t2, ALU.add))
    dve(lambda e: e.reciprocal(den, den))
    dve(lambda e: e.tensor_tensor(t1, xr, are, ALU.mult))
    dve(lambda e: e.tensor_tensor(t2, W["ab_im"][:], aim, ALU.mult))
    dve(lambda e: e.tensor_tensor(t1, t1, t2, ALU.add))
    dve(lambda e: e.tensor_tensor(W["r_re"][:], t1, den, ALU.mult))
    dve(lambda e: e.tensor_tensor(t1, W["ab_im"][:], are, ALU.mult))
    dve(lambda e: e.tensor_tensor(t2, xr, aim, ALU.mult))
    dve(lambda e: e.tensor_tensor(t1, t1, t2, ALU.subtract))
    dve(lambda e: e.tensor_tensor(W["r_im"][:], t1, den, ALU.mult))


def build_nc(debug=None, nst=NST):
    nc = bass.Bass("TRN2", target_bir_lowering=False)
    din = lambda n, s: nc.dram_tensor(n, s, F32, kind="ExternalInput").ap()
    x_d = din("x", [SEQ, DM])
    wall_d = din("wall", [NS, 128, 2048])
    ccol_d = din("ccol", [128, NCOL])
    crow_d = din("crow", [128, 1032])
    wg_d = din("wg", [128, 64])
    s5X_d = din("s5X", [128, 3, 512])
    s5T_d = din("s5T", [128, 3, 512])
    Bt_d = din("BtD", [128, 2, 512])
    Bx_d = din("BxD", [128, 2, 512])
    Cx_d = din("CxD", [128, 2, 512])
    y_d = nc.dram_tensor("y", [SEQ, DM], F32, kind="ExternalOutput").ap()
    wsl_d = nc.dram_tensor("wsl", [NS, 128, 2048], BF16, kind="Internal").ap()

    with contextlib.ExitStack() as st0:
        def T0(name, shape, dt=F32):
            return st0.enter_context(nc.sbuf_tensor("s_" + name, shape, dt))

        SEMS = Sched.make_sems(nc, st0)
        WZt = T0("WZt", [128, 4, 2, 8, 128], BF16)
        WI = T0("WI", [128, 16, 2, 8, 32], BF16)
        Kt = T0("Kt", [128, 4, 8, 128], BF16)
        Ec = T0("Ec", [128, 16, 64])
        Es = T0("Es", [128, 16, 64])
        Rtab = T0("Rtab", [128, 16, 64])
        Rl = T0("Rl", [128, 16])
        ccol = T0("ccol", [128, NCOL])
        chalf = T0("chalf", [128, NCOL])
        crow = T0("crow", [128, 1032])
        wgb = T0("wgb", [128, 64], BF16)
        ident = T0("ident", [128, 128], BF16)
        cmask = T0("cmask", [128, 128], BF16)
        LT = T0("LT", [128, 128])
        ONES = T0("ONES", [128, 128])
        small = T0("small", [128, 256])
        psf = [st0.enter_context(nc.psum_tensor("psf%d" % i, [128, 512], F32)) for i in range(6)]
        psb = [st0.enter_context(nc.psum_tensor("psb%d" % i, [128, 1024], BF16)) for i in range(2)]

        def cc(off, i=0):
            return ccol[:, off + i:off + i + 1]

        def ch(off, i=0):
            return chalf[:, off + i:off + i + 1]

        with contextlib.ExitStack() as st1:
            def T1(name, shape, dt=F32):
                return st1.enter_context(nc.sbuf_tensor("a_" + name, shape, dt))

            S = Sched(nc, SEMS)
            dve = lambda fn, r=(), w=(): S.op("dve", fn, r, w)
            act = lambda fn, r=(), w=(): S.op("act", fn, r, w)
            pool = lambda fn, r=(), w=(): S.op("pool", fn, r, w)
            pe = lambda fn, r=(), w=(): S.op("pe", fn, r, w)
            NSTG = 3
            w32 = [T1("w32_%d" % i, [128, 2048]) for i in range(NSTG)]
            w16 = [T1("w16_%d" % i, [128, 2048], BF16) for i in range(NSTG)]
            S.dma("sp", ccol[:], ccol_d, writes=["ccol"])
            S.dma("sp", crow[:], crow_d, writes=["crow"])
            S.dma("sp", w32[0][:, 0:64], wg_d, writes=["w32_0"])
            act(lambda e: e.copy(wgb[:], w32[0][:, 0:64]), ["w32_0"], ["wgb"])
            act(lambda e: e.mul(chalf[:], ccol[:], 0.5), ["ccol"], ["chalf"])
            pool(lambda e: e.memset(ident[:], 0.0), [], ["ident"])
            pool(lambda e: e.affine_select(out=ident[:], in_=ident[:], compare_op=ALU.not_equal, fill=1.0, base=0,
                                           pattern=[[-1, 128]], channel_multiplier=1), ["ident"], ["ident"])
            pool(lambda e: e.memset(cmask[:], 1.0), [], ["cmask"])
            pool(lambda e: e.affine_select(out=cmask[:], in_=cmask[:], compare_op=ALU.is_ge, fill=0.0, base=0,
                                           pattern=[[1, 128]], channel_multiplier=-1), ["cmask"], ["cmask"])
            pool(lambda e: e.memset(LT[:], 1.0), [], ["LT"])
            pool(lambda e: e.affine_select(out=LT[:], in_=LT[:], compare_op=ALU.is_ge, fill=0.0, base=-1,
                                           pattern=[[-1, 128]], channel_multiplier=1), ["LT"], ["LT"])
            pool(lambda e: e.memset(ONES[:], 1.0), [], ["ONES"])
            pool(lambda e: e.memset(small[:], 0.0), [], ["small"])
            pool(lambda e: e.memset(small[:, 250:251], EPS), ["small"], ["small"])
            pool(lambda e: e.memset(small[:, 251:252], 1.0), ["small"], ["small"])
            pool(lambda e: e.memset(small[:, 252:253], math.pi / 2.0), ["small"], ["small"])
            k5 = ["s5"]
            d5 = lambda fn: S.op("dve", fn, k5, k5)
            a5 = lambda fn: S.op("act", fn, k5, k5)
            parT = T1("parT", [128, 3, 512])
            parX = T1("parX", [128, 3, 512])
            BtS = T1("BtS", [128, 2, 512])
            BxS = T1("BxS", [128, 2, 512])
            CxS = T1("CxS", [128, 2, 512])
            S.dma("sp", parT[:], s5T_d, writes=["ld0"])
            S.dma("sp", parX[:], s5X_d, writes=["ld1"])
            S.dma("sp", BtS[:], Bt_d, writes=["ld2"])
            S.dma("sp", BxS[:], Bx_d, writes=["ld3"])
            S.dma("sp", CxS[:], Cx_d, writes=["ld4"])
            S.op("dve", lambda e: e.memset(small[:, 253:254], 0.0), ["ld0", "ld1", "ld2", "ld3", "ld4", "small"], k5)
            for s_ in range(NS):
                S.dma("pool", wsl_d[s_], wall_d[s_], reads=[], writes=["wsl%d" % s_])
            names = ["w%d" % i for i in range(8)] + ["ab_re", "ab_im", "r_re", "r_im", "c8", "s8", "R8"]
            WT = {n: T1("T_" + n, [128, 512]) for n in names}
            WX = {n: T1("X_" + n, [128, 512]) for n in names}
            WT["hpi"] = small[:, 252:253]
            WX["hpi"] = small[:, 252:253]
            _s5_params(S, parT, WT)
            _s5_params(S, parX, WX)
            cur_re, cur_im, t1, t2 = T1("cur_re", [128, 512]), T1("cur_im", [128, 512]), T1("t1", [128, 512]), T1("t2", [128, 512])
            _cmul(d5, cur_re[:], cur_im[:], WT["r_re"][:], WT["r_im"][:], BtS[:, 0, :], BtS[:, 1, :], t1[:], t2[:])
            for kk in range(8):
                j = 7 - kk
                a5(lambda e, j=j: e.copy(WZt[:, :, 0, j, :], cur_re[:, :].rearrange("p (c m) -> p c m", c=4)))
                a5(lambda e, j=j: e.copy(WZt[:, :, 1, j, :], cur_im[:, :].rearrange("p (c m) -> p c m", c=4)))
                if kk < 7:
                    _cmul(d5, cur_re[:], cur_im[:], cur_re[:], cur_im[:], WT["ab_re"][:], WT["ab_im"][:], t1[:], t2[:])
            _cmul(d5, cur_re[:], cur_im[:], CxS[:, 0, :], CxS[:, 1, :], WX["ab_re"][:], WX["ab_im"][:], t1[:], t2[:])
            for j in range(8):
                a5(lambda e, j=j: e.copy(WI[:, :, 0, j, :], cur_re[:, :].rearrange("p (g m) -> p g m", g=16)))
                a5(lambda e, j=j: e.mul(WI[:, :, 1, j, :], cur_im[:, :].rearrange("p (g m) -> p g m", g=16), -1.0))
                if j < 7:
                    _cmul(d5, cur_re[:], cur_im[:], cur_re[:], cur_im[:], WX["ab_re"][:], WX["ab_im"][:], t1[:], t2[:])
            Cb_re = T1("Cb_re", [128, 512], BF16)
            nCb_im = T1("nCb_im", [128, 512], BF16)
            Xb_re = T1("Xb_re", [128, 512], BF16)
            Xb_im = T1("Xb_im", [128, 512], BF16)
            a5(lambda e: e.copy(Cb_re[:], CxS[:, 0, :]))
            a5(lambda e: e.mul(nCb_im[:], CxS[:, 1, :], -1.0))
            _cmul(d5, cur_re[:], cur_im[:], WX["r_re"][:], WX["r_im"][:], BxS[:, 0, :], BxS[:, 1, :], t1[:], t2[:])
            for tau in range(8):
                a5(lambda e: e.copy(Xb_re[:], cur_re[:]))
                a5(lambda e: e.copy(Xb_im[:], cur_im[:]))
                d5(lambda e: e.memset(psf[0][:, :], 0.0))
                for gp in range(16):
                    chunk, win = gp // 4, gp % 4
                    o = psf[0][32 * win:32 * win + 32, chunk * 128 + 32 * win:chunk * 128 + 32 * win + 32]
                    S.op("pe", lambda e, o=o, gp=gp, win=win: e.matmul(o, lhsT=Xb_re[:, gp * 32:(gp + 1) * 32], rhs=Cb_re[:, gp * 32:(gp + 1) * 32],
                                                                     start=True, stop=False, tile_position=(0, 32 * win)), k5, k5)
                    S.op("pe", lambda e, o=o, gp=gp, win=win: e.matmul(o, lhsT=Xb_im[:, gp * 32:(gp + 1) * 32], rhs=nCb_im[:, gp * 32:(gp + 1) * 32],
                                                                     start=False, stop=True, tile_position=(0, 32 * win)), k5, k5)
                if tau == 0:
                    for chunk in range(4):
                        S.op("dve", lambda e, chunk=chunk: e.scalar_tensor_tensor(Kt[:, chunk, 0, :], ident[:], cc(C_SD, chunk), psf[0][:, chunk * 128:(chunk + 1) * 128],
                                                                               ALU.mult, ALU.add), k5 + ["ident", "ccol"], k5)
                else:
                    d5(lambda e, tau=tau: e.tensor_copy(Kt[:, :, tau, :], psf[0][:, :].rearrange("p (c m) -> p c m", c=4)))
                if tau < 7:
                    _cmul(d5, cur_re[:], cur_im[:], cur_re[:], cur_im[:], WX["ab_re"][:], WX["ab_im"][:], t1[:], t2[:])
            c8 = WX["c8"][:, :].rearrange("p (g m) -> p g m", g=16)
            s8 = WX["s8"][:, :].rearrange("p (g m) -> p g m", g=16)
            R8 = WX["R8"][:, :].rearrange("p (g m) -> p g m", g=16)
            d5(lambda e: e.tensor_copy(Ec[:, :, 0:1], c8[:, :, 0:1]))
            d5(lambda e: e.tensor_copy(Es[:, :, 0:1], s8[:, :, 0:1]))
            d5(lambda e: e.tensor_copy(Rl[:, :].rearrange("p (g a) -> p g a", a=1), R8[:, :, 0:1]))
            d5(lambda e: e.memset(Rtab[:], 0.0))
            m = 1
            while m < 64:
                bre_b = Ec[:, :, m - 1:m].to_broadcast([128, 16, m])
                bim_b = Es[:, :, m - 1:m].to_broadcast([128, 16, m])
                u1 = t1[:, 0:16 * m].rearrange("p (g k) -> p g k", g=16)
                u2 = t2[:, 0:16 * m].rearrange("p (g k) -> p g k", g=16)
                d5(lambda e, m=m, bre_b=bre_b, u1=u1: e.tensor_tensor(u1, Ec[:, :, 0:m], bre_b, ALU.mult))
                d5(lambda e, m=m, bim_b=bim_b, u2=u2: e.tensor_tensor(u2, Es[:, :, 0:m], bim_b, ALU.mult))
                d5(lambda e, m=m, u1=u1, u2=u2: e.tensor_tensor(u1, u1, u2, ALU.subtract))
                d5(lambda e, m=m, bre_b=bre_b, u2=u2: e.tensor_tensor(u2, Es[:, :, 0:m], bre_b, ALU.mult))
                d5(lambda e, m=m, u1=u1: e.tensor_copy(Ec[:, :, m:2 * m], u1))
                d5(lambda e, m=m, bim_b=bim_b, u1=u1: e.tensor_tensor(u1, Ec[:, :, 0:m], bim_b, ALU.mult))
                d5(lambda e, m=m, u1=u1, u2=u2: e.tensor_tensor(Es[:, :, m:2 * m], u1, u2, ALU.add))
                m *= 2
            d5(lambda e: e.memset(Rtab[:, :, 1:64], 1.0))
            d5(lambda e: e.tensor_tensor(Rtab[:, :, 1:64], Rtab[:, :, 1:64], Rl[:, :].rearrange("p (g a) -> p g a", a=1).to_broadcast([128, 16, 63]), ALU.mult))
            S.emit()

        with contextlib.ExitStack() as st:
            def T(name, shape, dt=F32):
                return st.enter_context(nc.sbuf_tensor("m_" + name, shape, dt))

            S = Sched(nc, SEMS)
            dve = lambda fn, r=(), w=(): S.op("dve", fn, r, w)
            act = lambda fn, r=(), w=(): S.op("act", fn, r, w)
            pool = lambda fn, r=(), w=(): S.op("pool", fn, r, w)
            pe = lambda fn, r=(), w=(): S.op("pe", fn, r, w)
            x_sb = T("x_sb", [128, 4, DM])
            hT = T("hT", [128, 8, TT], BF16)
            xmT = T("xmT", [128, 8, TT + 4], BF16)
            xcar = T("xcar", [128, 8, 4], BF16)
            som = T("som", [128, 8, TT], BF16)
            usT = T("usT", [128, 4, TT], BF16)
            xcT = T("xcT", [128, 8, TT], BF16)
            R1 = T("R1", [128, 12288], BF16)
            vp = T("vp", [128, 4, 4, 258], BF16)
            C32 = T("C32", [128, 4, 2, 257])
            Cb = T("Cb", [128, 4, 2, 258], BF16)
            glT = T("glT", [128, 4, TT], BF16)
            ring = T("ring", [128, RING, 2048], BF16)
            Sre = T("Sre", [128, 16, 65])
            Sim = T("Sim", [128, 16, 65])
            Sbre = T("Sbre", [128, 16, 64], BF16)
            Sbim = T("Sbim", [128, 16, 64], BF16)
            NF, NB = 6, 3
            fring = T("fring", [128, NF, 512])
            bring = T("bring", [128, NB, 512], BF16)
            rstate = {"f": 0, "b": 0}

            def f32buf():
                rstate["f"] = (rstate["f"] + 1) % NF
                return fring[:, rstate["f"], :], "fr%d" % rstate["f"]

            def bfbuf():
                rstate["b"] = (rstate["b"] + 1) % NB
                return bring[:, rstate["b"], :], "br%d" % rstate["b"]
            fcar = T("fcar", [128, 44, 2])
            XM = ["xmT%d" % c for c in range(8)]
            VP = ["vp%d" % s_ for s_ in range(4)]
            rk = lambda b: "R1_%d" % b
            hn_tm = xmT[:, :, :].rearrange("p a b -> p (a b)")[:, 0:4096].rearrange("p (s f) -> p s f", s=4)
            qT = R1[:, 0:4096].rearrange("p (c t) -> p c t", c=8)
            kT = R1[:, 4096:8192].rearrange("p (c t) -> p c t", c=8)
            k_tm = R1[:, 8192:12288].rearrange("p (s f) -> p s f", s=4)
            sga, sgb = qT, kT
            gT = R1[:, 0:11264].rearrange("p (c t) -> p c t", c=22)
            hnb_t = R1[:, 0:4096].rearrange("p (s f) -> p s f", s=4)
            mT = vp[:, :, :, :].rearrange("p a b c -> p (a b c)")[:, 0:4096].rearrange("p (c t) -> p c t", c=8)
            aoutT, boutT = xcT, usT
            epsc, onec = small[:, 250:251], small[:, 251:252]

            pool(lambda e: e.memset(C32[:], 0.0), [], ["C32_%d" % h for h in range(4)])
            pool(lambda e: e.memset(xcar[:], 0.0), [], ["xcar"])
            pool(lambda e: e.memset(fcar[:], 0.0), [], ["fcar"])
            pool(lambda e: e.memset(Sre[:], 0.0), [], ["Sre"])
            pool(lambda e: e.memset(Sim[:], 0.0), [], ["Sim"])
            pool(lambda e: e.memset(vp[:], 0.0), [], VP)
            pool(lambda e: e.memset(Cb[:], 0.0), [], ["Cb%d" % h for h in range(4)])

            seq = [sid for _ in range(nst) for sid in ORDER]
            wstate = {"issued": 0, "cur": -1}

            def wnext():
                wstate["cur"] += 1
                i = wstate["cur"]
                while wstate["issued"] < min(len(seq), i + RING):
                    n = wstate["issued"]
                    S.dma("sp", ring[:, n % RING, :], wsl_d[seq[n]], reads=[], writes=["ring%d" % (n % RING)])
                    wstate["issued"] += 1
                return ring[:, i % RING, :], "ring%d" % (i % RING)

            def sig(out, in_, rkeys, wkeys, bias=None, scale=1.0, eng2="pool"):
                if bias is None:
                    act(lambda e: e.activation(out, in_, AF.Tanh, scale=0.5 * scale), rkeys, wkeys)
                else:
                    act(lambda e: e.activation(out, in_, AF.Tanh, bias=bias, scale=0.5 * scale), list(rkeys) + ["chalf"], wkeys)
                S.op(eng2, lambda e: e.tensor_scalar(out, out, 0.5, 0.5, ALU.mult, ALU.add), wkeys, wkeys)

            psrr = {"i": 0}

            def nextps():
                psrr["i"] = (psrr["i"] + 1) % 4
                return psrr["i"], psf[psrr["i"]], "psf%d" % psrr["i"]

            def rms_rstd():
                ssq, rs = small[:, 0:4], small[:, 4:8]
                pool(lambda e: e.memset(ssq, 0.0), [], ["ssq"])
                for sub in range(4):
                    act(lambda e, sub=sub: e.activation(hnb_t[:, sub, :], x_sb[:, sub, :], AF.Square, accum_out=ssq[:, sub:sub + 1]),
                        ["x_sb%d" % sub, "ssq"], [rk(2 * sub), rk(2 * sub + 1), "ssq"])
                act(lambda e: e.activation(rs, ssq, AF.Ln, bias=epsc, scale=1.0 / DM), ["ssq"], ["rs"])
                act(lambda e: e.activation(rs, rs, AF.Exp, scale=-0.5), ["rs"], ["rs"])
                return rs

            def norm_T(gcol_off):
                rs = rms_rstd()
                for sub in range(4):
                    dve(lambda e, sub=sub: e.tensor_scalar_mul(hnb_t[:, sub, :], x_sb[:, sub, :], rs[:, sub:sub + 1]),
                        ["x_sb%d" % sub, "rs"], [rk(2 * sub), rk(2 * sub + 1)])
                for fc in range(8):
                    pb = psb[fc % 2]
                    for sub in range(4):
                        pe(lambda e, fc=fc, sub=sub, pb=pb: e.transpose(pb[:, sub * 128:(sub + 1) * 128], hnb_t[:, sub, fc * 128:(fc + 1) * 128], ident[:]),
                           [rk(2 * sub), rk(2 * sub + 1)], ["psb%d" % (fc % 2)])
                    dve(lambda e, fc=fc, pb=pb: e.tensor_scalar_mul(hT[:, fc, :], pb[:, 0:512], cc(gcol_off, fc)),
                        ["psb%d" % (fc % 2)], ["hT%d" % fc])

            def dump_T(src, nchunk, keys, stg):
                for c in range(nchunk):
                    fb, fk = f32buf()
                    dve(lambda e, c=c, fb=fb: e.tensor_copy(fb, src[:, c, :]), list(keys), [fk])
                    S.dma("pool", y_d[c * 128:(c + 1) * 128, stg * 512:(stg + 1) * 512], fb, reads=[fk], writes=["y"])

            def inproj_chunk(mc, slot, skey):
                pi, ps, pk = nextps()
                base = ((mc % 2) * 8) * 128
                for kt in range(8):
                    pe(lambda e, kt=kt, ps=ps: e.matmul(ps[:, :], lhsT=slot[:, base + kt * 128: base + (kt + 1) * 128], rhs=hT[:, kt, :],
                                                          start=(kt == 0), stop=(kt == 7)), [skey, "hT%d" % kt], [pk])
                return ps, pk

            for stg in range(nst):
                t0 = stg * TT
                for sub in range(4):
                    S.dma("pool", x_sb[:, sub, :], x_d[t0 + sub * 128:t0 + (sub + 1) * 128, :], writes=["x_sb%d" % sub])
                norm_T(C_G1)
                pool(lambda e: e.tensor_copy(xmT[:, :, 1:4], xcar[:, :, 1:4]), ["xcar"], XM)
                def conv_chunk(c):
                    z, zk = f32buf()
                    sg, sk = bfbuf()
                    dve(lambda e: e.tensor_scalar(z, xmT[:, c, 1:TT + 1], cc(C_MCW, c * 4 + 0), cc(C_MCB, c), ALU.mult, ALU.add),
                        ["xmT%d" % c], [zk])
                    for k in range(1, 4):
                        dve(lambda e, k=k: e.scalar_tensor_tensor(z, xmT[:, c, 1 + k:TT + 1 + k], cc(C_MCW, c * 4 + k), z, ALU.mult, ALU.add),
                            ["xmT%d" % c, zk], [zk])
                    sig(sg, z, [zk], [sk])
                    dve(lambda e: e.tensor_tensor(xcT[:, c, :], z, sg, ALU.mult), [zk, sk], ["xcT%d" % c])

                for mc in range(20):
                    if mc % 2 == 0:
                        slot, skey = wnext()
                    ps, pk = inproj_chunk(mc, slot, skey)
                    if mc < 8:
                        act(lambda e, mc=mc, ps=ps: e.activation(xmT[:, mc, 4:TT + 4], ps[:, :], AF.Identity, bias=cc(C_BIN, mc)),
                            [pk], ["xmT%d" % mc])
                        conv_chunk(mc)
                    elif mc < 16:
                        sig(som[:, mc - 8, :], ps[:, :], [pk], ["som%d" % (mc - 8)], bias=ch(C_BIN, mc))
                    else:
                        act(lambda e, mc=mc, ps=ps: e.activation(usT[:, mc - 16, :], ps[:, :], AF.Identity, bias=cc(C_BIN, mc)),
                            [pk], ["usT%d" % (mc - 16)])
                if debug == "h":
                    dump_T(hT, 8, ["hT%d" % c for c in range(8)], stg)
                if debug == "xm":
                    dump_T(xmT[:, :, 4:TT + 4], 8, XM, stg)
                gsb = small[:, 16:48].rearrange("p (s g) -> p s g", s=4)
                for sub in range(4):
                    for kt in range(8):
                        pe(lambda e, sub=sub, kt=kt: e.matmul(psf[4][:, sub * 8:(sub + 1) * 8], lhsT=hT[:, kt, sub * 128:(sub + 1) * 128],
                                                              rhs=wgb[:, kt * 8:(kt + 1) * 8], start=(kt == 0), stop=(kt == 7)),
                           ["hT%d" % kt], ["psf4"])
                for sub in range(4):
                    dve(lambda e, sub=sub: e.tensor_tensor(gsb[:, sub, :], psf[4][:, sub * 8:(sub + 1) * 8], crow[:, 1024:1032], ALU.add),
                        ["psf4"], ["gsb"])
                pool(lambda e: e.tensor_copy(xcar[:, :, 1:4], xmT[:, :, TT + 1:TT + 4]), XM, ["xcar"])
                if debug == "xc":
                    dump_T(xcT, 8, ["xcT%d" % c for c in range(8)], stg)
                lfn = small[:, 48:64].rearrange("p (s h) -> p s h", s=4)
                act(lambda e: e.activation(lfn, gsb[:, :, 4:8], AF.Exp, scale=-1.0), ["gsb"], ["lfn"])
                act(lambda e: e.activation(lfn, lfn, AF.Ln, bias=onec, scale=1.0), ["lfn"], ["lfn"])
                for sub in range(4):
                    pe(lambda e, sub=sub: e.matmul(psf[4][:, 64 + sub * 8:64 + sub * 8 + 4], lhsT=LT[:], rhs=lfn[:, sub, :], start=True, stop=True),
                       ["lfn"], ["psf4"])
                    pe(lambda e, sub=sub: e.matmul(psf[4][:, 64 + sub * 8 + 4:64 + sub * 8 + 8], lhsT=ONES[:], rhs=lfn[:, sub, :], start=True, stop=True),
                       ["lfn"], ["psf4"])
                Pm = psf[4][:, 64:96].rearrange("p (s g) -> p s g", s=4)
                wcol = small[:, 64:80].rearrange("p (s h) -> p s h", s=4)
                e2c = small[:, 80:96].rearrange("p (s h) -> p s h", s=4)
                dcol = small[:, 96:112].rearrange("p (s h) -> p s h", s=4)
                dve(lambda e: e.tensor_tensor(wcol, gsb[:, :, 0:4], Pm[:, :, 0:4], ALU.subtract), ["gsb", "psf4"], ["wcol"])
                act(lambda e: e.activation(wcol, wcol, AF.Exp), ["wcol"], ["wcol"])
                act(lambda e: e.activation(e2c, Pm[:, :, 0:4], AF.Exp, scale=-1.0), ["psf4"], ["e2c"])
                act(lambda e: e.activation(dcol, Pm[:, :, 4:8], AF.Exp, scale=-1.0), ["psf4"], ["dcol"])
                for hh in range(2):
                    slot, skey = wnext()
                    for hl in range(2):
                        h = hh * 2 + hl
                        for qk in range(2):
                            for ec in range(2):
                                pi, ps, pk = nextps()
                                for kt in range(2):
                                    col = ((((hl * 2 + qk) * 2 + ec) * 2 + kt)) * 128
                                    pe(lambda e, ps=ps, col=col, h=h, kt=kt, slot=slot: e.matmul(ps[:, :], lhsT=slot[:, col:col + 128], rhs=xcT[:, h * 2 + kt, :],
                                                                                                start=(kt == 0), stop=(kt == 1)),
                                       [skey, "xcT%d" % (h * 2 + kt)], [pk])
                                dst = (qT, kT)[qk]
                                act(lambda e, ps=ps, dst=dst, h=h, ec=ec, qk=qk: e.activation(dst[:, h * 2 + ec, :], ps[:, :], AF.Identity, scale=(1.0, 1.0 / 16.0)[qk]),
                                    [pk], [rk(qk * 8 + h * 2 + ec)])
                slot, skey = wnext()
                for sub in range(4):
                    for hp in range(2):
                        pi, ps, pk = nextps()
                        for hl in range(2):
                            h = hp * 2 + hl
                            for kt in range(2):
                                pe(lambda e, ps=ps, h=h, hl=hl, kt=kt, sub=sub, slot=slot: e.matmul(ps[:, hl * 256:(hl + 1) * 256], lhsT=xcT[:, h * 2 + kt, sub * 128:(sub + 1) * 128],
                                                                                                   rhs=slot[:, (h * 2 + kt) * 256:(h * 2 + kt + 1) * 256], start=(kt == 0), stop=(kt == 1)),
                                   [skey, "xcT%d" % (h * 2 + kt)], [pk])
                        act(lambda e, ps=ps, sub=sub, hp=hp: e.activation(k_tm[:, sub, hp * 512:(hp + 1) * 512], ps[:, :], AF.Identity, scale=1.0 / 16.0),
                            [pk], [rk(16 + sub * 2 + hp)])
                slot, skey = wnext()
                for sub in range(4):
                    for hp in range(2):
                        pi, ps, pk = nextps()
                        for hl in range(2):
                            h = hp * 2 + hl
                            for kt in range(2):
                                pe(lambda e, ps=ps, h=h, hl=hl, kt=kt, sub=sub, slot=slot: e.matmul(ps[:, hl * 256:(hl + 1) * 256], lhsT=xmT[:, h * 2 + kt, 4 + sub * 128:4 + (sub + 1) * 128],
                                                                                                   rhs=slot[:, (h * 2 + kt) * 256:(h * 2 + kt + 1) * 256], start=(kt == 0), stop=(kt == 1)),
                                   [skey, "xmT%d" % (h * 2 + kt)], [pk])
                        for hl in range(2):
                            h = hp * 2 + hl
                            dve(lambda e, ps=ps, sub=sub, h=h, hl=hl: e.tensor_scalar_mul(vp[:, sub, h, 0:256], ps[:, hl * 256:(hl + 1) * 256], wcol[:, sub, h:h + 1]),
                                [pk, "wcol"], VP)
                    dve(lambda e, sub=sub: e.tensor_copy(vp[:, sub, :, 256:257], wcol[:, sub, :].rearrange("p (h a) -> p h a", a=1)), ["wcol"], VP)
                for sub in range(4):
                    tok = slice(sub * 128, (sub + 1) * 128)
                    for h in range(4):
                        for dk in range(2):
                            act(lambda e, h=h, dk=dk, sub=sub: e.activation(Cb[:, h, dk, 0:257], C32[:, h, dk, :], AF.Identity, scale=dcol[:, sub, h:h + 1]),
                                ["C32_%d" % h, "dcol"], ["Cb%d" % h])
                        for ec in range(2):
                            pe(lambda e, h=h, ec=ec, tok=tok: e.matmul(psf[5][:, 0:128], lhsT=kT[:, h * 2 + ec, tok], rhs=qT[:, h * 2 + ec, tok],
                                                                       start=(ec == 0), stop=(ec == 1)),
                               [rk(8 + h * 2 + ec), rk(h * 2 + ec)], ["psf5"])
                        sTb, sTk = bfbuf()
                        dve(lambda e, sTb=sTb: e.tensor_tensor(sTb[:, 0:128], psf[5][:, 0:128], cmask[:], ALU.mult), ["psf5"], [sTk])
                        pn, pnk = psf[h % 2], "psf%d" % (h % 2)
                        pe(lambda e, pn=pn, sub=sub, h=h, sTb=sTb: e.matmul(pn[:, 0:257], lhsT=sTb[:, 0:128], rhs=vp[:, sub, h, 0:257], start=True, stop=False),
                           [sTk] + VP, [pnk])
                        for ec in range(2):
                            pe(lambda e, pn=pn, h=h, ec=ec, tok=tok: e.matmul(pn[:, 0:257], lhsT=qT[:, h * 2 + ec, tok], rhs=Cb[:, h, ec, 0:257],
                                                                              start=False, stop=(ec == 1)),
                               [rk(h * 2 + ec), "Cb%d" % h], [pnk])
                        for dk in range(2):
                            pu, puk = psf[2 + dk], "psf%d" % (2 + dk)
                            pe(lambda e, pu=pu, sub=sub, h=h, dk=dk: e.matmul(pu[:, 0:257], lhsT=k_tm[:, sub, h * 256 + dk * 128:h * 256 + (dk + 1) * 128],
                                                                              rhs=vp[:, sub, h, 0:257], start=True, stop=True),
                               [rk(16 + sub * 2 + h // 2)] + VP, [puk])
                            dve(lambda e, pu=pu, h=h, dk=dk, sub=sub: e.scalar_tensor_tensor(C32[:, h, dk, :], C32[:, h, dk, :], dcol[:, sub, h:h + 1], pu[:, 0:257], ALU.mult, ALU.add),
                                [puk, "dcol", "C32_%d" % h], ["C32_%d" % h])
                        st6, mv, dd, mx, rr = small[:, 112:118], small[:, 118:120], small[:, 120:121], small[:, 121:122], small[:, 122:123]
                        dve(lambda e, pn=pn: e.bn_stats(st6, pn[:, 0:256]), [pnk], ["st6"])
                        dve(lambda e: e.bn_aggr(mv, st6), ["st6"], ["mv"])
                        dve(lambda e, pn=pn: e.tensor_copy(dd, pn[:, 256:257]), [pnk], ["dd"])
                        dve(lambda e: e.scalar_tensor_tensor(mx, dd, -1.0, dd, ALU.mult, ALU.max), ["dd"], ["mx"])
                        dve(lambda e, sub=sub, h=h: e.tensor_tensor(mx, mx, e2c[:, sub, h:h + 1], ALU.max), ["mx", "e2c"], ["mx"])
                        dve(lambda e: e.tensor_tensor(mx, mx, mx, ALU.mult), ["mx"], ["mx"])
                        dve(lambda e: e.scalar_tensor_tensor(rr, mx, EPS, mv[:, 1:2], ALU.mult, ALU.add), ["mx", "mv"], ["rr"])
                        act(lambda e: e.activation(rr, rr, AF.Ln), ["rr"], ["rr"])
                        act(lambda e: e.activation(rr, rr, AF.Exp, scale=-0.5), ["rr"], ["rr"])
                        dve(lambda e, pn=pn, sub=sub, h=h: e.tensor_scalar(hn_tm[:, sub, h * 256:(h + 1) * 256], pn[:, 0:256], mv[:, 0:1], rr, ALU.subtract, ALU.mult),
                            [pnk, "mv", "rr"], XM)
                for fc in range(8):
                    pb = psb[fc % 2]
                    for sub in range(4):
                        pe(lambda e, fc=fc, sub=sub, pb=pb: e.transpose(pb[:, sub * 128:(sub + 1) * 128], hn_tm[:, sub, fc * 128:(fc + 1) * 128], ident[:]),
                           XM, ["psb%d" % (fc % 2)])
                    hb, hk = bfbuf()
                    act(lambda e, fc=fc, pb=pb, hb=hb: e.activation(hb, pb[:, 0:512], AF.Identity, scale=cc(C_HG, fc)), ["psb%d" % (fc % 2)], [hk])
                    dve(lambda e, fc=fc, hb=hb: e.tensor_tensor(hb, hb, som[:, fc, :], ALU.mult), [hk, "som%d" % fc], [hk])
                    dve(lambda e, fc=fc, hb=hb: e.scalar_tensor_tensor(aoutT[:, fc, :], xcT[:, fc, :], cc(C_SK, fc), hb, ALU.mult, ALU.add),
                        [hk, "xcT%d" % fc], ["xcT%d" % fc])
                if debug == "aout":
                    dump_T(aoutT, 8, ["xcT%d" % c for c in range(8)], stg)
                US = ["usT%d" % c for c in range(4)]
                def s5_half(half, stg=stg):
                    for gl_ in range(8):
                        gp = half * 8 + gl_
                        chunk, win = gp // 4, gp % 4
                        cl = gl_ // 4
                        for ri in range(2):
                            for j in range(8):
                                pe(lambda e, ri=ri, j=j, cl=cl, chunk=chunk, win=win: e.matmul(
                                    psf[win][:, (cl * 2 + ri) * 64:(cl * 2 + ri + 1) * 64], lhsT=WZt[32 * win:32 * win + 32, chunk, ri, j, :],
                                    rhs=usT[32 * win:32 * win + 32, chunk, :].rearrange("p (b j) -> p b j", j=8)[:, :, j],
                                    start=(j == 0), stop=(j == 7), tile_position=(32 * win, 0)), US, ["psf%d" % win])
                    gs = slice(half * 8, half * 8 + 8)
                    EcH = Ec[:, gs, :].rearrange("p g k -> p (g k)")
                    EsH = Es[:, gs, :].rearrange("p g k -> p (g k)")
                    RtH = Rtab[:, gs, :].rearrange("p g k -> p (g k)")
                    (bre, kbre), (bim, kbim), (ore, kore), (oim, koim), (tt, ktt) = f32buf(), f32buf(), f32buf(), f32buf(), f32buf()
                    w4 = lambda a, w: a.rearrange("p (c w k) -> p c w k", c=2, w=4)[:, :, w, :]
                    for w in range(4):
                        Zw = psf[w][:, 0:256].rearrange("p (c r k) -> p c r k", c=2, r=2)
                        Zre_w, Zim_w = Zw[:, :, 0, :], Zw[:, :, 1, :]
                        pk_ = "psf%d" % w
                        dve(lambda e, w=w, Zre_w=Zre_w: e.tensor_tensor(w4(bre, w), Zre_w, w4(EcH, w), ALU.mult), [pk_], [kbre])
                        dve(lambda e, w=w, Zim_w=Zim_w: e.tensor_tensor(w4(tt, w), Zim_w, w4(EsH, w), ALU.mult), [pk_], [ktt])
                        dve(lambda e, w=w, Zim_w=Zim_w: e.tensor_tensor(w4(bim, w), Zim_w, w4(EcH, w), ALU.mult), [pk_], [kbim])
                        dve(lambda e, w=w, Zre_w=Zre_w: e.tensor_tensor(w4(ore, w), Zre_w, w4(EsH, w), ALU.mult), [pk_], [kore])
                    dve(lambda e: e.tensor_tensor(bre, bre, tt, ALU.add), [kbre, ktt], [kbre])
                    dve(lambda e: e.tensor_tensor(bim, bim, ore, ALU.subtract), [kbim, kore], [kbim])
                    c0 = small[:, 128:136]
                    for (bb, SS, sk, bk) in ((bre, Sre, "Sre", kbre), (bim, Sim, "Sim", kbim)):
                        dve(lambda e, SS=SS: e.tensor_tensor(c0.rearrange("p (g a) -> p g a", a=1), SS[:, gs, 0:1], Rl[:, gs].rearrange("p (g a) -> p g a", a=1), ALU.mult),
                            [sk], ["c0"])
                        dve(lambda e, bb=bb: e.tensor_tensor(bb.rearrange("p (g k) -> p g k", g=8)[:, :, 0:1], bb.rearrange("p (g k) -> p g k", g=8)[:, :, 0:1],
                                                            c0.rearrange("p (g a) -> p g a", a=1), ALU.add), ["c0", bk], [bk])
                    dve(lambda e: e.tensor_tensor_scan(ore, RtH, bre, 0.0, ALU.mult, ALU.add), [kbre, kbim, kore], [kore])
                    dve(lambda e: e.tensor_tensor_scan(oim, RtH, bim, 0.0, ALU.mult, ALU.add), [kbim], [koim])
                    v3 = lambda a: a.rearrange("p (g k) -> p g k", g=8)
                    dve(lambda e: e.tensor_tensor(tt, oim, EsH, ALU.mult), [koim], [ktt])
                    dve(lambda e: e.tensor_tensor(bre, ore, EcH, ALU.mult), [kore], [kbre])
                    dve(lambda e: e.tensor_tensor(Sre[:, gs, 1:65], v3(bre), v3(tt), ALU.subtract), [kbre, ktt, "Sbre"], ["Sre"])
                    dve(lambda e: e.tensor_tensor(tt, ore, EsH, ALU.mult), [kore], [ktt])
                    dve(lambda e: e.tensor_tensor(bim, oim, EcH, ALU.mult), [koim], [kbim])
                    dve(lambda e: e.tensor_tensor(Sim[:, gs, 1:65], v3(bim), v3(tt), ALU.add), [kbim, ktt, "Sbim"], ["Sim"])
                    pool(lambda e: e.tensor_copy(Sbre[:, gs, :], Sre[:, gs, 0:64]), ["Sre"], ["Sbre"])
                    pool(lambda e: e.tensor_copy(Sbim[:, gs, :], Sim[:, gs, 0:64]), ["Sim"], ["Sbim"])
                    pool(lambda e: e.tensor_copy(Sre[:, gs, 0:1], Sre[:, gs, 64:65]), ["Sre", "Sbre"], ["Sre"])
                    pool(lambda e: e.tensor_copy(Sim[:, gs, 0:1], Sim[:, gs, 64:65]), ["Sim", "Sbim"], ["Sim"])
                    for cl in range(2):
                        chunk = half * 2 + cl
                        Y, yk = psf[4 + cl], "psf%d" % (4 + cl)
                        Y3 = Y[:, :].rearrange("p (b j) -> p b j", j=8)
                        U3 = usT[:, chunk, :].rearrange("p (b j) -> p b j", j=8)
                        for tau in range(8):
                            pe(lambda e, tau=tau, chunk=chunk, Y3=Y3, U3=U3: e.matmul(Y3[:, :, tau:8], lhsT=Kt[:, chunk, tau, :], rhs=U3[:, :, 0:8 - tau],
                                                                                      start=(tau == 0), stop=False), US, [yk])
                        for win in range(4):
                            gp = chunk * 4 + win
                            for j in range(8):
                                for ri in range(2):
                                    last = (win == 3 and j == 7 and ri == 1)
                                    SB = (Sbre, Sbim)[ri]
                                    pe(lambda e, win=win, gp=gp, j=j, ri=ri, SB=SB, Y3=Y3, last=last: e.matmul(
                                        Y3[32 * win:32 * win + 32, :, j], lhsT=WI[:, gp, ri, j, :], rhs=SB[:, gp, :],
                                        start=False, stop=last, tile_position=(0, 32 * win)), ["Sbre", "Sbim"], [yk])
                        (ysb, yk2), (z2, zk2) = f32buf(), f32buf()
                        sgg, sgk = bfbuf()
                        act(lambda e, Y=Y, ysb=ysb: e.activation(ysb, Y[:, :], AF.Identity), [yk], [yk2])
                        act(lambda e, Y=Y, z2=z2: e.activation(z2, Y[:, :], AF.Square), [yk], [zk2])
                        pool(lambda e, z2=z2: e.tensor_scalar(z2, z2, 0.044715, 1.0, ALU.mult, ALU.add), [zk2], [zk2])
                        pool(lambda e, z2=z2, ysb=ysb: e.tensor_tensor(z2, z2, ysb, ALU.mult), [zk2, yk2], [zk2])
                        sig(sgg, z2, [zk2], [sgk], scale=2.0 * C1G)
                        dve(lambda e, chunk=chunk, ysb=ysb, sgg=sgg: e.tensor_tensor(glT[:, chunk, :], ysb, sgg, ALU.mult), [yk2, sgk], ["glT%d" % chunk])
                for half in range(2):
                    s5_half(half)
                if debug == "gl":
                    dump_T(glT, 4, ["glT%d" % c for c in range(4)], stg)
                slot, skey = wnext()
                for jc in range(4):
                    pi, ps, pk = nextps()
                    for kt in range(4):
                        pe(lambda e, ps=ps, jc=jc, kt=kt, slot=slot: e.matmul(ps[:, :], lhsT=slot[:, (jc * 4 + kt) * 128:(jc * 4 + kt + 1) * 128], rhs=glT[:, kt, :],
                                                                             start=(kt == 0), stop=(kt == 3)), [skey, "glT%d" % kt], [pk])
                    sgg, sgk = bfbuf()
                    sig(sgg, ps[:, :], [pk], [sgk], bias=ch(C_BGLU, jc))
                    dve(lambda e, jc=jc, sgg=sgg: e.tensor_tensor(boutT[:, jc, :], glT[:, jc, :], sgg, ALU.mult), [sgk, "glT%d" % jc], ["usT%d" % jc])
                if debug == "bout":
                    dump_T(boutT, 4, US, stg)
                for mc in range(20, 36):
                    if mc % 2 == 0:
                        slot, skey = wnext()
                    ps, pk = inproj_chunk(mc, slot, skey)
                    sig(qT[:, mc - 20, :] if mc < 28 else kT[:, mc - 28, :], ps[:, :], [pk], [rk(mc - 20)], bias=ch(C_BIN, mc))
                for mc in range(8):
                    if mc % 2 == 0:
                        slot, skey = wnext()
                    pi, ps, pk = nextps()
                    for kt in range(8):
                        pe(lambda e, ps=ps, mc=mc, kt=kt, slot=slot: e.matmul(ps[:, :], lhsT=slot[:, ((mc % 2) * 8 + kt) * 128:((mc % 2) * 8 + kt + 1) * 128], rhs=aoutT[:, kt, :],
                                                                             start=(kt == 0), stop=(kt == 7)), [skey, "xcT%d" % kt], [pk])
                    dve(lambda e, ps=ps, mc=mc: e.tensor_tensor(mT[:, mc, :], ps[:, :], sga[:, mc, :], ALU.mult), [pk, rk(mc)], VP)
                for mc in range(8):
                    if mc % 4 == 0:
                        slot, skey = wnext()
                    pi, ps, pk = nextps()
                    for kt in range(4):
                        pe(lambda e, ps=ps, mc=mc, kt=kt, slot=slot: e.matmul(ps[:, :], lhsT=slot[:, ((mc % 4) * 4 + kt) * 128:((mc % 4) * 4 + kt + 1) * 128], rhs=boutT[:, kt, :],
                                                                             start=(kt == 0), stop=(kt == 3)), [skey, "usT%d" % kt], [pk])
                    tb_, tk_ = bfbuf()
                    dve(lambda e, ps=ps, mc=mc, tb_=tb_: e.tensor_tensor(tb_, ps[:, :], sgb[:, mc, :], ALU.mult), [pk, rk(8 + mc)], [tk_])
                    pool(lambda e, mc=mc, tb_=tb_: e.tensor_tensor(mT[:, mc, :], mT[:, mc, :], tb_, ALU.add), [tk_] + VP, VP)
                if debug == "merged":
                    dump_T(mT, 8, VP, stg)
                for nh in range(2):
                    pss = [nextps() for _ in range(4)]
                    for kq in range(2):
                        slot, skey = wnext()
                        for k4 in range(4):
                            kt = kq * 4 + k4
                            for sub in range(4):
                                pi, ps, pk = pss[sub]
                                pe(lambda e, ps=ps, kt=kt, k4=k4, sub=sub, slot=slot: e.matmul(ps[:, :], lhsT=mT[:, kt, sub * 128:(sub + 1) * 128], rhs=slot[:, k4 * 512:(k4 + 1) * 512],
                                                                                              start=(kt == 0), stop=(kt == 7)), [skey] + VP, [pk])
                    for sub in range(4):
                        pi, ps, pk = pss[sub]
                        dve(lambda e, ps=ps, sub=sub, nh=nh: e.tensor_tensor(x_sb[:, sub, nh * 512:(nh + 1) * 512], x_sb[:, sub, nh * 512:(nh + 1) * 512], ps[:, :], ALU.add),
                            [pk, "x_sb%d" % sub], ["x_sb%d" % sub])
                if debug == "x1":
                    for sub in range(4):
                        S.dma("pool", y_d[t0 + sub * 128:t0 + (sub + 1) * 128, :], x_sb[:, sub, :], reads=["x_sb%d" % sub], writes=["y"])
                norm_T(C_G2)
                for i in range(22):
                    slot, skey = wnext()
                    accs = (f32buf(), f32buf())
                    for vg in range(2):
                        mc = i + 22 * vg
                        pi, ps, pk = nextps()
                        for kt in range(8):
                            pe(lambda e, ps=ps, vg=vg, kt=kt, slot=slot: e.matmul(ps[:, :], lhsT=slot[:, (vg * 8 + kt) * 128:(vg * 8 + kt + 1) * 128], rhs=hT[:, kt, :],
                                                                                 start=(kt == 0), stop=(kt == 7)), [skey, "hT%d" % kt], [pk])
                        acc, akey = accs[vg]
                        act(lambda e, ps=ps, mc=mc, acc=acc: e.activation(acc, ps[:, :], AF.Identity, bias=cc(C_FCB, mc), scale=cc(C_FCW, mc * 3 + 2)),
                            [pk], [akey])
                        dve(lambda e, ps=ps, mc=mc, acc=acc: e.scalar_tensor_tensor(acc[:, 1:512], ps[:, 0:511], cc(C_FCW, mc * 3 + 1), acc[:, 1:512], ALU.mult, ALU.add),
                            [pk, akey], [akey])
                        dve(lambda e, ps=ps, mc=mc, acc=acc: e.scalar_tensor_tensor(acc[:, 2:512], ps[:, 0:510], cc(C_FCW, mc * 3 + 0), acc[:, 2:512], ALU.mult, ALU.add),
                            [pk, akey], [akey])
                        dve(lambda e, mc=mc, acc=acc: e.scalar_tensor_tensor(acc[:, 0:1], fcar[:, mc, 1:2], cc(C_FCW, mc * 3 + 1), acc[:, 0:1], ALU.mult, ALU.add),
                             ["fcar", akey], [akey])
                        dve(lambda e, mc=mc, acc=acc: e.scalar_tensor_tensor(acc[:, 0:2], fcar[:, mc, 0:2], cc(C_FCW, mc * 3 + 0), acc[:, 0:2], ALU.mult, ALU.add),
                             ["fcar", akey], [akey])
                        act(lambda e, ps=ps, mc=mc: e.copy(fcar[:, mc, 0:2], ps[:, 510:512]), [pk, akey], ["fcar"])
                    (av, avk), (ag, agk) = accs
                    sgg, sgk = bfbuf()
                    sig(sgg, ag, [agk], [sgk])
                    pool(lambda e, ag=ag, sgg=sgg: e.tensor_tensor(ag, ag, sgg, ALU.mult), [agk, sgk], [agk])
                    pool(lambda e, i=i, ag=ag, av=av: e.tensor_tensor(gT[:, i, :], ag, av, ALU.mult), [avk, agk], [rk(i)])
                for nh in range(2):
                    pss = [nextps() for _ in range(4)]
                    for kq in range(6):
                        slot, skey = wnext()
                        for k4 in range(4):
                            kt = kq * 4 + k4
                            if kt >= 22:
                                continue
                            for sub in range(4):
                                pi, ps, pk = pss[sub]
                                pe(lambda e, ps=ps, kt=kt, k4=k4, sub=sub, slot=slot: e.matmul(ps[:, :], lhsT=gT[:, kt, sub * 128:(sub + 1) * 128], rhs=slot[:, k4 * 512:(k4 + 1) * 512],
                                                                                              start=(kt == 0), stop=(kt == 21)), [skey, rk(kt)], [pk])
                    for sub in range(4):
                        pi, ps, pk = pss[sub]
                        dve(lambda e, ps=ps, sub=sub, nh=nh: e.tensor_tensor(x_sb[:, sub, nh * 512:(nh + 1) * 512], x_sb[:, sub, nh * 512:(nh + 1) * 512], ps[:, :], ALU.add),
                            [pk, "x_sb%d" % sub], ["x_sb%d" % sub])
                if debug is None:
                    rs = rms_rstd()
                    for sub in range(4):
                        for nh in range(2):
                            ob, okk = f32buf()
                            dve(lambda e, sub=sub, ob=ob, nh=nh: e.scalar_tensor_tensor(ob, x_sb[:, sub, nh * 512:(nh + 1) * 512], rs[:, sub:sub + 1],
                                                                                       crow[:, nh * 512:(nh + 1) * 512], ALU.mult, ALU.mult),
                                ["x_sb%d" % sub, "rs"], [okk])
                            S.dma("pool", y_d[t0 + sub * 128:t0 + (sub + 1) * 128, nh * 512:(nh + 1) * 512], ob, reads=[okk], writes=["y"])
            S.emit()
    return nc


_CACHE = {}


def kernel(**inputs):
    prep = _prep(inputs)
    x = np.ascontiguousarray(np.asarray(inputs["x"], dtype=np.float32))
    if "nc" not in _CACHE:
        _CACHE["nc"] = build_nc()
    nc = _CACHE["nc"]
    in_maps = []
    for c in range(8):
        m = {"x": x[c]}
        m.update(prep)
        in_maps.append(m)
    res = run_bass_kernel_spmd(nc, in_maps, core_ids=list(range(8)))
    return np.stack([np.asarray(r["y"], dtype=np.float32) for r in res.results], axis=0)
```

```python
import contextlib
import math
import numpy as np
import concourse.bass as bass
import concourse.mybir as mybir
from concourse.bass_utils import run_bass_kernel_spmd

F32 = mybir.dt.float32
BF16 = mybir.dt.bfloat16
ALU = mybir.AluOpType
AF = mybir.ActivationFunctionType

SEQ = 4096
DM = 1024
TT = 512
NST = SEQ // TT
NS = 67
RING = 4
EPS = 1e-6


class _Op:
    __slots__ = ("eng", "fn", "is_dma", "deps", "inc", "sem", "semval")

    def __init__(self, eng, fn, is_dma):
        self.eng = eng
        self.fn = fn
        self.is_dma = is_dma
        self.deps = []
        self.inc = False
        self.sem = None
        self.semval = 0


class Sched:
    ENGS = ("pe", "act", "dve", "pool", "sp")
    N_DMA_SEMS = 24

    _uid = [0]

    @staticmethod
    def make_sems(nc, st):
        csem = {e: st.enter_context(nc.semaphore("cs_" + e)) for e in ("pe", "act", "dve", "pool")}
        dsem = {e: [st.enter_context(nc.semaphore("ds_%s%d" % (e, i))) for i in range(Sched.N_DMA_SEMS)]
                for e in ("sp", "pool")}
        return dict(csem=csem, dsem=dsem, ccount={e: 0 for e in csem}, dcount={e: [0] * Sched.N_DMA_SEMS for e in dsem})

    def __init__(self, nc, sems=None):
        self.nc = nc
        self.sems = sems
        Sched._uid[0] += 1
        self.uid = Sched._uid[0]
        self.ops = {e: [] for e in self.ENGS}
        self.last_w = {}
        self.readers = {}

    def _add(self, eng, fn, is_dma, reads, writes):
        op = _Op(eng, fn, is_dma)
        deps = []
        raw = set()
        for k in reads:
            w = self.last_w.get(k)
            if w is not None:
                deps.append(w)
                raw.add(id(w))
        for k in writes:
            w = self.last_w.get(k)
            if w is not None:
                deps.append(w)
            deps.extend(self.readers.get(k, ()))
        for k in writes:
            self.last_w[k] = op
            self.readers[k] = []
        for k in reads:
            if k not in writes:
                self.readers.setdefault(k, []).append(op)
        seen = set()
        for d in deps:
            if d is op or id(d) in seen:
                continue
            seen.add(id(d))
            if d.eng == eng and not d.is_dma and not is_dma:
                if eng == "pe" or id(d) not in raw:
                    continue
            op.deps.append(d)
            d.inc = True
        self.ops[eng].append(op)
        return op

    def op(self, eng, fn, reads=(), writes=()):
        return self._add(eng, fn, False, tuple(reads), tuple(writes))

    def dma(self, eng, out, in_, reads=(), writes=()):
        return self._add(eng, lambda e: e.dma_start(out=out, in_=in_), True, tuple(reads), tuple(writes))

    def emit(self):
        nc = self.nc
        with contextlib.ExitStack() as st:
            pool_ = self.sems
            csem, dsem, ccount, dcount = pool_["csem"], pool_["dsem"], pool_["ccount"], pool_["dcount"]
            for e in self.ENGS:
                nd = 0
                for op in self.ops[e]:
                    if op.is_dma:
                        i = nd % self.N_DMA_SEMS
                        nd += 1
                        dcount[e][i] += 16
                        op.sem = dsem[e][i]
                        op.semval = dcount[e][i]
                    elif op.inc:
                        ccount[e] += 1
                        op.sem = csem[e]
                        op.semval = ccount[e]
            block = st.enter_context(nc.Block())

            def run(ename, eng):
                waited = {}
                last = {}
                for op in self.ops[ename]:
                    need = {}
                    for d in op.deps:
                        key = id(d.sem)
                        if waited.get(key, 0) >= d.semval:
                            continue
                        if key not in need or need[key][1] < d.semval:
                            need[key] = (d.sem, d.semval)
                    for key, (sem, val) in need.items():
                        eng.wait_ge(sem, val)
                        waited[key] = val
                    ins = op.fn(eng)
                    if op.is_dma:
                        ins.then_inc(op.sem, 16)
                        last[id(op.sem)] = (op.sem, op.semval)
                    elif op.inc:
                        ins.then_inc(op.sem, 1)
                for key, (sem, val) in last.items():
                    if waited.get(key, 0) < val:
                        eng.wait_ge(sem, val)

            block.tensor(lambda e: run("pe", e))
            block.scalar(lambda e: run("act", e))
            block.vector(lambda e: run("dve", e))
            block.gpsimd(lambda e: run("pool", e))
            block.sync(lambda e: run("sp", e))


C_BIN, C_G1, C_G2, C_MCW, C_MCB, C_HG, C_SK, C_FCW, C_FCB, C_BGLU, C_SD = 0, 36, 44, 52, 84, 92, 100, 108, 240, 284, 288
NCOL = 292
IN_PERM = np.concatenate([np.arange(0, 2048), np.arange(2056, 4616)])
SL_A, SL_B, SL_KT, SL_VT, SL_GLU, SL_BRA, SL_BRB, SL_WO, SL_UP, SL_DN = 0, 18, 20, 21, 22, 23, 27, 29, 33, 55
ORDER = (list(range(0, 10)) + [18, 19, 20, 21, 22] + list(range(10, 18)) + list(range(23, 67)))


def _prep(inp):
    f = lambda a: np.ascontiguousarray(np.asarray(a, dtype=np.float32))
    w_in = f(inp["w_in"][0])
    w_in_p = w_in[:, IN_PERM]
    b_in = f(inp["b_in"][0])
    slabs = np.zeros((NS, 128, 2048), np.float32)

    def put(s, col, blk):
        slabs[s, :, col:col + blk.shape[1]] = blk

    for mc in range(36):
        for kt in range(8):
            put(SL_A + mc // 2, ((mc % 2) * 8 + kt) * 128, w_in_p[kt * 128:(kt + 1) * 128, mc * 128:(mc + 1) * 128])
    wq, wk, wv = f(inp["m_wq"][0]), f(inp["m_wk"][0]), f(inp["m_wv"][0])
    for h in range(4):
        for qk, W in enumerate((wq, wk)):
            for ec in range(2):
                for kt in range(2):
                    col = ((((h % 2) * 2 + qk) * 2 + ec) * 2 + kt) * 128
                    put(SL_B + h // 2, col, W[h, kt * 128:(kt + 1) * 128, ec * 128:(ec + 1) * 128])
        for kt in range(2):
            put(SL_KT, (h * 2 + kt) * 256, wk[h, kt * 128:(kt + 1) * 128, :])
            put(SL_VT, (h * 2 + kt) * 256, wv[h, kt * 128:(kt + 1) * 128, :])
    wglu = f(inp["s_w_glu"][0])
    for jc in range(4):
        for kt in range(4):
            put(SL_GLU, (jc * 4 + kt) * 128, wglu[kt * 128:(kt + 1) * 128, jc * 128:(jc + 1) * 128])
    wa, wb, wo = f(inp["w_branch_a"][0]), f(inp["w_branch_b"][0]), f(inp["w_out"][0])
    for mc in range(8):
        for kt in range(8):
            put(SL_BRA + mc // 2, ((mc % 2) * 8 + kt) * 128, wa[kt * 128:(kt + 1) * 128, mc * 128:(mc + 1) * 128])
        for kt in range(4):
            put(SL_BRB + mc // 4, ((mc % 4) * 4 + kt) * 128, wb[kt * 128:(kt + 1) * 128, mc * 128:(mc + 1) * 128])
    for nh in range(2):
        for kt in range(8):
            put(SL_WO + nh * 2 + kt // 4, (kt % 4) * 512, wo[kt * 128:(kt + 1) * 128, nh * 512:(nh + 1) * 512])
    wup, wdn = f(inp["w_up"][0]), f(inp["w_down"][0])
    for i in range(22):
        for vg in range(2):
            mc = i + 22 * vg
            for kt in range(8):
                put(SL_UP + i, (vg * 8 + kt) * 128, wup[kt * 128:(kt + 1) * 128, mc * 128:(mc + 1) * 128])
    for nh in range(2):
        for kt in range(22):
            put(SL_DN + nh * 6 + kt // 4, (kt % 4) * 512, wdn[kt * 128:(kt + 1) * 128, nh * 512:(nh + 1) * 512])

    ccol = np.zeros((128, NCOL), np.float32)
    col = lambda v, n: f(v).reshape(n, 128).T
    ccol[:, C_BIN:C_BIN + 36] = col(b_in[IN_PERM], 36)
    ccol[:, C_G1:C_G1 + 8] = col(inp["mix_norm_g"][0], 8)
    ccol[:, C_G2:C_G2 + 8] = col(inp["ffn_norm_g"][0], 8)
    mcw = f(inp["m_conv_w"][0])
    ccol[:, C_MCW:C_MCW + 32] = mcw.T.reshape(8, 128, 4).transpose(1, 0, 2).reshape(128, 32)
    ccol[:, C_MCB:C_MCB + 8] = col(inp["m_conv_b"][0], 8)
    ccol[:, C_HG:C_HG + 8] = col(inp["m_head_g"][0], 8)
    ccol[:, C_SK:C_SK + 8] = col(inp["m_skip"][0], 8)
    fcw = f(inp["ffn_conv_w"][0])
    ccol[:, C_FCW:C_FCW + 132] = fcw.T.reshape(44, 128, 3).transpose(1, 0, 2).reshape(128, 132)
    ccol[:, C_FCB:C_FCB + 44] = col(inp["ffn_conv_b"][0], 44)
    ccol[:, C_BGLU:C_BGLU + 4] = col(inp["s_b_glu"][0], 4)
    ccol[:, C_SD:C_SD + 4] = col(f(inp["s_d"][0]).reshape(-1), 4)
    crow = np.zeros((128, 1032), np.float32)
    crow[:, 0:1024] = f(inp["final_norm_g"])[None, :]
    crow[:, 1024:1032] = b_in[2048:2056][None, :]
    wg = np.ascontiguousarray(w_in[:, 2048:2056].reshape(8, 128, 8).transpose(1, 0, 2).reshape(128, 64))

    are, aim, ldt = f(inp["s_a_re"][0]), f(inp["s_a_im"][0]), f(inp["s_log_dt"][0])
    bre, bim = f(inp["s_b_re"][0]), f(inp["s_b_im"][0])
    cre, cim = f(inp["s_c_re"][0]), f(inp["s_c_im"][0])
    toL = lambda a: a.reshape(16, 2, 64).transpose(1, 2, 0).reshape(128, 16)
    rep = lambda a: np.broadcast_to(toL(a)[:, :, None], (128, 16, 32)).reshape(128, 512)
    s5X = np.stack([rep(are), rep(aim), rep(np.broadcast_to(ldt[:, None], (32, 64)))], axis=1)

    def toT(a):
        t = a.reshape(4, 4, 2, 64)
        t = np.broadcast_to(t[:, :, None, None, :, :], (4, 4, 2, 16, 2, 64))
        return t.transpose(1, 2, 3, 0, 4, 5).reshape(128, 4, 128)

    s5T = np.stack([toT(are), toT(aim), toT(np.broadcast_to(ldt[:, None], (32, 64)))], axis=1)

    def bT(b):
        t = b.reshape(4, 4, 2, 64, 16)
        o = np.zeros((4, 2, 16, 4, 2, 64), np.float32)
        for g2 in range(2):
            o[:, g2, :, :, g2, :] = t[:, :, g2].transpose(1, 3, 0, 2)
        return o.reshape(128, 4, 128)

    def bX(b):
        t = b.reshape(16, 2, 64, 16)
        o = np.zeros((2, 64, 16, 2, 16), np.float32)
        for g2 in range(2):
            o[g2, :, :, g2, :] = t[:, g2].transpose(1, 0, 2)
        return o.reshape(128, 16, 32)

    BtD = np.stack([bT(bre), bT(bim)], axis=1)
    BxD = np.stack([bX(bre), bX(bim)], axis=1)
    CxD = np.stack([bX(cre.transpose(0, 2, 1)), bX(cim.transpose(0, 2, 1))], axis=1)
    return dict(wall=slabs, ccol=ccol, crow=crow, wg=wg,
                s5X=np.ascontiguousarray(s5X), s5T=np.ascontiguousarray(s5T.reshape(128, 3, 512)),
                BtD=np.ascontiguousarray(BtD.reshape(128, 2, 512)), BxD=np.ascontiguousarray(BxD.reshape(128, 2, 512)),
                CxD=np.ascontiguousarray(CxD.reshape(128, 2, 512)))


C1G = math.sqrt(2.0 / math.pi)


def _cmul(dve, o_re, o_im, a_re, a_im, b_re, b_im, t1, t2):
    dve(lambda e: e.tensor_tensor(t1, a_re, b_re, ALU.mult))
    dve(lambda e: e.tensor_tensor(t2, a_im, b_im, ALU.mult))
    dve(lambda e: e.tensor_tensor(t1, t1, t2, ALU.subtract))
    dve(lambda e: e.tensor_tensor(t2, a_re, b_im, ALU.mult))
    dve(lambda e: e.tensor_tensor(o_im, a_im, b_re, ALU.mult))
    dve(lambda e: e.tensor_tensor(o_im, o_im, t2, ALU.add))
    dve(lambda e: e.tensor_copy(o_re, t1))


def _s5_params(S, par, W):
    k = ["s5"]
    dve = lambda fn: S.op("dve", fn, k, k)
    act = lambda fn: S.op("act", fn, k, k)
    are, aim, ldt = par[:, 0, :], par[:, 1, :], par[:, 2, :]
    dt, dre, dim, mag, c, s, t1, t2 = (W["w%d" % i][:] for i in range(8))
    act(lambda e: e.activation(dt, ldt, AF.Exp))
    dve(lambda e: e.tensor_tensor(dre, dt, are, ALU.mult))
    dve(lambda e: e.tensor_tensor(dim, dt, aim, ALU.mult))
    act(lambda e: e.activation(mag, dre, AF.Exp))
    act(lambda e: e.activation(W["R8"][:], dre, AF.Exp, scale=8.0))
    act(lambda e: e.activation(s, dim, AF.Sin, scale=1.0 / 16.0))
    act(lambda e: e.activation(c, dim, AF.Sin, bias=W["hpi"], scale=1.0 / 16.0))

    def square():
        dve(lambda e: e.tensor_tensor(t1, c, c, ALU.mult))
        dve(lambda e: e.tensor_tensor(t2, s, s, ALU.mult))
        dve(lambda e: e.scalar_tensor_tensor(s, c, 2.0, s, ALU.mult, ALU.mult))
        dve(lambda e: e.tensor_tensor(c, t1, t2, ALU.subtract))

    for _ in range(4):
        square()
    dve(lambda e: e.tensor_tensor(W["ab_re"][:], mag, c, ALU.mult))
    dve(lambda e: e.tensor_tensor(W["ab_im"][:], mag, s, ALU.mult))
    for _ in range(3):
        square()
    dve(lambda e: e.tensor_copy(W["c8"][:], c))
    dve(lambda e: e.tensor_copy(W["s8"][:], s))
    xr, den = dt, dre
    dve(lambda e: e.tensor_scalar_add(xr, W["ab_re"][:], -1.0))
    dve(lambda e: e.tensor_tensor(t1, are, are, ALU.mult))
    dve(lambda e: e.tensor_tensor(t2, aim, aim, ALU.mult))
    dve(lambda e: e.tensor_tensor(den, t1, t2, ALU.add))
    dve(lambda e: e.reciprocal(den, den))
    dve(lambda e: e.tensor_tensor(t1, xr, are, ALU.mult))
    dve(lambda e: e.tensor_tensor(t2, W["ab_im"][:], aim, ALU.mult))
    dve(lambda e: e.tensor_tensor(t1, t1, t2, ALU.add))
    dve(lambda e: e.tensor_tensor(W["r_re"][:], t1, den, ALU.mult))
    dve(lambda e: e.tensor_tensor(t1, W["ab_im"][:], are, ALU.mult))
    dve(lambda e: e.tensor_tensor(t2, xr, aim, ALU.mult))
    dve(lambda e: e.tensor_tensor(t1, t1, t2, ALU.subtract))
    dve(lambda e: e.tensor_tensor(W["r_im"][:], t1, den, ALU.mult))


def build_nc(debug=None, nst=NST):
    nc = bass.Bass("TRN2", target_bir_lowering=False)
    din = lambda n, s: nc.dram_tensor(n, s, F32, kind="ExternalInput").ap()
    x_d = din("x", [SEQ, DM])
    wall_d = din("wall", [NS, 128, 2048])
    ccol_d = din("ccol", [128, NCOL])
    crow_d = din("crow", [128, 1032])
    wg_d = din("wg", [128, 64])
    s5X_d = din("s5X", [128, 3, 512])
    s5T_d = din("s5T", [128, 3, 512])
    Bt_d = din("BtD", [128, 2, 512])
    Bx_d = din("BxD", [128, 2, 512])
    Cx_d = din("CxD", [128, 2, 512])
    y_d = nc.dram_tensor("y", [SEQ, DM], F32, kind="ExternalOutput").ap()
    wsl_d = nc.dram_tensor("wsl", [NS, 128, 2048], BF16, kind="Internal").ap()

    with contextlib.ExitStack() as st0:
        def T0(name, shape, dt=F32):
            return st0.enter_context(nc.sbuf_tensor("s_" + name, shape, dt))

        SEMS = Sched.make_sems(nc, st0)
        WZt = T0("WZt", [128, 4, 2, 8, 128], BF16)
        WI = T0("WI", [128, 16, 2, 8, 32], BF16)
        Kt = T0("Kt", [128, 4, 8, 128], BF16)
        Ec = T0("Ec", [128, 16, 64])
        Es = T0("Es", [128, 16, 64])
        Rtab = T0("Rtab", [128, 16, 64])
        Rl = T0("Rl", [128, 16])
        ccol = T0("ccol", [128, NCOL])
        chalf = T0("chalf", [128, NCOL])
        crow = T0("crow", [128, 1032])
        wgb = T0("wgb", [128, 64], BF16)
        ident = T0("ident", [128, 128], BF16)
        cmask = T0("cmask", [128, 128], BF16)
        cmask4 = T0("cmask4", [128, 512], BF16)
        LT = T0("LT", [128, 128])
        ONES = T0("ONES", [128, 128])
        small = T0("small", [128, 256])
        psf = [st0.enter_context(nc.psum_tensor("psf%d" % i, [128, 512], F32)) for i in range(6)]
        psb = [st0.enter_context(nc.psum_tensor("psb%d" % i, [128, 1024], BF16)) for i in range(2)]

        def cc(off, i=0):
            return ccol[:, off + i:off + i + 1]

        def ch(off, i=0):
            return chalf[:, off + i:off + i + 1]

        with contextlib.ExitStack() as st1:
            def T1(name, shape, dt=F32):
                return st1.enter_context(nc.sbuf_tensor("a_" + name, shape, dt))

            S = Sched(nc, SEMS)
            dve = lambda fn, r=(), w=(): S.op("dve", fn, r, w)
            act = lambda fn, r=(), w=(): S.op("act", fn, r, w)
            pool = lambda fn, r=(), w=(): S.op("pool", fn, r, w)
            pe = lambda fn, r=(), w=(): S.op("pe", fn, r, w)
            NSTG = 3
            w32 = [T1("w32_%d" % i, [128, 2048]) for i in range(NSTG)]
            w16 = [T1("w16_%d" % i, [128, 2048], BF16) for i in range(NSTG)]
            S.dma("sp", ccol[:], ccol_d, writes=["ccol"])
            S.dma("sp", crow[:], crow_d, writes=["crow"])
            S.dma("sp", w32[0][:, 0:64], wg_d, writes=["w32_0"])
            act(lambda e: e.copy(wgb[:], w32[0][:, 0:64]), ["w32_0"], ["wgb"])
            act(lambda e: e.mul(chalf[:], ccol[:], 0.5), ["ccol"], ["chalf"])
            pool(lambda e: e.memset(ident[:], 0.0), [], ["ident"])
            pool(lambda e: e.affine_select(out=ident[:], in_=ident[:], compare_op=ALU.not_equal, fill=1.0, base=0,
                                           pattern=[[-1, 128]], channel_multiplier=1), ["ident"], ["ident"])
            pool(lambda e: e.memset(cmask[:], 1.0), [], ["cmask"])
            pool(lambda e: e.affine_select(out=cmask[:], in_=cmask[:], compare_op=ALU.is_ge, fill=0.0, base=0,
                                           pattern=[[1, 128]], channel_multiplier=-1), ["cmask"], ["cmask"])
            pool(lambda e: e.memset(cmask4[:], 1.0), [], ["cmask4"])
            pool(lambda e: e.affine_select(out=cmask4[:, :].rearrange("p (h q) -> p h q", h=4), in_=cmask4[:, :].rearrange("p (h q) -> p h q", h=4),
                                           compare_op=ALU.is_ge, fill=0.0, base=0, pattern=[[0, 4], [1, 128]], channel_multiplier=-1), ["cmask4"], ["cmask4"])
            pool(lambda e: e.memset(LT[:], 1.0), [], ["LT"])
            pool(lambda e: e.affine_select(out=LT[:], in_=LT[:], compare_op=ALU.is_ge, fill=0.0, base=-1,
                                           pattern=[[-1, 128]], channel_multiplier=1), ["LT"], ["LT"])
            pool(lambda e: e.memset(ONES[:], 1.0), [], ["ONES"])
            pool(lambda e: e.memset(small[:], 0.0), [], ["small"])
            pool(lambda e: e.memset(small[:, 250:251], EPS), ["small"], ["small"])
            pool(lambda e: e.memset(small[:, 251:252], 1.0), ["small"], ["small"])
            pool(lambda e: e.memset(small[:, 252:253], math.pi / 2.0), ["small"], ["small"])
            k5 = ["s5"]
            d5 = lambda fn: S.op("dve", fn, k5, k5)
            a5 = lambda fn: S.op("act", fn, k5, k5)
            parT = T1("parT", [128, 3, 512])
            parX = T1("parX", [128, 3, 512])
            BtS = T1("BtS", [128, 2, 512])
            BxS = T1("BxS", [128, 2, 512])
            CxS = T1("CxS", [128, 2, 512])
            S.dma("sp", parT[:], s5T_d, writes=["ld0"])
            S.dma("sp", parX[:], s5X_d, writes=["ld1"])
            S.dma("sp", BtS[:], Bt_d, writes=["ld2"])
            S.dma("sp", BxS[:], Bx_d, writes=["ld3"])
            S.dma("sp", CxS[:], Cx_d, writes=["ld4"])
            S.op("dve", lambda e: e.memset(small[:, 253:254], 0.0), ["ld0", "ld1", "ld2", "ld3", "ld4", "small"], k5)
            for s_ in range(NS):
                S.dma("pool", wsl_d[s_], wall_d[s_], reads=[], writes=["wsl%d" % s_])
            names = ["w%d" % i for i in range(8)] + ["ab_re", "ab_im", "r_re", "r_im", "c8", "s8", "R8"]
            WT = {n: T1("T_" + n, [128, 512]) for n in names}
            WX = {n: T1("X_" + n, [128, 512]) for n in names}
            WT["hpi"] = small[:, 252:253]
            WX["hpi"] = small[:, 252:253]
            _s5_params(S, parT, WT)
            _s5_params(S, parX, WX)
            cur_re, cur_im, t1, t2 = T1("cur_re", [128, 512]), T1("cur_im", [128, 512]), T1("t1", [128, 512]), T1("t2", [128, 512])
            _cmul(d5, cur_re[:], cur_im[:], WT["r_re"][:], WT["r_im"][:], BtS[:, 0, :], BtS[:, 1, :], t1[:], t2[:])
            for kk in range(8):
                j = 7 - kk
                a5(lambda e, j=j: e.copy(WZt[:, :, 0, j, :], cur_re[:, :].rearrange("p (c m) -> p c m", c=4)))
                a5(lambda e, j=j: e.copy(WZt[:, :, 1, j, :], cur_im[:, :].rearrange("p (c m) -> p c m", c=4)))
                if kk < 7:
                    _cmul(d5, cur_re[:], cur_im[:], cur_re[:], cur_im[:], WT["ab_re"][:], WT["ab_im"][:], t1[:], t2[:])
            _cmul(d5, cur_re[:], cur_im[:], CxS[:, 0, :], CxS[:, 1, :], WX["ab_re"][:], WX["ab_im"][:], t1[:], t2[:])
            for j in range(8):
                a5(lambda e, j=j: e.copy(WI[:, :, 0, j, :], cur_re[:, :].rearrange("p (g m) -> p g m", g=16)))
                a5(lambda e, j=j: e.mul(WI[:, :, 1, j, :], cur_im[:, :].rearrange("p (g m) -> p g m", g=16), -1.0))
                if j < 7:
                    _cmul(d5, cur_re[:], cur_im[:], cur_re[:], cur_im[:], WX["ab_re"][:], WX["ab_im"][:], t1[:], t2[:])
            Cb_re = T1("Cb_re", [128, 512], BF16)
            nCb_im = T1("nCb_im", [128, 512], BF16)
            Xb_re = T1("Xb_re", [128, 512], BF16)
            Xb_im = T1("Xb_im", [128, 512], BF16)
            a5(lambda e: e.copy(Cb_re[:], CxS[:, 0, :]))
            a5(lambda e: e.mul(nCb_im[:], CxS[:, 1, :], -1.0))
            _cmul(d5, cur_re[:], cur_im[:], WX["r_re"][:], WX["r_im"][:], BxS[:, 0, :], BxS[:, 1, :], t1[:], t2[:])
            for tau in range(8):
                a5(lambda e: e.copy(Xb_re[:], cur_re[:]))
                a5(lambda e: e.copy(Xb_im[:], cur_im[:]))
                d5(lambda e: e.memset(psf[0][:, :], 0.0))
                for gp in range(16):
                    chunk, win = gp // 4, gp % 4
                    o = psf[0][32 * win:32 * win + 32, chunk * 128 + 32 * win:chunk * 128 + 32 * win + 32]
                    S.op("pe", lambda e, o=o, gp=gp, win=win: e.matmul(o, lhsT=Xb_re[:, gp * 32:(gp + 1) * 32], rhs=Cb_re[:, gp * 32:(gp + 1) * 32],
                                                                     start=True, stop=False, tile_position=(0, 32 * win)), k5, k5)
                    S.op("pe", lambda e, o=o, gp=gp, win=win: e.matmul(o, lhsT=Xb_im[:, gp * 32:(gp + 1) * 32], rhs=nCb_im[:, gp * 32:(gp + 1) * 32],
                                                                     start=False, stop=True, tile_position=(0, 32 * win)), k5, k5)
                if tau == 0:
                    for chunk in range(4):
                        S.op("dve", lambda e, chunk=chunk: e.scalar_tensor_tensor(Kt[:, chunk, 0, :], ident[:], cc(C_SD, chunk), psf[0][:, chunk * 128:(chunk + 1) * 128],
                                                                               ALU.mult, ALU.add), k5 + ["ident", "ccol"], k5)
                else:
                    d5(lambda e, tau=tau: e.tensor_copy(Kt[:, :, tau, :], psf[0][:, :].rearrange("p (c m) -> p c m", c=4)))
                if tau < 7:
                    _cmul(d5, cur_re[:], cur_im[:], cur_re[:], cur_im[:], WX["ab_re"][:], WX["ab_im"][:], t1[:], t2[:])
            c8 = WX["c8"][:, :].rearrange("p (g m) -> p g m", g=16)
            s8 = WX["s8"][:, :].rearrange("p (g m) -> p g m", g=16)
            R8 = WX["R8"][:, :].rearrange("p (g m) -> p g m", g=16)
            d5(lambda e: e.tensor_copy(Ec[:, :, 0:1], c8[:, :, 0:1]))
            d5(lambda e: e.tensor_copy(Es[:, :, 0:1], s8[:, :, 0:1]))
            d5(lambda e: e.tensor_copy(Rl[:, :].rearrange("p (g a) -> p g a", a=1), R8[:, :, 0:1]))
            d5(lambda e: e.memset(Rtab[:], 0.0))
            m = 1
            while m < 64:
                bre_b = Ec[:, :, m - 1:m].to_broadcast([128, 16, m])
                bim_b = Es[:, :, m - 1:m].to_broadcast([128, 16, m])
                u1 = t1[:, 0:16 * m].rearrange("p (g k) -> p g k", g=16)
                u2 = t2[:, 0:16 * m].rearrange("p (g k) -> p g k", g=16)
                d5(lambda e, m=m, bre_b=bre_b, u1=u1: e.tensor_tensor(u1, Ec[:, :, 0:m], bre_b, ALU.mult))
                d5(lambda e, m=m, bim_b=bim_b, u2=u2: e.tensor_tensor(u2, Es[:, :, 0:m], bim_b, ALU.mult))
                d5(lambda e, m=m, u1=u1, u2=u2: e.tensor_tensor(u1, u1, u2, ALU.subtract))
                d5(lambda e, m=m, bre_b=bre_b, u2=u2: e.tensor_tensor(u2, Es[:, :, 0:m], bre_b, ALU.mult))
                d5(lambda e, m=m, u1=u1: e.tensor_copy(Ec[:, :, m:2 * m], u1))
                d5(lambda e, m=m, bim_b=bim_b, u1=u1: e.tensor_tensor(u1, Ec[:, :, 0:m], bim_b, ALU.mult))
                d5(lambda e, m=m, u1=u1, u2=u2: e.tensor_tensor(Es[:, :, m:2 * m], u1, u2, ALU.add))
                m *= 2
            d5(lambda e: e.memset(Rtab[:, :, 1:64], 1.0))
            d5(lambda e: e.tensor_tensor(Rtab[:, :, 1:64], Rtab[:, :, 1:64], Rl[:, :].rearrange("p (g a) -> p g a", a=1).to_broadcast([128, 16, 63]), ALU.mult))
            S.emit()

        with contextlib.ExitStack() as st:
            def T(name, shape, dt=F32):
                return st.enter_context(nc.sbuf_tensor("m_" + name, shape, dt))

            S = Sched(nc, SEMS)
            dve = lambda fn, r=(), w=(): S.op("dve", fn, r, w)
            act = lambda fn, r=(), w=(): S.op("act", fn, r, w)
            pool = lambda fn, r=(), w=(): S.op("pool", fn, r, w)
            pe = lambda fn, r=(), w=(): S.op("pe", fn, r, w)
            x_sb = T("x_sb", [128, 4, DM])
            hT = T("hT", [128, 8, TT], BF16)
            xmT = T("xmT", [128, 8, TT + 4], BF16)
            xcar = T("xcar", [128, 8, 4], BF16)
            som = T("som", [128, 8, TT], BF16)
            usT = T("usT", [128, 4, TT], BF16)
            xcT = T("xcT", [128, 8, TT], BF16)
            R1 = T("R1", [128, 12288], BF16)
            vp = T("vp", [128, 4, 4, 256], BF16)
            C32 = T("C32", [128, 4, 2, 256])
            Cb = T("Cb", [128, 4, 2, 256], BF16)
            n32 = T("n32", [128, 4, 2])
            nb = T("nb", [128, 4, 2], BF16)
            wb16 = T("wb16", [128, 4, 4], BF16)
            glT = T("glT", [128, 4, TT], BF16)
            ring = T("ring", [128, RING, 2048], BF16)
            Sre = T("Sre", [128, 16, 65])
            Sim = T("Sim", [128, 16, 65])
            Sbre = T("Sbre", [128, 16, 64], BF16)
            Sbim = T("Sbim", [128, 16, 64], BF16)
            NF, NB = 6, 3
            fring = T("fring", [128, NF, 512])
            bring = T("bring", [128, NB, 512], BF16)
            rstate = {"f": 0, "b": 0}

            def f32buf():
                rstate["f"] = (rstate["f"] + 1) % NF
                return fring[:, rstate["f"], :], "fr%d" % rstate["f"]

            def bfbuf():
                rstate["b"] = (rstate["b"] + 1) % NB
                return bring[:, rstate["b"], :], "br%d" % rstate["b"]
            fcar = T("fcar", [128, 44, 2])
            bnd = T("bnd", [128, 44, 2])
            XM = ["xmT%d" % c for c in range(8)]
            VP = ["vp%d" % s_ for s_ in range(4)]
            rk = lambda b: "R1_%d" % b
            hn_tm = xmT[:, :, :].rearrange("p a b -> p (a b)")[:, 0:4096].rearrange("p (s f) -> p s f", s=4)
            qT = R1[:, 0:4096].rearrange("p (c t) -> p c t", c=8)
            kT = R1[:, 4096:8192].rearrange("p (c t) -> p c t", c=8)
            k_tm = R1[:, 8192:12288].rearrange("p (s f) -> p s f", s=4)
            sga, sgb = qT, kT
            gT = R1[:, 0:11264].rearrange("p (c t) -> p c t", c=22)
            hnb_t = R1[:, 0:4096].rearrange("p (s f) -> p s f", s=4)
            mT = vp[:, :, :, :].rearrange("p a b c -> p (a b c)").rearrange("p (c t) -> p c t", c=8)
            aoutT, boutT = xcT, usT
            epsc, onec = small[:, 250:251], small[:, 251:252]

            pool(lambda e: e.memset(C32[:], 0.0), [], ["C32_%d" % h for h in range(4)])
            pool(lambda e: e.memset(xcar[:], 0.0), [], ["xcar"])
            pool(lambda e: e.memset(fcar[:], 0.0), [], ["fcar"])
            pool(lambda e: e.memset(Sre[:], 0.0), [], ["Sre"])
            pool(lambda e: e.memset(Sim[:], 0.0), [], ["Sim"])
            pool(lambda e: e.memset(vp[:], 0.0), [], VP)
            pool(lambda e: e.memset(Cb[:], 0.0), [], ["Cb%d" % h for h in range(4)])
            pool(lambda e: e.memset(n32[:], 0.0), [], ["n32"])
            pool(lambda e: e.memset(nb[:], 0.0), [], ["nb"])

            seq = [sid for _ in range(nst) for sid in ORDER]
            wstate = {"issued": 0, "cur": -1}

            def wnext():
                wstate["cur"] += 1
                i = wstate["cur"]
                while wstate["issued"] < min(len(seq), i + RING):
                    n = wstate["issued"]
                    S.dma("sp", ring[:, n % RING, :], wsl_d[seq[n]], reads=[], writes=["ring%d" % (n % RING)])
                    wstate["issued"] += 1
                return ring[:, i % RING, :], "ring%d" % (i % RING)

            def sig(out, in_, rkeys, wkeys, bias=None, scale=1.0, eng2="pool"):
                if bias is None:
                    act(lambda e: e.activation(out, in_, AF.Tanh, scale=0.5 * scale), rkeys, wkeys)
                else:
                    act(lambda e: e.activation(out, in_, AF.Tanh, bias=bias, scale=0.5 * scale), list(rkeys) + ["chalf"], wkeys)
                S.op(eng2, lambda e: e.tensor_scalar(out, out, 0.5, 0.5, ALU.mult, ALU.add), wkeys, wkeys)

            psrr = {"i": 0}

            def nextps():
                psrr["i"] = (psrr["i"] + 1) % 4
                return psrr["i"], psf[psrr["i"]], "psf%d" % psrr["i"]

            def nextps6():
                psrr["i"] = (psrr["i"] + 1) % 6
                return psrr["i"], psf[psrr["i"]], "psf%d" % psrr["i"]

            def rms_rstd():
                ssq, rs = small[:, 0:4], small[:, 4:8]
                pool(lambda e: e.memset(ssq, 0.0), [], ["ssq"])
                for sub in range(4):
                    act(lambda e, sub=sub: e.activation(hnb_t[:, sub, :], x_sb[:, sub, :], AF.Square, accum_out=ssq[:, sub:sub + 1]),
                        ["x_sb%d" % sub, "ssq"], [rk(2 * sub), rk(2 * sub + 1), "ssq"])
                act(lambda e: e.activation(rs, ssq, AF.Ln, bias=epsc, scale=1.0 / DM), ["ssq"], ["rs"])
                act(lambda e: e.activation(rs, rs, AF.Exp, scale=-0.5), ["rs"], ["rs"])
                return rs

            def norm_T(gcol_off):
                rs = rms_rstd()
                for sub in range(4):
                    dve(lambda e, sub=sub: e.tensor_scalar_mul(hnb_t[:, sub, :], x_sb[:, sub, :], rs[:, sub:sub + 1]),
                        ["x_sb%d" % sub, "rs"], [rk(2 * sub), rk(2 * sub + 1)])
                for fc in range(8):
                    pb = psb[fc % 2]
                    for sub in range(4):
                        pe(lambda e, fc=fc, sub=sub, pb=pb: e.transpose(pb[:, sub * 128:(sub + 1) * 128], hnb_t[:, sub, fc * 128:(fc + 1) * 128], ident[:]),
                           [rk(2 * sub), rk(2 * sub + 1)], ["psb%d" % (fc % 2)])
                    dve(lambda e, fc=fc, pb=pb: e.tensor_scalar_mul(hT[:, fc, :], pb[:, 0:512], cc(gcol_off, fc)),
                        ["psb%d" % (fc % 2)], ["hT%d" % fc])

            def dump_T(src, nchunk, keys, stg):
                for c in range(nchunk):
                    fb, fk = f32buf()
                    dve(lambda e, c=c, fb=fb: e.tensor_copy(fb, src[:, c, :]), list(keys), [fk])
                    S.dma("pool", y_d[c * 128:(c + 1) * 128, stg * 512:(stg + 1) * 512], fb, reads=[fk], writes=["y"])

            def inproj_chunk(mc, slot, skey):
                pi, ps, pk = nextps()
                base = ((mc % 2) * 8) * 128
                for kt in range(8):
                    pe(lambda e, kt=kt, ps=ps: e.matmul(ps[:, :], lhsT=slot[:, base + kt * 128: base + (kt + 1) * 128], rhs=hT[:, kt, :],
                                                          start=(kt == 0), stop=(kt == 7)), [skey, "hT%d" % kt], [pk])
                return ps, pk

            for stg in range(nst):
                t0 = stg * TT
                for sub in range(4):
                    S.dma("pool", x_sb[:, sub, :], x_d[t0 + sub * 128:t0 + (sub + 1) * 128, :], writes=["x_sb%d" % sub])
                norm_T(C_G1)
                pool(lambda e: e.tensor_copy(xmT[:, :, 1:4], xcar[:, :, 1:4]), ["xcar"], XM)
                def conv_chunk(c):
                    z, zk = f32buf()
                    sg, sk = bfbuf()
                    dve(lambda e: e.tensor_scalar(z, xmT[:, c, 1:TT + 1], cc(C_MCW, c * 4 + 0), cc(C_MCB, c), ALU.mult, ALU.add),
                        ["xmT%d" % c], [zk])
                    for k in range(1, 4):
                        dve(lambda e, k=k: e.scalar_tensor_tensor(z, xmT[:, c, 1 + k:TT + 1 + k], cc(C_MCW, c * 4 + k), z, ALU.mult, ALU.add),
                            ["xmT%d" % c, zk], [zk])
                    sig(sg, z, [zk], [sk])
                    dve(lambda e: e.tensor_tensor(xcT[:, c, :], z, sg, ALU.mult), [zk, sk], ["xcT%d" % c])

                for mc in range(20):
                    if mc % 2 == 0:
                        slot, skey = wnext()
                    ps, pk = inproj_chunk(mc, slot, skey)
                    if mc < 8:
                        act(lambda e, mc=mc, ps=ps: e.activation(xmT[:, mc, 4:TT + 4], ps[:, :], AF.Identity, bias=cc(C_BIN, mc)),
                            [pk], ["xmT%d" % mc])
                        conv_chunk(mc)
                    elif mc < 16:
                        sig(som[:, mc - 8, :], ps[:, :], [pk], ["som%d" % (mc - 8)], bias=ch(C_BIN, mc))
                    else:
                        act(lambda e, mc=mc, ps=ps: e.activation(usT[:, mc - 16, :], ps[:, :], AF.Identity, bias=cc(C_BIN, mc)),
                            [pk], ["usT%d" % (mc - 16)])
                if debug == "h":
                    dump_T(hT, 8, ["hT%d" % c for c in range(8)], stg)
                if debug == "xm":
                    dump_T(xmT[:, :, 4:TT + 4], 8, XM, stg)
                gsb = small[:, 16:48].rearrange("p (s g) -> p s g", s=4)
                for sub in range(4):
                    for kt in range(8):
                        pe(lambda e, sub=sub, kt=kt: e.matmul(psf[4][:, sub * 8:(sub + 1) * 8], lhsT=hT[:, kt, sub * 128:(sub + 1) * 128],
                                                              rhs=wgb[:, kt * 8:(kt + 1) * 8], start=(kt == 0), stop=(kt == 7)),
                           ["hT%d" % kt], ["psf4"])
                for sub in range(4):
                    dve(lambda e, sub=sub: e.tensor_tensor(gsb[:, sub, :], psf[4][:, sub * 8:(sub + 1) * 8], crow[:, 1024:1032], ALU.add),
                        ["psf4"], ["gsb"])
                pool(lambda e: e.tensor_copy(xcar[:, :, 1:4], xmT[:, :, TT + 1:TT + 4]), XM, ["xcar"])
                if debug == "xc":
                    dump_T(xcT, 8, ["xcT%d" % c for c in range(8)], stg)
                lfn = small[:, 48:64].rearrange("p (s h) -> p s h", s=4)
                act(lambda e: e.activation(lfn, gsb[:, :, 4:8], AF.Exp, scale=-1.0), ["gsb"], ["lfn"])
                act(lambda e: e.activation(lfn, lfn, AF.Ln, bias=onec, scale=1.0), ["lfn"], ["lfn"])
                for sub in range(4):
                    pe(lambda e, sub=sub: e.matmul(psf[4][:, 64 + sub * 8:64 + sub * 8 + 4], lhsT=LT[:], rhs=lfn[:, sub, :], start=True, stop=True),
                       ["lfn"], ["psf4"])
                    pe(lambda e, sub=sub: e.matmul(psf[4][:, 64 + sub * 8 + 4:64 + sub * 8 + 8], lhsT=ONES[:], rhs=lfn[:, sub, :], start=True, stop=True),
                       ["lfn"], ["psf4"])
                Pm = psf[4][:, 64:96].rearrange("p (s g) -> p s g", s=4)
                wcol = small[:, 64:80].rearrange("p (s h) -> p s h", s=4)
                e2c = small[:, 80:96].rearrange("p (s h) -> p s h", s=4)
                dcol = small[:, 96:112].rearrange("p (s h) -> p s h", s=4)
                dve(lambda e: e.tensor_tensor(wcol, gsb[:, :, 0:4], Pm[:, :, 0:4], ALU.subtract), ["gsb", "psf4"], ["wcol"])
                act(lambda e: e.activation(wcol, wcol, AF.Exp), ["wcol"], ["wcol"])
                act(lambda e: e.copy(wb16[:], wcol), ["wcol"], ["wb16"])
                act(lambda e: e.activation(e2c, Pm[:, :, 0:4], AF.Exp, scale=-1.0), ["psf4"], ["e2c"])
                act(lambda e: e.activation(dcol, Pm[:, :, 4:8], AF.Exp, scale=-1.0), ["psf4"], ["dcol"])
                for hh in range(2):
                    slot, skey = wnext()
                    for hl in range(2):
                        h = hh * 2 + hl
                        for qk in range(2):
                            for ec in range(2):
                                pi, ps, pk = nextps()
                                for kt in range(2):
                                    col = ((((hl * 2 + qk) * 2 + ec) * 2 + kt)) * 128
                                    pe(lambda e, ps=ps, col=col, h=h, kt=kt, slot=slot: e.matmul(ps[:, :], lhsT=slot[:, col:col + 128], rhs=xcT[:, h * 2 + kt, :],
                                                                                                start=(kt == 0), stop=(kt == 1)),
                                       [skey, "xcT%d" % (h * 2 + kt)], [pk])
                                dst = (qT, kT)[qk]
                                act(lambda e, ps=ps, dst=dst, h=h, ec=ec, qk=qk: e.activation(dst[:, h * 2 + ec, :], ps[:, :], AF.Identity, scale=(1.0, 1.0 / 16.0)[qk]),
                                    [pk], [rk(qk * 8 + h * 2 + ec)])
                slot, skey = wnext()
                for sub in range(4):
                    for hp in range(2):
                        pi, ps, pk = nextps()
                        for hl in range(2):
                            h = hp * 2 + hl
                            for kt in range(2):
                                pe(lambda e, ps=ps, h=h, hl=hl, kt=kt, sub=sub, slot=slot: e.matmul(ps[:, hl * 256:(hl + 1) * 256], lhsT=xcT[:, h * 2 + kt, sub * 128:(sub + 1) * 128],
                                                                                                   rhs=slot[:, (h * 2 + kt) * 256:(h * 2 + kt + 1) * 256], start=(kt == 0), stop=(kt == 1)),
                                   [skey, "xcT%d" % (h * 2 + kt)], [pk])
                        act(lambda e, ps=ps, sub=sub, hp=hp: e.activation(k_tm[:, sub, hp * 512:(hp + 1) * 512], ps[:, :], AF.Identity, scale=1.0 / 16.0),
                            [pk], [rk(16 + sub * 2 + hp)])
                slot, skey = wnext()
                for sub in range(4):
                    for hp in range(2):
                        pi, ps, pk = nextps()
                        for hl in range(2):
                            h = hp * 2 + hl
                            for kt in range(2):
                                pe(lambda e, ps=ps, h=h, hl=hl, kt=kt, sub=sub, slot=slot: e.matmul(ps[:, hl * 256:(hl + 1) * 256], lhsT=xmT[:, h * 2 + kt, 4 + sub * 128:4 + (sub + 1) * 128],
                                                                                                   rhs=slot[:, (h * 2 + kt) * 256:(h * 2 + kt + 1) * 256], start=(kt == 0), stop=(kt == 1)),
                                   [skey, "xmT%d" % (h * 2 + kt)], [pk])
                        for hl in range(2):
                            h = hp * 2 + hl
                            dve(lambda e, ps=ps, sub=sub, h=h, hl=hl: e.tensor_scalar_mul(vp[:, sub, h, 0:256], ps[:, hl * 256:(hl + 1) * 256], wcol[:, sub, h:h + 1]),
                                [pk, "wcol"], VP)
                st6 = small[:, 144:168].rearrange("p (h s) -> p h s", h=4)
                mv = small[:, 168:176].rearrange("p (h s) -> p h s", h=4)
                dd, mx, rr = small[:, 176:180], small[:, 180:184], small[:, 184:188]
                for sub in range(4):
                    tok = slice(sub * 128, (sub + 1) * 128)
                    CBK = ["Cb%d" % h for h in range(4)]
                    for h in range(4):
                        act(lambda e, h=h, sub=sub: e.activation(Cb[:, h, :, :], C32[:, h, :, :], AF.Identity, scale=dcol[:, sub, h:h + 1]),
                            ["C32_%d" % h, "dcol"], ["Cb%d" % h])
                    dve(lambda e, sub=sub: e.tensor_tensor(nb[:], n32[:], dcol[:, sub, :].rearrange("p (h a) -> p h a", a=1).to_broadcast([128, 4, 2]), ALU.mult),
                        ["n32", "dcol"], ["nb"])
                    for h in range(4):
                        for ec in range(2):
                            pe(lambda e, h=h, ec=ec, tok=tok: e.matmul(psf[5][:, h * 128:(h + 1) * 128], lhsT=kT[:, h * 2 + ec, tok], rhs=qT[:, h * 2 + ec, tok],
                                                                       start=(ec == 0), stop=(ec == 1)),
                               [rk(8 + h * 2 + ec), rk(h * 2 + ec)], ["psf5"])
                    sTb, sTk = bfbuf()
                    dve(lambda e, sTb=sTb: e.tensor_tensor(sTb, psf[5][:, :], cmask4[:], ALU.mult), ["psf5"], [sTk])
                    for h in range(4):
                        pn, pnk = psf[h // 2][:, (h % 2) * 256:(h % 2 + 1) * 256], "psf%d" % (h // 2)
                        pe(lambda e, pn=pn, sub=sub, h=h, sTb=sTb: e.matmul(pn, lhsT=sTb[:, h * 128:(h + 1) * 128], rhs=vp[:, sub, h, :], start=True, stop=False),
                           [sTk] + VP, [pnk])
                        for ec in range(2):
                            pe(lambda e, pn=pn, h=h, ec=ec, tok=tok: e.matmul(pn, lhsT=qT[:, h * 2 + ec, tok], rhs=Cb[:, h, ec, :], start=False, stop=(ec == 1)),
                               [rk(h * 2 + ec), "Cb%d" % h], [pnk])
                        pd = psf[4][:, 128 + h:129 + h]
                        pe(lambda e, pd=pd, sub=sub, h=h, sTb=sTb: e.matmul(pd, lhsT=sTb[:, h * 128:(h + 1) * 128], rhs=wb16[:, sub, h:h + 1], start=True, stop=False),
                           [sTk, "wb16"], ["psf4"])
                        for ec in range(2):
                            pe(lambda e, pd=pd, h=h, ec=ec, tok=tok: e.matmul(pd, lhsT=qT[:, h * 2 + ec, tok], rhs=nb[:, h, ec:ec + 1], start=False, stop=(ec == 1)),
                               [rk(h * 2 + ec), "nb"], ["psf4"])
                    for h in range(4):
                        pu, puk = psf[2 + h % 2], "psf%d" % (2 + h % 2)
                        for dk in range(2):
                            pe(lambda e, pu=pu, sub=sub, h=h, dk=dk: e.matmul(pu[:, dk * 256:(dk + 1) * 256], lhsT=k_tm[:, sub, h * 256 + dk * 128:h * 256 + (dk + 1) * 128],
                                                                              rhs=vp[:, sub, h, :], start=True, stop=True),
                               [rk(16 + sub * 2 + h // 2)] + VP, [puk])
                            pe(lambda e, sub=sub, h=h, dk=dk: e.matmul(psf[4][:, 136 + h * 2 + dk:137 + h * 2 + dk], lhsT=k_tm[:, sub, h * 256 + dk * 128:h * 256 + (dk + 1) * 128],
                                                                       rhs=wb16[:, sub, h:h + 1], start=True, stop=True),
                               [rk(16 + sub * 2 + h // 2), "wb16"], ["psf4"])
                        dve(lambda e, pu=pu, h=h, sub=sub: e.scalar_tensor_tensor(C32[:, h, :, :].rearrange("p a b -> p (a b)"), C32[:, h, :, :].rearrange("p a b -> p (a b)"),
                                                                                  dcol[:, sub, h:h + 1], pu[:, :], ALU.mult, ALU.add),
                            [puk, "dcol", "C32_%d" % h], ["C32_%d" % h])
                    dve(lambda e, sub=sub: e.tensor_tensor(n32[:], n32[:], dcol[:, sub, :].rearrange("p (h a) -> p h a", a=1).to_broadcast([128, 4, 2]), ALU.mult),
                        ["n32", "dcol", "nb"], ["n32"])
                    dve(lambda e: e.tensor_tensor(n32[:], n32[:], psf[4][:, 136:144].rearrange("p (h a) -> p h a", a=2), ALU.add), ["n32", "psf4"], ["n32"])
                    for h in range(4):
                        pn, pnk = psf[h // 2][:, (h % 2) * 256:(h % 2 + 1) * 256], "psf%d" % (h // 2)
                        dve(lambda e, pn=pn, h=h: e.bn_stats(st6[:, h, :], pn), [pnk], ["st6"])
                    for h in range(4):
                        dve(lambda e, h=h: e.bn_aggr(mv[:, h, :], st6[:, h, :]), ["st6"], ["mv"])
                    dve(lambda e: e.tensor_copy(dd, psf[4][:, 128:132]), ["psf4"], ["dd"])
                    dve(lambda e: e.scalar_tensor_tensor(mx, dd, -1.0, dd, ALU.mult, ALU.max), ["dd"], ["mx"])
                    dve(lambda e, sub=sub: e.tensor_tensor(mx, mx, e2c[:, sub, :], ALU.max), ["mx", "e2c"], ["mx"])
                    dve(lambda e: e.tensor_tensor(mx, mx, mx, ALU.mult), ["mx"], ["mx"])
                    dve(lambda e: e.scalar_tensor_tensor(rr, mx, EPS, mv[:, :, 1], ALU.mult, ALU.add), ["mx", "mv"], ["rr"])
                    act(lambda e: e.activation(rr, rr, AF.Ln), ["rr"], ["rr"])
                    act(lambda e: e.activation(rr, rr, AF.Exp, scale=-0.5), ["rr"], ["rr"])
                    for h in range(4):
                        pn, pnk = psf[h // 2][:, (h % 2) * 256:(h % 2 + 1) * 256], "psf%d" % (h // 2)
                        dve(lambda e, pn=pn, sub=sub, h=h: e.tensor_scalar(hn_tm[:, sub, h * 256:(h + 1) * 256], pn, mv[:, h, 0:1], rr[:, h:h + 1], ALU.subtract, ALU.mult),
                            [pnk, "mv", "rr"], XM)
                for fc in range(8):
                    pb = psb[fc % 2]
                    for sub in range(4):
                        pe(lambda e, fc=fc, sub=sub, pb=pb: e.transpose(pb[:, sub * 128:(sub + 1) * 128], hn_tm[:, sub, fc * 128:(fc + 1) * 128], ident[:]),
                           XM, ["psb%d" % (fc % 2)])
                    hb, hk = bfbuf()
                    act(lambda e, fc=fc, pb=pb, hb=hb: e.activation(hb, pb[:, 0:512], AF.Identity, scale=cc(C_HG, fc)), ["psb%d" % (fc % 2)], [hk])
                    dve(lambda e, fc=fc, hb=hb: e.tensor_tensor(hb, hb, som[:, fc, :], ALU.mult), [hk, "som%d" % fc], [hk])
                    dve(lambda e, fc=fc, hb=hb: e.scalar_tensor_tensor(aoutT[:, fc, :], xcT[:, fc, :], cc(C_SK, fc), hb, ALU.mult, ALU.add),
                        [hk, "xcT%d" % fc], ["xcT%d" % fc])
                if debug == "aout":
                    dump_T(aoutT, 8, ["xcT%d" % c for c in range(8)], stg)
                US = ["usT%d" % c for c in range(4)]
                def s5_half(half, stg=stg):
                    for gl_ in range(8):
                        gp = half * 8 + gl_
                        chunk, win = gp // 4, gp % 4
                        cl = gl_ // 4
                        for ri in range(2):
                            for j in range(8):
                                pe(lambda e, ri=ri, j=j, cl=cl, chunk=chunk, win=win: e.matmul(
                                    psf[win][:, (cl * 2 + ri) * 64:(cl * 2 + ri + 1) * 64], lhsT=WZt[32 * win:32 * win + 32, chunk, ri, j, :],
                                    rhs=usT[32 * win:32 * win + 32, chunk, :].rearrange("p (b j) -> p b j", j=8)[:, :, j],
                                    start=(j == 0), stop=(j == 7), tile_position=(32 * win, 0)), US, ["psf%d" % win])
                    gs = slice(half * 8, half * 8 + 8)
                    EcH = Ec[:, gs, :].rearrange("p g k -> p (g k)")
                    EsH = Es[:, gs, :].rearrange("p g k -> p (g k)")
                    RtH = Rtab[:, gs, :].rearrange("p g k -> p (g k)")
                    (bre, kbre), (bim, kbim), (ore, kore), (oim, koim), (tt, ktt) = f32buf(), f32buf(), f32buf(), f32buf(), f32buf()
                    w4 = lambda a, w: a.rearrange("p (c w k) -> p c w k", c=2, w=4)[:, :, w, :]
                    for w in range(4):
                        Zw = psf[w][:, 0:256].rearrange("p (c r k) -> p c r k", c=2, r=2)
                        Zre_w, Zim_w = Zw[:, :, 0, :], Zw[:, :, 1, :]
                        pk_ = "psf%d" % w
                        dve(lambda e, w=w, Zre_w=Zre_w: e.tensor_tensor(w4(bre, w), Zre_w, w4(EcH, w), ALU.mult), [pk_], [kbre])
                        dve(lambda e, w=w, Zim_w=Zim_w: e.tensor_tensor(w4(tt, w), Zim_w, w4(EsH, w), ALU.mult), [pk_], [ktt])
                        dve(lambda e, w=w, Zim_w=Zim_w: e.tensor_tensor(w4(bim, w), Zim_w, w4(EcH, w), ALU.mult), [pk_], [kbim])
                        dve(lambda e, w=w, Zre_w=Zre_w: e.tensor_tensor(w4(ore, w), Zre_w, w4(EsH, w), ALU.mult), [pk_], [kore])
                    dve(lambda e: e.tensor_tensor(bre, bre, tt, ALU.add), [kbre, ktt], [kbre])
                    dve(lambda e: e.tensor_tensor(bim, bim, ore, ALU.subtract), [kbim, kore], [kbim])
                    c0 = small[:, 128:136]
                    for (bb, SS, sk, bk) in ((bre, Sre, "Sre", kbre), (bim, Sim, "Sim", kbim)):
                        dve(lambda e, SS=SS: e.tensor_tensor(c0.rearrange("p (g a) -> p g a", a=1), SS[:, gs, 0:1], Rl[:, gs].rearrange("p (g a) -> p g a", a=1), ALU.mult),
                            [sk], ["c0"])
                        dve(lambda e, bb=bb: e.tensor_tensor(bb.rearrange("p (g k) -> p g k", g=8)[:, :, 0:1], bb.rearrange("p (g k) -> p g k", g=8)[:, :, 0:1],
                                                            c0.rearrange("p (g a) -> p g a", a=1), ALU.add), ["c0", bk], [bk])
                    dve(lambda e: e.tensor_tensor_scan(ore, RtH, bre, 0.0, ALU.mult, ALU.add), [kbre, kbim, kore], [kore])
                    dve(lambda e: e.tensor_tensor_scan(oim, RtH, bim, 0.0, ALU.mult, ALU.add), [kbim], [koim])
                    v3 = lambda a: a.rearrange("p (g k) -> p g k", g=8)
                    dve(lambda e: e.tensor_tensor(tt, oim, EsH, ALU.mult), [koim], [ktt])
                    dve(lambda e: e.tensor_tensor(bre, ore, EcH, ALU.mult), [kore], [kbre])
                    dve(lambda e: e.tensor_tensor(Sre[:, gs, 1:65], v3(bre), v3(tt), ALU.subtract), [kbre, ktt, "Sbre"], ["Sre"])
                    dve(lambda e: e.tensor_tensor(tt, ore, EsH, ALU.mult), [kore], [ktt])
                    dve(lambda e: e.tensor_tensor(bim, oim, EcH, ALU.mult), [koim], [kbim])
                    dve(lambda e: e.tensor_tensor(Sim[:, gs, 1:65], v3(bim), v3(tt), ALU.add), [kbim, ktt, "Sbim"], ["Sim"])
                    pool(lambda e: e.tensor_copy(Sbre[:, gs, :], Sre[:, gs, 0:64]), ["Sre"], ["Sbre"])
                    pool(lambda e: e.tensor_copy(Sbim[:, gs, :], Sim[:, gs, 0:64]), ["Sim"], ["Sbim"])
                    pool(lambda e: e.tensor_copy(Sre[:, gs, 0:1], Sre[:, gs, 64:65]), ["Sre", "Sbre"], ["Sre"])
                    pool(lambda e: e.tensor_copy(Sim[:, gs, 0:1], Sim[:, gs, 64:65]), ["Sim", "Sbim"], ["Sim"])
                    for cl in range(2):
                        chunk = half * 2 + cl
                        Y, yk = psf[4 + cl], "psf%d" % (4 + cl)
                        Y3 = Y[:, :].rearrange("p (b j) -> p b j", j=8)
                        U3 = usT[:, chunk, :].rearrange("p (b j) -> p b j", j=8)
                        for tau in range(8):
                            pe(lambda e, tau=tau, chunk=chunk, Y3=Y3, U3=U3: e.matmul(Y3[:, :, tau:8], lhsT=Kt[:, chunk, tau, :], rhs=U3[:, :, 0:8 - tau],
                                                                                      start=(tau == 0), stop=False), US, [yk])
                        for win in range(4):
                            gp = chunk * 4 + win
                            for j in range(8):
                                for ri in range(2):
                                    last = (win == 3 and j == 7 and ri == 1)
                                    SB = (Sbre, Sbim)[ri]
                                    pe(lambda e, win=win, gp=gp, j=j, ri=ri, SB=SB, Y3=Y3, last=last: e.matmul(
                                        Y3[32 * win:32 * win + 32, :, j], lhsT=WI[:, gp, ri, j, :], rhs=SB[:, gp, :],
                                        start=False, stop=last, tile_position=(0, 32 * win)), ["Sbre", "Sbim"], [yk])
                        (ysb, yk2), (z2, zk2) = f32buf(), f32buf()
                        sgg, sgk = bfbuf()
                        act(lambda e, Y=Y, ysb=ysb: e.activation(ysb, Y[:, :], AF.Identity), [yk], [yk2])
                        act(lambda e, Y=Y, z2=z2: e.activation(z2, Y[:, :], AF.Square), [yk], [zk2])
                        pool(lambda e, z2=z2: e.tensor_scalar(z2, z2, 0.044715, 1.0, ALU.mult, ALU.add), [zk2], [zk2])
                        pool(lambda e, z2=z2, ysb=ysb: e.tensor_tensor(z2, z2, ysb, ALU.mult), [zk2, yk2], [zk2])
                        sig(sgg, z2, [zk2], [sgk], scale=2.0 * C1G)
                        dve(lambda e, chunk=chunk, ysb=ysb, sgg=sgg: e.tensor_tensor(glT[:, chunk, :], ysb, sgg, ALU.mult), [yk2, sgk], ["glT%d" % chunk])
                for half in range(2):
                    s5_half(half)
                if debug == "gl":
                    dump_T(glT, 4, ["glT%d" % c for c in range(4)], stg)
                slot, skey = wnext()
                for jc in range(4):
                    pi, ps, pk = nextps()
                    for kt in range(4):
                        pe(lambda e, ps=ps, jc=jc, kt=kt, slot=slot: e.matmul(ps[:, :], lhsT=slot[:, (jc * 4 + kt) * 128:(jc * 4 + kt + 1) * 128], rhs=glT[:, kt, :],
                                                                             start=(kt == 0), stop=(kt == 3)), [skey, "glT%d" % kt], [pk])
                    sgg, sgk = bfbuf()
                    sig(sgg, ps[:, :], [pk], [sgk], bias=ch(C_BGLU, jc))
                    dve(lambda e, jc=jc, sgg=sgg: e.tensor_tensor(boutT[:, jc, :], glT[:, jc, :], sgg, ALU.mult), [sgk, "glT%d" % jc], ["usT%d" % jc])
                if debug == "bout":
                    dump_T(boutT, 4, US, stg)
                for mc in range(20, 36):
                    if mc % 2 == 0:
                        slot, skey = wnext()
                    ps, pk = inproj_chunk(mc, slot, skey)
                    sig(qT[:, mc - 20, :] if mc < 28 else kT[:, mc - 28, :], ps[:, :], [pk], [rk(mc - 20)], bias=ch(C_BIN, mc))
                for mc in range(8):
                    if mc % 2 == 0:
                        slot, skey = wnext()
                    pi, ps, pk = nextps()
                    for kt in range(8):
                        pe(lambda e, ps=ps, mc=mc, kt=kt, slot=slot: e.matmul(ps[:, :], lhsT=slot[:, ((mc % 2) * 8 + kt) * 128:((mc % 2) * 8 + kt + 1) * 128], rhs=aoutT[:, kt, :],
                                                                             start=(kt == 0), stop=(kt == 7)), [skey, "xcT%d" % kt], [pk])
                    dve(lambda e, ps=ps, mc=mc: e.tensor_tensor(mT[:, mc, :], ps[:, :], sga[:, mc, :], ALU.mult), [pk, rk(mc)], VP)
                for mc in range(8):
                    if mc % 4 == 0:
                        slot, skey = wnext()
                    pi, ps, pk = nextps()
                    for kt in range(4):
                        pe(lambda e, ps=ps, mc=mc, kt=kt, slot=slot: e.matmul(ps[:, :], lhsT=slot[:, ((mc % 4) * 4 + kt) * 128:((mc % 4) * 4 + kt + 1) * 128], rhs=boutT[:, kt, :],
                                                                             start=(kt == 0), stop=(kt == 3)), [skey, "usT%d" % kt], [pk])
                    tb_, tk_ = bfbuf()
                    dve(lambda e, ps=ps, mc=mc, tb_=tb_: e.tensor_tensor(tb_, ps[:, :], sgb[:, mc, :], ALU.mult), [pk, rk(8 + mc)], [tk_])
                    pool(lambda e, mc=mc, tb_=tb_: e.tensor_tensor(mT[:, mc, :], mT[:, mc, :], tb_, ALU.add), [tk_] + VP, VP)
                if debug == "merged":
                    dump_T(mT, 8, VP, stg)
                for nh in range(2):
                    pss = [nextps() for _ in range(4)]
                    for kq in range(2):
                        slot, skey = wnext()
                        for k4 in range(4):
                            kt = kq * 4 + k4
                            for sub in range(4):
                                pi, ps, pk = pss[sub]
                                pe(lambda e, ps=ps, kt=kt, k4=k4, sub=sub, slot=slot: e.matmul(ps[:, :], lhsT=mT[:, kt, sub * 128:(sub + 1) * 128], rhs=slot[:, k4 * 512:(k4 + 1) * 512],
                                                                                              start=(kt == 0), stop=(kt == 7)), [skey] + VP, [pk])
                    for sub in range(4):
                        pi, ps, pk = pss[sub]
                        dve(lambda e, ps=ps, sub=sub, nh=nh: e.tensor_tensor(x_sb[:, sub, nh * 512:(nh + 1) * 512], x_sb[:, sub, nh * 512:(nh + 1) * 512], ps[:, :], ALU.add),
                            [pk, "x_sb%d" % sub], ["x_sb%d" % sub])
                if debug == "x1":
                    for sub in range(4):
                        S.dma("pool", y_d[t0 + sub * 128:t0 + (sub + 1) * 128, :], x_sb[:, sub, :], reads=["x_sb%d" % sub], writes=["y"])
                norm_T(C_G2)
                fcw3 = ccol[:, C_FCW:C_FCW + 132].rearrange("p (m k) -> p m k", k=3)
                bt = small[:, 192:236]
                dve(lambda e: e.tensor_tensor(bt, fcar[:, :, 1], fcw3[:, :, 1], ALU.mult), ["fcar"], ["bt"])
                dve(lambda e: e.tensor_tensor(bnd[:, :, 1], fcar[:, :, 1], fcw3[:, :, 0], ALU.mult), ["fcar"], ["bnd"])
                dve(lambda e: e.tensor_tensor(bnd[:, :, 0], fcar[:, :, 0], fcw3[:, :, 0], ALU.mult), ["fcar", "bnd"], ["bnd"])
                dve(lambda e: e.tensor_tensor(bnd[:, :, 0], bnd[:, :, 0], bt, ALU.add), ["bt", "bnd"], ["bnd"])
                for i in range(22):
                    slot, skey = wnext()
                    accs = (f32buf(), f32buf())
                    pss2 = []
                    for vg in range(2):
                        pi, ps, pk = nextps6()
                        pss2.append((ps, pk))
                        for kt in range(8):
                            pe(lambda e, ps=ps, vg=vg, kt=kt, slot=slot: e.matmul(ps[:, :], lhsT=slot[:, (vg * 8 + kt) * 128:(vg * 8 + kt + 1) * 128], rhs=hT[:, kt, :],
                                                                                 start=(kt == 0), stop=(kt == 7)), [skey, "hT%d" % kt], [pk])
                    for vg in range(2):
                        mc = i + 22 * vg
                        (acc, akey), (ps, pk) = accs[vg], pss2[vg]
                        act(lambda e, ps=ps, mc=mc, acc=acc: e.activation(acc, ps[:, :], AF.Identity, bias=cc(C_FCB, mc), scale=cc(C_FCW, mc * 3 + 2)),
                            [pk], [akey])
                    for tap, sh in ((1, 1), (0, 2)):
                        for vg in range(2):
                            mc = i + 22 * vg
                            (acc, akey), (ps, pk) = accs[vg], pss2[vg]
                            dve(lambda e, ps=ps, mc=mc, acc=acc, tap=tap, sh=sh: e.scalar_tensor_tensor(acc[:, sh:512], ps[:, 0:512 - sh], cc(C_FCW, mc * 3 + tap),
                                                                                                    acc[:, sh:512], ALU.mult, ALU.add),
                                [pk, akey], [akey])
                    for vg in range(2):
                        mc = i + 22 * vg
                        (acc, akey), (ps, pk) = accs[vg], pss2[vg]
                        dve(lambda e, mc=mc, acc=acc: e.tensor_tensor(acc[:, 0:2], acc[:, 0:2], bnd[:, mc, :], ALU.add), ["bnd", akey], [akey])
                        act(lambda e, ps=ps, mc=mc: e.copy(fcar[:, mc, 0:2], ps[:, 510:512]), [pk, akey, "bnd"], ["fcar"])
                    (av, avk), (ag, agk) = accs
                    sgg, sgk = bfbuf()
                    sig(sgg, ag, [agk], [sgk])
                    pool(lambda e, ag=ag, sgg=sgg: e.tensor_tensor(ag, ag, sgg, ALU.mult), [agk, sgk], [agk])
                    pool(lambda e, i=i, ag=ag, av=av: e.tensor_tensor(gT[:, i, :], ag, av, ALU.mult), [avk, agk], [rk(i)])
                for nh in range(2):
                    pss = [nextps() for _ in range(4)]
                    for kq in range(6):
                        slot, skey = wnext()
                        for k4 in range(4):
                            kt = kq * 4 + k4
                            if kt >= 22:
                                continue
                            for sub in range(4):
                                pi, ps, pk = pss[sub]
                                pe(lambda e, ps=ps, kt=kt, k4=k4, sub=sub, slot=slot: e.matmul(ps[:, :], lhsT=gT[:, kt, sub * 128:(sub + 1) * 128], rhs=slot[:, k4 * 512:(k4 + 1) * 512],
                                                                                              start=(kt == 0), stop=(kt == 21)), [skey, rk(kt)], [pk])
                    for sub in range(4):
                        pi, ps, pk = pss[sub]
                        dve(lambda e, ps=ps, sub=sub, nh=nh: e.tensor_tensor(x_sb[:, sub, nh * 512:(nh + 1) * 512], x_sb[:, sub, nh * 512:(nh + 1) * 512], ps[:, :], ALU.add),
                            [pk, "x_sb%d" % sub], ["x_sb%d" % sub])
                if debug is None:
                    rs = rms_rstd()
                    for sub in range(4):
                        for nh in range(2):
                            ob, okk = f32buf()
                            dve(lambda e, sub=sub, ob=ob, nh=nh: e.scalar_tensor_tensor(ob, x_sb[:, sub, nh * 512:(nh + 1) * 512], rs[:, sub:sub + 1],
                                                                                       crow[:, nh * 512:(nh + 1) * 512], ALU.mult, ALU.mult),
                                ["x_sb%d" % sub, "rs"], [okk])
                            S.dma("pool", y_d[t0 + sub * 128:t0 + (sub + 1) * 128, nh * 512:(nh + 1) * 512], ob, reads=[okk], writes=["y"])
            S.emit()
    return nc


_CACHE = {}


def kernel(**inputs):
    prep = _prep(inputs)
    x = np.ascontiguousarray(np.asarray(inputs["x"], dtype=np.float32))
    if "nc" not in _CACHE:
        _CACHE["nc"] = build_nc()
    nc = _CACHE["nc"]
    in_maps = []
    for c in range(8):
        m = {"x": x[c]}
        m.update(prep)
        in_maps.append(m)
    res = run_bass_kernel_spmd(nc, in_maps, core_ids=list(range(8)))
    return np.stack([np.asarray(r["y"], dtype=np.float32) for r in res.results], axis=0)
```

```python
import contextlib
import math
import numpy as np
import concourse.bass as bass
import concourse.mybir as mybir
from concourse.bass_utils import run_bass_kernel_spmd

F32 = mybir.dt.float32
BF16 = mybir.dt.bfloat16
ALU = mybir.AluOpType
AF = mybir.ActivationFunctionType

SEQ = 4096
DM = 1024
TT = 512
NST = SEQ // TT
NS = 67
RING = 4
EPS = 1e-6


class _Op:
    __slots__ = ("eng", "fn", "is_dma", "deps", "inc", "sem", "semval")

    def __init__(self, eng, fn, is_dma):
        self.eng = eng
        self.fn = fn
        self.is_dma = is_dma
        self.deps = []
        self.inc = False
        self.sem = None
        self.semval = 0


class Sched:
    ENGS = ("pe", "act", "dve", "pool", "sp")
    N_DMA_SEMS = 24

    _uid = [0]

    @staticmethod
    def make_sems(nc, st):
        csem = {e: st.enter_context(nc.semaphore("cs_" + e)) for e in ("pe", "act", "dve", "pool")}
        dsem = {e: [st.enter_context(nc.semaphore("ds_%s%d" % (e, i))) for i in range(Sched.N_DMA_SEMS)]
                for e in ("sp", "pool")}
        return dict(csem=csem, dsem=dsem, ccount={e: 0 for e in csem}, dcount={e: [0] * Sched.N_DMA_SEMS for e in dsem})

    def __init__(self, nc, sems=None):
        self.nc = nc
        self.sems = sems
        Sched._uid[0] += 1
        self.uid = Sched._uid[0]
        self.ops = {e: [] for e in self.ENGS}
        self.last_w = {}
        self.readers = {}

    def _add(self, eng, fn, is_dma, reads, writes):
        op = _Op(eng, fn, is_dma)
        deps = []
        raw = set()
        for k in reads:
            w = self.last_w.get(k)
            if w is not None:
                deps.append(w)
                raw.add(id(w))
        for k in writes:
            w = self.last_w.get(k)
            if w is not None:
                deps.append(w)
            deps.extend(self.readers.get(k, ()))
        for k in writes:
            self.last_w[k] = op
            self.readers[k] = []
        for k in reads:
            if k not in writes:
                self.readers.setdefault(k, []).append(op)
        seen = set()
        for d in deps:
            if d is op or id(d) in seen:
                continue
            seen.add(id(d))
            if d.eng == eng and not d.is_dma and not is_dma:
                if eng == "pe" or id(d) not in raw:
                    continue
            op.deps.append(d)
            d.inc = True
        self.ops[eng].append(op)
        return op

    def op(self, eng, fn, reads=(), writes=()):
        return self._add(eng, fn, False, tuple(reads), tuple(writes))

    def dma(self, eng, out, in_, reads=(), writes=()):
        return self._add(eng, lambda e: e.dma_start(out=out, in_=in_), True, tuple(reads), tuple(writes))

    def emit(self):
        nc = self.nc
        with contextlib.ExitStack() as st:
            pool_ = self.sems
            csem, dsem, ccount, dcount = pool_["csem"], pool_["dsem"], pool_["ccount"], pool_["dcount"]
            for e in self.ENGS:
                nd = 0
                for op in self.ops[e]:
                    if op.is_dma:
                        i = nd % self.N_DMA_SEMS
                        nd += 1
                        dcount[e][i] += 16
                        op.sem = dsem[e][i]
                        op.semval = dcount[e][i]
                    elif op.inc:
                        ccount[e] += 1
                        op.sem = csem[e]
                        op.semval = ccount[e]
            block = st.enter_context(nc.Block())

            def run(ename, eng):
                waited = {}
                last = {}
                for op in self.ops[ename]:
                    need = {}
                    for d in op.deps:
                        key = id(d.sem)
                        if waited.get(key, 0) >= d.semval:
                            continue
                        if key not in need or need[key][1] < d.semval:
                            need[key] = (d.sem, d.semval)
                    for key, (sem, val) in need.items():
                        eng.wait_ge(sem, val)
                        waited[key] = val
                    ins = op.fn(eng)
                    if op.is_dma:
                        ins.then_inc(op.sem, 16)
                        last[id(op.sem)] = (op.sem, op.semval)
                    elif op.inc:
                        ins.then_inc(op.sem, 1)
                for key, (sem, val) in last.items():
                    if waited.get(key, 0) < val:
                        eng.wait_ge(sem, val)

            block.tensor(lambda e: run("pe", e))
            block.scalar(lambda e: run("act", e))
            block.vector(lambda e: run("dve", e))
            block.gpsimd(lambda e: run("pool", e))
            block.sync(lambda e: run("sp", e))


C_BIN, C_G1, C_G2, C_MCW, C_MCB, C_HG, C_SK, C_FCW, C_FCB, C_BGLU, C_SD = 0, 36, 44, 52, 84, 92, 100, 108, 240, 284, 288
NCOL = 292
IN_PERM = np.concatenate([np.arange(0, 2048), np.arange(2056, 4616)])
SL_A, SL_B, SL_KT, SL_VT, SL_GLU, SL_BRA, SL_BRB, SL_WO, SL_UP, SL_DN = 0, 18, 20, 21, 22, 23, 27, 29, 33, 55
ORDER = (list(range(0, 10)) + [18, 19, 20, 21, 22] + list(range(10, 18)) + list(range(23, 67)))


def _prep(inp):
    f = lambda a: np.ascontiguousarray(np.asarray(a, dtype=np.float32))
    w_in = f(inp["w_in"][0])
    w_in_p = w_in[:, IN_PERM]
    b_in = f(inp["b_in"][0])
    slabs = np.zeros((NS, 128, 2048), np.float32)

    def put(s, col, blk):
        slabs[s, :, col:col + blk.shape[1]] = blk

    for mc in range(36):
        for kt in range(8):
            put(SL_A + mc // 2, ((mc % 2) * 8 + kt) * 128, w_in_p[kt * 128:(kt + 1) * 128, mc * 128:(mc + 1) * 128])
    wq, wk, wv = f(inp["m_wq"][0]), f(inp["m_wk"][0]), f(inp["m_wv"][0])
    for h in range(4):
        for qk, W in enumerate((wq, wk)):
            for ec in range(2):
                for kt in range(2):
                    col = ((((h % 2) * 2 + qk) * 2 + ec) * 2 + kt) * 128
                    put(SL_B + h // 2, col, W[h, kt * 128:(kt + 1) * 128, ec * 128:(ec + 1) * 128])
        for kt in range(2):
            put(SL_KT, (h * 2 + kt) * 256, wk[h, kt * 128:(kt + 1) * 128, :])
            put(SL_VT, (h * 2 + kt) * 256, wv[h, kt * 128:(kt + 1) * 128, :])
    wglu = f(inp["s_w_glu"][0])
    for jc in range(4):
        for kt in range(4):
            put(SL_GLU, (jc * 4 + kt) * 128, wglu[kt * 128:(kt + 1) * 128, jc * 128:(jc + 1) * 128])
    wa, wb, wo = f(inp["w_branch_a"][0]), f(inp["w_branch_b"][0]), f(inp["w_out"][0])
    for mc in range(8):
        for kt in range(8):
            put(SL_BRA + mc // 2, ((mc % 2) * 8 + kt) * 128, wa[kt * 128:(kt + 1) * 128, mc * 128:(mc + 1) * 128])
        for kt in range(4):
            put(SL_BRB + mc // 4, ((mc % 4) * 4 + kt) * 128, wb[kt * 128:(kt + 1) * 128, mc * 128:(mc + 1) * 128])
    for nh in range(2):
        for kt in range(8):
            put(SL_WO + nh * 2 + kt // 4, (kt % 4) * 512, wo[kt * 128:(kt + 1) * 128, nh * 512:(nh + 1) * 512])
    wup, wdn = f(inp["w_up"][0]), f(inp["w_down"][0])
    for i in range(22):
        for vg in range(2):
            mc = i + 22 * vg
            for kt in range(8):
                put(SL_UP + i, (vg * 8 + kt) * 128, wup[kt * 128:(kt + 1) * 128, mc * 128:(mc + 1) * 128])
    for nh in range(2):
        for kt in range(22):
            put(SL_DN + nh * 6 + kt // 4, (kt % 4) * 512, wdn[kt * 128:(kt + 1) * 128, nh * 512:(nh + 1) * 512])

    ccol = np.zeros((128, NCOL), np.float32)
    col = lambda v, n: f(v).reshape(n, 128).T
    ccol[:, C_BIN:C_BIN + 36] = col(b_in[IN_PERM], 36)
    ccol[:, C_G1:C_G1 + 8] = col(inp["mix_norm_g"][0], 8)
    ccol[:, C_G2:C_G2 + 8] = col(inp["ffn_norm_g"][0], 8)
    mcw = f(inp["m_conv_w"][0])
    ccol[:, C_MCW:C_MCW + 32] = mcw.T.reshape(8, 128, 4).transpose(1, 0, 2).reshape(128, 32)
    ccol[:, C_MCB:C_MCB + 8] = col(inp["m_conv_b"][0], 8)
    ccol[:, C_HG:C_HG + 8] = col(inp["m_head_g"][0], 8)
    ccol[:, C_SK:C_SK + 8] = col(inp["m_skip"][0], 8)
    fcw = f(inp["ffn_conv_w"][0])
    ccol[:, C_FCW:C_FCW + 132] = fcw.T.reshape(44, 128, 3).transpose(1, 0, 2).reshape(128, 132)
    ccol[:, C_FCB:C_FCB + 44] = col(inp["ffn_conv_b"][0], 44)
    ccol[:, C_BGLU:C_BGLU + 4] = col(inp["s_b_glu"][0], 4)
    ccol[:, C_SD:C_SD + 4] = col(f(inp["s_d"][0]).reshape(-1), 4)
    crow = np.zeros((128, 1032), np.float32)
    crow[:, 0:1024] = f(inp["final_norm_g"])[None, :]
    crow[:, 1024:1032] = b_in[2048:2056][None, :]
    wg = np.ascontiguousarray(w_in[:, 2048:2056].reshape(8, 128, 8).transpose(1, 0, 2).reshape(128, 64))

    are, aim, ldt = f(inp["s_a_re"][0]), f(inp["s_a_im"][0]), f(inp["s_log_dt"][0])
    bre, bim = f(inp["s_b_re"][0]), f(inp["s_b_im"][0])
    cre, cim = f(inp["s_c_re"][0]), f(inp["s_c_im"][0])
    toL = lambda a: a.reshape(16, 2, 64).transpose(1, 2, 0).reshape(128, 16)
    rep = lambda a: np.broadcast_to(toL(a)[:, :, None], (128, 16, 32)).reshape(128, 512)
    s5X = np.stack([rep(are), rep(aim), rep(np.broadcast_to(ldt[:, None], (32, 64)))], axis=1)

    def toT(a):
        t = a.reshape(4, 4, 2, 64)
        t = np.broadcast_to(t[:, :, None, None, :, :], (4, 4, 2, 16, 2, 64))
        return t.transpose(1, 2, 3, 0, 4, 5).reshape(128, 4, 128)

    s5T = np.stack([toT(are), toT(aim), toT(np.broadcast_to(ldt[:, None], (32, 64)))], axis=1)

    def bT(b):
        t = b.reshape(4, 4, 2, 64, 16)
        o = np.zeros((4, 2, 16, 4, 2, 64), np.float32)
        for g2 in range(2):
            o[:, g2, :, :, g2, :] = t[:, :, g2].transpose(1, 3, 0, 2)
        return o.reshape(128, 4, 128)

    def bX(b):
        t = b.reshape(16, 2, 64, 16)
        o = np.zeros((2, 64, 16, 2, 16), np.float32)
        for g2 in range(2):
            o[g2, :, :, g2, :] = t[:, g2].transpose(1, 0, 2)
        return o.reshape(128, 16, 32)

    BtD = np.stack([bT(bre), bT(bim)], axis=1)
    BxD = np.stack([bX(bre), bX(bim)], axis=1)
    CxD = np.stack([bX(cre.transpose(0, 2, 1)), bX(cim.transpose(0, 2, 1))], axis=1)
    return dict(wall=slabs, ccol=ccol, crow=crow, wg=wg,
                s5X=np.ascontiguousarray(s5X), s5T=np.ascontiguousarray(s5T.reshape(128, 3, 512)),
                BtD=np.ascontiguousarray(BtD.reshape(128, 2, 512)), BxD=np.ascontiguousarray(BxD.reshape(128, 2, 512)),
                CxD=np.ascontiguousarray(CxD.reshape(128, 2, 512)))


C1G = math.sqrt(2.0 / math.pi)


def _cmul(dve, o_re, o_im, a_re, a_im, b_re, b_im, t1, t2):
    dve(lambda e: e.tensor_tensor(t1, a_re, b_re, ALU.mult))
    dve(lambda e: e.tensor_tensor(t2, a_im, b_im, ALU.mult))
    dve(lambda e: e.tensor_tensor(t1, t1, t2, ALU.subtract))
    dve(lambda e: e.tensor_tensor(t2, a_re, b_im, ALU.mult))
    dve(lambda e: e.tensor_tensor(o_im, a_im, b_re, ALU.mult))
    dve(lambda e: e.tensor_tensor(o_im, o_im, t2, ALU.add))
    dve(lambda e: e.tensor_copy(o_re, t1))


def _s5_params(S, par, W):
    k = ["s5"]
    dve = lambda fn: S.op("dve", fn, k, k)
    act = lambda fn: S.op("act", fn, k, k)
    are, aim, ldt = par[:, 0, :], par[:, 1, :], par[:, 2, :]
    dt, dre, dim, mag, c, s, t1, t2 = (W["w%d" % i][:] for i in range(8))
    act(lambda e: e.activation(dt, ldt, AF.Exp))
    dve(lambda e: e.tensor_tensor(dre, dt, are, ALU.mult))
    dve(lambda e: e.tensor_tensor(dim, dt, aim, ALU.mult))
    act(lambda e: e.activation(mag, dre, AF.Exp))
    act(lambda e: e.activation(W["R8"][:], dre, AF.Exp, scale=8.0))
    act(lambda e: e.activation(s, dim, AF.Sin, scale=1.0 / 16.0))
    act(lambda e: e.activation(c, dim, AF.Sin, bias=W["hpi"], scale=1.0 / 16.0))

    def square():
        dve(lambda e: e.tensor_tensor(t1, c, c, ALU.mult))
        dve(lambda e: e.tensor_tensor(t2, s, s, ALU.mult))
        dve(lambda e: e.scalar_tensor_tensor(s, c, 2.0, s, ALU.mult, ALU.mult))
        dve(lambda e: e.tensor_tensor(c, t1, t2, ALU.subtract))

    for _ in range(4):
        square()
    dve(lambda e: e.tensor_tensor(W["ab_re"][:], mag, c, ALU.mult))
    dve(lambda e: e.tensor_tensor(W["ab_im"][:], mag, s, ALU.mult))
    for _ in range(3):
        square()
    dve(lambda e: e.tensor_copy(W["c8"][:], c))
    dve(lambda e: e.tensor_copy(W["s8"][:], s))
    xr, den = dt, dre
    dve(lambda e: e.tensor_scalar_add(xr, W["ab_re"][:], -1.0))
    dve(lambda e: e.tensor_tensor(t1, are, are, ALU.mult))
    dve(lambda e: e.tensor_tensor(t2, aim, aim, ALU.mult))
    dve(lambda e: e.tensor_tensor(den, t1, t2, ALU.add))
    dve(lambda e: e.reciprocal(den, den))
    dve(lambda e: e.tensor_tensor(t1, xr, are, ALU.mult))
    dve(lambda e: e.tensor_tensor(t2, W["ab_im"][:], aim, ALU.mult))
    dve(lambda e: e.tensor_tensor(t1, t1, t2, ALU.add))
    dve(lambda e: e.tensor_tensor(W["r_re"][:], t1, den, ALU.mult))
    dve(lambda e: e.tensor_tensor(t1, W["ab_im"][:], are, ALU.mult))
    dve(lambda e: e.tensor_tensor(t2, xr, aim, ALU.mult))
    dve(lambda e: e.tensor_tensor(t1, t1, t2, ALU.subtract))
    dve(lambda e: e.tensor_tensor(W["r_im"][:], t1, den, ALU.mult))


def build_nc(debug=None, nst=NST):
    nc = bass.Bass("TRN2", target_bir_lowering=False)
    din = lambda n, s: nc.dram_tensor(n, s, F32, kind="ExternalInput").ap()
    x_d = din("x", [SEQ, DM])
    wall_d = din("wall", [NS, 128, 2048])
    ccol_d = din("ccol", [128, NCOL])
    crow_d = din("crow", [128, 1032])
    wg_d = din("wg", [128, 64])
    s5X_d = din("s5X", [128, 3, 512])
    s5T_d = din("s5T", [128, 3, 512])
    Bt_d = din("BtD", [128, 2, 512])
    Bx_d = din("BxD", [128, 2, 512])
    Cx_d = din("CxD", [128, 2, 512])
    y_d = nc.dram_tensor("y", [SEQ, DM], F32, kind="ExternalOutput").ap()
    wsl_d = nc.dram_tensor("wsl", [NS, 128, 2048], BF16, kind="Internal").ap()

    with contextlib.ExitStack() as st0:
        def T0(name, shape, dt=F32):
            return st0.enter_context(nc.sbuf_tensor("s_" + name, shape, dt))

        SEMS = Sched.make_sems(nc, st0)
        WZt = T0("WZt", [128, 4, 2, 8, 128], BF16)
        WI = T0("WI", [128, 16, 2, 8, 32], BF16)
        Kt = T0("Kt", [128, 4, 8, 128], BF16)
        Ec = T0("Ec", [128, 16, 64])
        Es = T0("Es", [128, 16, 64])
        Rtab = T0("Rtab", [128, 16, 64])
        Rl = T0("Rl", [128, 16])
        ccol = T0("ccol", [128, NCOL])
        chalf = T0("chalf", [128, NCOL])
        crow = T0("crow", [128, 1032])
        wgb = T0("wgb", [128, 64], BF16)
        ident = T0("ident", [128, 128], BF16)
        cmask = T0("cmask", [128, 128], BF16)
        cmask4 = T0("cmask4", [128, 512], BF16)
        LT = T0("LT", [128, 128])
        ONES = T0("ONES", [128, 128])
        small = T0("small", [128, 256])
        psf = [st0.enter_context(nc.psum_tensor("psf%d" % i, [128, 512], F32)) for i in range(6)]
        psb = [st0.enter_context(nc.psum_tensor("psb%d" % i, [128, 1024], BF16)) for i in range(2)]

        def cc(off, i=0):
            return ccol[:, off + i:off + i + 1]

        def ch(off, i=0):
            return chalf[:, off + i:off + i + 1]

        with contextlib.ExitStack() as st1:
            def T1(name, shape, dt=F32):
                return st1.enter_context(nc.sbuf_tensor("a_" + name, shape, dt))

            S = Sched(nc, SEMS)
            dve = lambda fn, r=(), w=(): S.op("dve", fn, r, w)
            act = lambda fn, r=(), w=(): S.op("act", fn, r, w)
            pool = lambda fn, r=(), w=(): S.op("pool", fn, r, w)
            pe = lambda fn, r=(), w=(): S.op("pe", fn, r, w)
            NSTG = 3
            w32 = [T1("w32_%d" % i, [128, 2048]) for i in range(NSTG)]
            w16 = [T1("w16_%d" % i, [128, 2048], BF16) for i in range(NSTG)]
            S.dma("sp", ccol[:], ccol_d, writes=["ccol"])
            S.dma("sp", crow[:], crow_d, writes=["crow"])
            S.dma("sp", w32[0][:, 0:64], wg_d, writes=["w32_0"])
            act(lambda e: e.copy(wgb[:], w32[0][:, 0:64]), ["w32_0"], ["wgb"])
            act(lambda e: e.mul(chalf[:], ccol[:], 0.5), ["ccol"], ["chalf"])
            pool(lambda e: e.memset(ident[:], 0.0), [], ["ident"])
            pool(lambda e: e.affine_select(out=ident[:], in_=ident[:], compare_op=ALU.not_equal, fill=1.0, base=0,
                                           pattern=[[-1, 128]], channel_multiplier=1), ["ident"], ["ident"])
            pool(lambda e: e.memset(cmask[:], 1.0), [], ["cmask"])
            pool(lambda e: e.affine_select(out=cmask[:], in_=cmask[:], compare_op=ALU.is_ge, fill=0.0, base=0,
                                           pattern=[[1, 128]], channel_multiplier=-1), ["cmask"], ["cmask"])
            pool(lambda e: e.memset(cmask4[:], 1.0), [], ["cmask4"])
            pool(lambda e: e.affine_select(out=cmask4[:, :].rearrange("p (h q) -> p h q", h=4), in_=cmask4[:, :].rearrange("p (h q) -> p h q", h=4),
                                           compare_op=ALU.is_ge, fill=0.0, base=0, pattern=[[0, 4], [1, 128]], channel_multiplier=-1), ["cmask4"], ["cmask4"])
            pool(lambda e: e.memset(LT[:], 1.0), [], ["LT"])
            pool(lambda e: e.affine_select(out=LT[:], in_=LT[:], compare_op=ALU.is_ge, fill=0.0, base=-1,
                                           pattern=[[-1, 128]], channel_multiplier=1), ["LT"], ["LT"])
            pool(lambda e: e.memset(ONES[:], 1.0), [], ["ONES"])
            pool(lambda e: e.memset(small[:], 0.0), [], ["small"])
            pool(lambda e: e.memset(small[:, 250:251], EPS), ["small"], ["small"])
            pool(lambda e: e.memset(small[:, 251:252], 1.0), ["small"], ["small"])
            pool(lambda e: e.memset(small[:, 252:253], math.pi / 2.0), ["small"], ["small"])
            k5 = ["s5"]
            d5 = lambda fn: S.op("dve", fn, k5, k5)
            a5 = lambda fn: S.op("act", fn, k5, k5)
            parT = T1("parT", [128, 3, 512])
            parX = T1("parX", [128, 3, 512])
            BtS = T1("BtS", [128, 2, 512])
            BxS = T1("BxS", [128, 2, 512])
            CxS = T1("CxS", [128, 2, 512])
            S.dma("sp", parT[:], s5T_d, writes=["ld0"])
            S.dma("sp", parX[:], s5X_d, writes=["ld1"])
            S.dma("sp", BtS[:], Bt_d, writes=["ld2"])
            S.dma("sp", BxS[:], Bx_d, writes=["ld3"])
            S.dma("sp", CxS[:], Cx_d, writes=["ld4"])
            S.op("dve", lambda e: e.memset(small[:, 253:254], 0.0), ["ld0", "ld1", "ld2", "ld3", "ld4", "small"], k5)
            for s_ in range(NS):
                S.dma("pool", wsl_d[s_], wall_d[s_], reads=[], writes=["wsl%d" % s_])
            names = ["w%d" % i for i in range(8)] + ["ab_re", "ab_im", "r_re", "r_im", "c8", "s8", "R8"]
            WT = {n: T1("T_" + n, [128, 512]) for n in names}
            WX = {n: T1("X_" + n, [128, 512]) for n in names}
            WT["hpi"] = small[:, 252:253]
            WX["hpi"] = small[:, 252:253]
            _s5_params(S, parT, WT)
            _s5_params(S, parX, WX)
            cur_re, cur_im, t1, t2 = T1("cur_re", [128, 512]), T1("cur_im", [128, 512]), T1("t1", [128, 512]), T1("t2", [128, 512])
            _cmul(d5, cur_re[:], cur_im[:], WT["r_re"][:], WT["r_im"][:], BtS[:, 0, :], BtS[:, 1, :], t1[:], t2[:])
            for kk in range(8):
                j = 7 - kk
                a5(lambda e, j=j: e.copy(WZt[:, :, 0, j, :], cur_re[:, :].rearrange("p (c m) -> p c m", c=4)))
                a5(lambda e, j=j: e.copy(WZt[:, :, 1, j, :], cur_im[:, :].rearrange("p (c m) -> p c m", c=4)))
                if kk < 7:
                    _cmul(d5, cur_re[:], cur_im[:], cur_re[:], cur_im[:], WT["ab_re"][:], WT["ab_im"][:], t1[:], t2[:])
            _cmul(d5, cur_re[:], cur_im[:], CxS[:, 0, :], CxS[:, 1, :], WX["ab_re"][:], WX["ab_im"][:], t1[:], t2[:])
            for j in range(8):
                a5(lambda e, j=j: e.copy(WI[:, :, 0, j, :], cur_re[:, :].rearrange("p (g m) -> p g m", g=16)))
                a5(lambda e, j=j: e.mul(WI[:, :, 1, j, :], cur_im[:, :].rearrange("p (g m) -> p g m", g=16), -1.0))
                if j < 7:
                    _cmul(d5, cur_re[:], cur_im[:], cur_re[:], cur_im[:], WX["ab_re"][:], WX["ab_im"][:], t1[:], t2[:])
            Cb_re = T1("Cb_re", [128, 512], BF16)
            nCb_im = T1("nCb_im", [128, 512], BF16)
            Xb_re = T1("Xb_re", [128, 512], BF16)
            Xb_im = T1("Xb_im", [128, 512], BF16)
            a5(lambda e: e.copy(Cb_re[:], CxS[:, 0, :]))
            a5(lambda e: e.mul(nCb_im[:], CxS[:, 1, :], -1.0))
            _cmul(d5, cur_re[:], cur_im[:], WX["r_re"][:], WX["r_im"][:], BxS[:, 0, :], BxS[:, 1, :], t1[:], t2[:])
            for tau in range(8):
                a5(lambda e: e.copy(Xb_re[:], cur_re[:]))
                a5(lambda e: e.copy(Xb_im[:], cur_im[:]))
                d5(lambda e: e.memset(psf[0][:, :], 0.0))
                for gp in range(16):
                    chunk, win = gp // 4, gp % 4
                    o = psf[0][32 * win:32 * win + 32, chunk * 128 + 32 * win:chunk * 128 + 32 * win + 32]
                    S.op("pe", lambda e, o=o, gp=gp, win=win: e.matmul(o, lhsT=Xb_re[:, gp * 32:(gp + 1) * 32], rhs=Cb_re[:, gp * 32:(gp + 1) * 32],
                                                                     start=True, stop=False, tile_position=(0, 32 * win)), k5, k5)
                    S.op("pe", lambda e, o=o, gp=gp, win=win: e.matmul(o, lhsT=Xb_im[:, gp * 32:(gp + 1) * 32], rhs=nCb_im[:, gp * 32:(gp + 1) * 32],
                                                                     start=False, stop=True, tile_position=(0, 32 * win)), k5, k5)
                if tau == 0:
                    for chunk in range(4):
                        S.op("dve", lambda e, chunk=chunk: e.scalar_tensor_tensor(Kt[:, chunk, 0, :], ident[:], cc(C_SD, chunk), psf[0][:, chunk * 128:(chunk + 1) * 128],
                                                                               ALU.mult, ALU.add), k5 + ["ident", "ccol"], k5)
                else:
                    d5(lambda e, tau=tau: e.tensor_copy(Kt[:, :, tau, :], psf[0][:, :].rearrange("p (c m) -> p c m", c=4)))
                if tau < 7:
                    _cmul(d5, cur_re[:], cur_im[:], cur_re[:], cur_im[:], WX["ab_re"][:], WX["ab_im"][:], t1[:], t2[:])
            c8 = WX["c8"][:, :].rearrange("p (g m) -> p g m", g=16)
            s8 = WX["s8"][:, :].rearrange("p (g m) -> p g m", g=16)
            R8 = WX["R8"][:, :].rearrange("p (g m) -> p g m", g=16)
            d5(lambda e: e.tensor_copy(Ec[:, :, 0:1], c8[:, :, 0:1]))
            d5(lambda e: e.tensor_copy(Es[:, :, 0:1], s8[:, :, 0:1]))
            d5(lambda e: e.tensor_copy(Rl[:, :].rearrange("p (g a) -> p g a", a=1), R8[:, :, 0:1]))
            d5(lambda e: e.memset(Rtab[:], 0.0))
            m = 1
            while m < 64:
                bre_b = Ec[:, :, m - 1:m].to_broadcast([128, 16, m])
                bim_b = Es[:, :, m - 1:m].to_broadcast([128, 16, m])
                u1 = t1[:, 0:16 * m].rearrange("p (g k) -> p g k", g=16)
                u2 = t2[:, 0:16 * m].rearrange("p (g k) -> p g k", g=16)
                d5(lambda e, m=m, bre_b=bre_b, u1=u1: e.tensor_tensor(u1, Ec[:, :, 0:m], bre_b, ALU.mult))
                d5(lambda e, m=m, bim_b=bim_b, u2=u2: e.tensor_tensor(u2, Es[:, :, 0:m], bim_b, ALU.mult))
                d5(lambda e, m=m, u1=u1, u2=u2: e.tensor_tensor(u1, u1, u2, ALU.subtract))
                d5(lambda e, m=m, bre_b=bre_b, u2=u2: e.tensor_tensor(u2, Es[:, :, 0:m], bre_b, ALU.mult))
                d5(lambda e, m=m, u1=u1: e.tensor_copy(Ec[:, :, m:2 * m], u1))
                d5(lambda e, m=m, bim_b=bim_b, u1=u1: e.tensor_tensor(u1, Ec[:, :, 0:m], bim_b, ALU.mult))
                d5(lambda e, m=m, u1=u1, u2=u2: e.tensor_tensor(Es[:, :, m:2 * m], u1, u2, ALU.add))
                m *= 2
            d5(lambda e: e.memset(Rtab[:, :, 1:64], 1.0))
            d5(lambda e: e.tensor_tensor(Rtab[:, :, 1:64], Rtab[:, :, 1:64], Rl[:, :].rearrange("p (g a) -> p g a", a=1).to_broadcast([128, 16, 63]), ALU.mult))
            S.emit()

        with contextlib.ExitStack() as st:
            def T(name, shape, dt=F32):
                return st.enter_context(nc.sbuf_tensor("m_" + name, shape, dt))

            S = Sched(nc, SEMS)
            dve = lambda fn, r=(), w=(): S.op("dve", fn, r, w)
            act = lambda fn, r=(), w=(): S.op("act", fn, r, w)
            pool = lambda fn, r=(), w=(): S.op("pool", fn, r, w)
            pe = lambda fn, r=(), w=(): S.op("pe", fn, r, w)
            x_sb = T("x_sb", [128, 4, DM])
            hT = T("hT", [128, 8, TT], BF16)
            xmT = T("xmT", [128, 8, TT + 4], BF16)
            xcar = T("xcar", [128, 8, 4], BF16)
            som = T("som", [128, 8, TT], BF16)
            usT = T("usT", [128, 4, TT], BF16)
            xcT = T("xcT", [128, 8, TT], BF16)
            R1 = T("R1", [128, 12288], BF16)
            vp = T("vp", [128, 4, 4, 256], BF16)
            C32 = T("C32", [128, 4, 2, 256])
            Cb = T("Cb", [128, 4, 2, 256], BF16)
            n32 = T("n32", [128, 4, 2])
            nb = T("nb", [128, 4, 2], BF16)
            wb16 = T("wb16", [128, 4, 4], BF16)
            glT = T("glT", [128, 4, TT], BF16)
            ring = T("ring", [128, RING, 2048], BF16)
            Sre = T("Sre", [128, 16, 65])
            Sim = T("Sim", [128, 16, 65])
            Sbre = T("Sbre", [128, 16, 64], BF16)
            Sbim = T("Sbim", [128, 16, 64], BF16)
            NF, NB = 6, 3
            fring = T("fring", [128, NF, 512])
            bring = T("bring", [128, NB, 512], BF16)
            rstate = {"f": 0, "b": 0}

            def f32buf():
                rstate["f"] = (rstate["f"] + 1) % NF
                return fring[:, rstate["f"], :], "fr%d" % rstate["f"]

            def bfbuf():
                rstate["b"] = (rstate["b"] + 1) % NB
                return bring[:, rstate["b"], :], "br%d" % rstate["b"]
            fcar = T("fcar", [128, 44, 2])
            bnd = T("bnd", [128, 44, 2])
            XM = ["xmT%d" % c for c in range(8)]
            VP = ["vp%d" % s_ for s_ in range(4)]
            rk = lambda b: "R1_%d" % b
            hn_tm = xmT[:, :, :].rearrange("p a b -> p (a b)")[:, 0:4096].rearrange("p (s f) -> p s f", s=4)
            qT = R1[:, 0:4096].rearrange("p (c t) -> p c t", c=8)
            kT = R1[:, 4096:8192].rearrange("p (c t) -> p c t", c=8)
            k_tm = R1[:, 8192:12288].rearrange("p (s f) -> p s f", s=4)
            sga, sgb = qT, kT
            gT = R1[:, 0:11264].rearrange("p (c t) -> p c t", c=22)
            hnb_t = R1[:, 0:4096].rearrange("p (s f) -> p s f", s=4)
            mT = vp[:, :, :, :].rearrange("p a b c -> p (a b c)").rearrange("p (c t) -> p c t", c=8)
            aoutT, boutT = xcT, usT
            epsc, onec = small[:, 250:251], small[:, 251:252]

            pool(lambda e: e.memset(C32[:], 0.0), [], ["C32_%d" % h for h in range(4)])
            pool(lambda e: e.memset(xcar[:], 0.0), [], ["xcar"])
            pool(lambda e: e.memset(fcar[:], 0.0), [], ["fcar"])
            pool(lambda e: e.memset(Sre[:], 0.0), [], ["Sre"])
            pool(lambda e: e.memset(Sim[:], 0.0), [], ["Sim"])
            pool(lambda e: e.memset(vp[:], 0.0), [], VP)
            pool(lambda e: e.memset(Cb[:], 0.0), [], ["Cb%d" % h for h in range(4)])
            pool(lambda e: e.memset(n32[:], 0.0), [], ["n32"])
            pool(lambda e: e.memset(nb[:], 0.0), [], ["nb"])

            seq = [sid for _ in range(nst) for sid in ORDER]
            wstate = {"issued": 0, "cur": -1}

            def wnext():
                wstate["cur"] += 1
                i = wstate["cur"]
                while wstate["issued"] < min(len(seq), i + RING):
                    n = wstate["issued"]
                    S.dma("sp", ring[:, n % RING, :], wsl_d[seq[n]], reads=[], writes=["ring%d" % (n % RING)])
                    wstate["issued"] += 1
                return ring[:, i % RING, :], "ring%d" % (i % RING)

            def sig(out, in_, rkeys, wkeys, bias=None, scale=1.0, eng2="pool"):
                if bias is None:
                    act(lambda e: e.activation(out, in_, AF.Tanh, scale=0.5 * scale), rkeys, wkeys)
                else:
                    act(lambda e: e.activation(out, in_, AF.Tanh, bias=bias, scale=0.5 * scale), list(rkeys) + ["chalf"], wkeys)
                S.op(eng2, lambda e: e.tensor_scalar(out, out, 0.5, 0.5, ALU.mult, ALU.add), wkeys, wkeys)

            psrr = {"i": 0}

            def nextps():
                psrr["i"] = (psrr["i"] + 1) % 4
                return psrr["i"], psf[psrr["i"]], "psf%d" % psrr["i"]

            def nextps6():
                psrr["i"] = (psrr["i"] + 1) % 6
                return psrr["i"], psf[psrr["i"]], "psf%d" % psrr["i"]

            def rms_rstd():
                ssq, rs = small[:, 0:4], small[:, 4:8]
                pool(lambda e: e.memset(ssq, 0.0), [], ["ssq%d" % i for i in range(4)])
                for sub in range(4):
                    act(lambda e, sub=sub: e.activation(hnb_t[:, sub, :], x_sb[:, sub, :], AF.Square, accum_out=ssq[:, sub:sub + 1]),
                        ["x_sb%d" % sub, "ssq%d" % sub], [rk(2 * sub), rk(2 * sub + 1), "ssq%d" % sub])
                    act(lambda e, sub=sub: e.activation(rs[:, sub:sub + 1], ssq[:, sub:sub + 1], AF.Ln, bias=epsc, scale=1.0 / DM), ["ssq%d" % sub], ["rs%d" % sub])
                    act(lambda e, sub=sub: e.activation(rs[:, sub:sub + 1], rs[:, sub:sub + 1], AF.Exp, scale=-0.5), ["rs%d" % sub], ["rs%d" % sub])
                return rs

            def norm_T(gcol_off):
                rs = rms_rstd()
                for sub in range(4):
                    dve(lambda e, sub=sub: e.tensor_scalar_mul(hnb_t[:, sub, :], x_sb[:, sub, :], rs[:, sub:sub + 1]),
                        ["x_sb%d" % sub, "rs%d" % sub], [rk(2 * sub), rk(2 * sub + 1)])
                for fc in range(8):
                    pb = psb[fc % 2]
                    for sub in range(4):
                        pe(lambda e, fc=fc, sub=sub, pb=pb: e.transpose(pb[:, sub * 128:(sub + 1) * 128], hnb_t[:, sub, fc * 128:(fc + 1) * 128], ident[:]),
                           [rk(2 * sub), rk(2 * sub + 1)], ["psb%d" % (fc % 2)])
                    dve(lambda e, fc=fc, pb=pb: e.tensor_scalar_mul(hT[:, fc, :], pb[:, 0:512], cc(gcol_off, fc)),
                        ["psb%d" % (fc % 2)], ["hT%d" % fc])

            def dump_T(src, nchunk, keys, stg):
                for c in range(nchunk):
                    fb, fk = f32buf()
                    dve(lambda e, c=c, fb=fb: e.tensor_copy(fb, src[:, c, :]), list(keys), [fk])
                    S.dma("pool", y_d[c * 128:(c + 1) * 128, stg * 512:(stg + 1) * 512], fb, reads=[fk], writes=["y"])

            def inproj_chunk(mc, slot, skey):
                pi, ps, pk = nextps()
                base = ((mc % 2) * 8) * 128
                for kt in range(8):
                    pe(lambda e, kt=kt, ps=ps: e.matmul(ps[:, :], lhsT=slot[:, base + kt * 128: base + (kt + 1) * 128], rhs=hT[:, kt, :],
                                                          start=(kt == 0), stop=(kt == 7)), [skey, "hT%d" % kt], [pk])
                return ps, pk

            for stg in range(nst):
                t0 = stg * TT
                for sub in range(4):
                    S.dma("pool", x_sb[:, sub, :], x_d[t0 + sub * 128:t0 + (sub + 1) * 128, :], writes=["x_sb%d" % sub])
                norm_T(C_G1)
                pool(lambda e: e.tensor_copy(xmT[:, :, 1:4], xcar[:, :, 1:4]), ["xcar"], XM)
                def conv_chunk(c):
                    z, zk = f32buf()
                    sg, sk = bfbuf()
                    dve(lambda e: e.tensor_scalar(z, xmT[:, c, 1:TT + 1], cc(C_MCW, c * 4 + 0), cc(C_MCB, c), ALU.mult, ALU.add),
                        ["xmT%d" % c], [zk])
                    for k in range(1, 4):
                        dve(lambda e, k=k: e.scalar_tensor_tensor(z, xmT[:, c, 1 + k:TT + 1 + k], cc(C_MCW, c * 4 + k), z, ALU.mult, ALU.add),
                            ["xmT%d" % c, zk], [zk])
                    sig(sg, z, [zk], [sk])
                    dve(lambda e: e.tensor_tensor(xcT[:, c, :], z, sg, ALU.mult), [zk, sk], ["xcT%d" % c])

                for mc in range(20):
                    if mc % 2 == 0:
                        slot, skey = wnext()
                    ps, pk = inproj_chunk(mc, slot, skey)
                    if mc < 8:
                        act(lambda e, mc=mc, ps=ps: e.activation(xmT[:, mc, 4:TT + 4], ps[:, :], AF.Identity, bias=cc(C_BIN, mc)),
                            [pk], ["xmT%d" % mc])
                        conv_chunk(mc)
                    elif mc < 16:
                        sig(som[:, mc - 8, :], ps[:, :], [pk], ["som%d" % (mc - 8)], bias=ch(C_BIN, mc))
                    else:
                        act(lambda e, mc=mc, ps=ps: e.activation(usT[:, mc - 16, :], ps[:, :], AF.Identity, bias=cc(C_BIN, mc)),
                            [pk], ["usT%d" % (mc - 16)])
                if debug == "h":
                    dump_T(hT, 8, ["hT%d" % c for c in range(8)], stg)
                if debug == "xm":
                    dump_T(xmT[:, :, 4:TT + 4], 8, XM, stg)
                gsb = small[:, 16:48].rearrange("p (s g) -> p s g", s=4)
                for sub in range(4):
                    for kt in range(8):
                        pe(lambda e, sub=sub, kt=kt: e.matmul(psf[4][:, sub * 8:(sub + 1) * 8], lhsT=hT[:, kt, sub * 128:(sub + 1) * 128],
                                                              rhs=wgb[:, kt * 8:(kt + 1) * 8], start=(kt == 0), stop=(kt == 7)),
                           ["hT%d" % kt], ["psf4"])
                for sub in range(4):
                    dve(lambda e, sub=sub: e.tensor_tensor(gsb[:, sub, :], psf[4][:, sub * 8:(sub + 1) * 8], crow[:, 1024:1032], ALU.add),
                        ["psf4"], ["gsb"])
                pool(lambda e: e.tensor_copy(xcar[:, :, 1:4], xmT[:, :, TT + 1:TT + 4]), XM, ["xcar"])
                if debug == "xc":
                    dump_T(xcT, 8, ["xcT%d" % c for c in range(8)], stg)
                lfn = small[:, 48:64].rearrange("p (s h) -> p s h", s=4)
                act(lambda e: e.activation(lfn, gsb[:, :, 4:8], AF.Exp, scale=-1.0), ["gsb"], ["lfn"])
                act(lambda e: e.activation(lfn, lfn, AF.Ln, bias=onec, scale=1.0), ["lfn"], ["lfn"])
                for sub in range(4):
                    pe(lambda e, sub=sub: e.matmul(psf[4][:, 64 + sub * 8:64 + sub * 8 + 4], lhsT=LT[:], rhs=lfn[:, sub, :], start=True, stop=True),
                       ["lfn"], ["psf4"])
                    pe(lambda e, sub=sub: e.matmul(psf[4][:, 64 + sub * 8 + 4:64 + sub * 8 + 8], lhsT=ONES[:], rhs=lfn[:, sub, :], start=True, stop=True),
                       ["lfn"], ["psf4"])
                Pm = psf[4][:, 64:96].rearrange("p (s g) -> p s g", s=4)
                wcol = small[:, 64:80].rearrange("p (s h) -> p s h", s=4)
                e2c = small[:, 80:96].rearrange("p (s h) -> p s h", s=4)
                dcol = small[:, 96:112].rearrange("p (s h) -> p s h", s=4)
                dve(lambda e: e.tensor_tensor(wcol, gsb[:, :, 0:4], Pm[:, :, 0:4], ALU.subtract), ["gsb", "psf4"], ["wcol"])
                act(lambda e: e.activation(wcol, wcol, AF.Exp), ["wcol"], ["wcol"])
                act(lambda e: e.copy(wb16[:], wcol), ["wcol"], ["wb16"])
                act(lambda e: e.activation(e2c, Pm[:, :, 0:4], AF.Exp, scale=-1.0), ["psf4"], ["e2c"])
                act(lambda e: e.activation(dcol, Pm[:, :, 4:8], AF.Exp, scale=-1.0), ["psf4"], ["dcol"])
                for hh in range(2):
                    slot, skey = wnext()
                    for hl in range(2):
                        h = hh * 2 + hl
                        for qk in range(2):
                            for ec in range(2):
                                pi, ps, pk = nextps()
                                for kt in range(2):
                                    col = ((((hl * 2 + qk) * 2 + ec) * 2 + kt)) * 128
                                    pe(lambda e, ps=ps, col=col, h=h, kt=kt, slot=slot: e.matmul(ps[:, :], lhsT=slot[:, col:col + 128], rhs=xcT[:, h * 2 + kt, :],
                                                                                                start=(kt == 0), stop=(kt == 1)),
                                       [skey, "xcT%d" % (h * 2 + kt)], [pk])
                                dst = (qT, kT)[qk]
                                act(lambda e, ps=ps, dst=dst, h=h, ec=ec, qk=qk: e.activation(dst[:, h * 2 + ec, :], ps[:, :], AF.Identity, scale=(1.0, 1.0 / 16.0)[qk]),
                                    [pk], [rk(qk * 8 + h * 2 + ec)])
                slot, skey = wnext()
                for sub in range(4):
                    for hp in range(2):
                        pi, ps, pk = nextps()
                        for hl in range(2):
                            h = hp * 2 + hl
                            for kt in range(2):
                                pe(lambda e, ps=ps, h=h, hl=hl, kt=kt, sub=sub, slot=slot: e.matmul(ps[:, hl * 256:(hl + 1) * 256], lhsT=xcT[:, h * 2 + kt, sub * 128:(sub + 1) * 128],
                                                                                                   rhs=slot[:, (h * 2 + kt) * 256:(h * 2 + kt + 1) * 256], start=(kt == 0), stop=(kt == 1)),
                                   [skey, "xcT%d" % (h * 2 + kt)], [pk])
                        act(lambda e, ps=ps, sub=sub, hp=hp: e.activation(k_tm[:, sub, hp * 512:(hp + 1) * 512], ps[:, :], AF.Identity, scale=1.0 / 16.0),
                            [pk], [rk(16 + sub * 2 + hp)])
                slot, skey = wnext()
                for sub in range(4):
                    for hp in range(2):
                        pi, ps, pk = nextps()
                        for hl in range(2):
                            h = hp * 2 + hl
                            for kt in range(2):
                                pe(lambda e, ps=ps, h=h, hl=hl, kt=kt, sub=sub, slot=slot: e.matmul(ps[:, hl * 256:(hl + 1) * 256], lhsT=xmT[:, h * 2 + kt, 4 + sub * 128:4 + (sub + 1) * 128],
                                                                                                   rhs=slot[:, (h * 2 + kt) * 256:(h * 2 + kt + 1) * 256], start=(kt == 0), stop=(kt == 1)),
                                   [skey, "xmT%d" % (h * 2 + kt)], [pk])
                        for hl in range(2):
                            h = hp * 2 + hl
                            dve(lambda e, ps=ps, sub=sub, h=h, hl=hl: e.tensor_scalar_mul(vp[:, sub, h, 0:256], ps[:, hl * 256:(hl + 1) * 256], wcol[:, sub, h:h + 1]),
                                [pk, "wcol"], VP)
                st6 = small[:, 144:168].rearrange("p (h s) -> p h s", h=4)
                mv = small[:, 168:176].rearrange("p (h s) -> p h s", h=4)
                dd, mx, rr = small[:, 176:180], small[:, 180:184], small[:, 184:188]
                for sub in range(4):
                    tok = slice(sub * 128, (sub + 1) * 128)
                    CBK = ["Cb%d" % h for h in range(4)]
                    for h in range(4):
                        act(lambda e, h=h, sub=sub: e.activation(Cb[:, h, :, :], C32[:, h, :, :], AF.Identity, scale=dcol[:, sub, h:h + 1]),
                            ["C32_%d" % h, "dcol"], ["Cb%d" % h])
                    dve(lambda e, sub=sub: e.tensor_tensor(nb[:], n32[:], dcol[:, sub, :].rearrange("p (h a) -> p h a", a=1).to_broadcast([128, 4, 2]), ALU.mult),
                        ["n32", "dcol"], ["nb"])
                    for h in range(4):
                        for ec in range(2):
                            pe(lambda e, h=h, ec=ec, tok=tok: e.matmul(psf[5][:, h * 128:(h + 1) * 128], lhsT=kT[:, h * 2 + ec, tok], rhs=qT[:, h * 2 + ec, tok],
                                                                       start=(ec == 0), stop=(ec == 1)),
                               [rk(8 + h * 2 + ec), rk(h * 2 + ec)], ["psf5"])
                    sTb, sTk = bfbuf()
                    dve(lambda e, sTb=sTb: e.tensor_tensor(sTb, psf[5][:, :], cmask4[:], ALU.mult), ["psf5"], [sTk])
                    for h in range(4):
                        pn, pnk = psf[h // 2][:, (h % 2) * 256:(h % 2 + 1) * 256], "psf%d" % (h // 2)
                        pe(lambda e, pn=pn, sub=sub, h=h, sTb=sTb: e.matmul(pn, lhsT=sTb[:, h * 128:(h + 1) * 128], rhs=vp[:, sub, h, :], start=True, stop=False),
                           [sTk] + VP, [pnk])
                        for ec in range(2):
                            pe(lambda e, pn=pn, h=h, ec=ec, tok=tok: e.matmul(pn, lhsT=qT[:, h * 2 + ec, tok], rhs=Cb[:, h, ec, :], start=False, stop=(ec == 1)),
                               [rk(h * 2 + ec), "Cb%d" % h], [pnk])
                        pd = psf[4][:, 128 + h:129 + h]
                        pe(lambda e, pd=pd, sub=sub, h=h, sTb=sTb: e.matmul(pd, lhsT=sTb[:, h * 128:(h + 1) * 128], rhs=wb16[:, sub, h:h + 1], start=True, stop=False),
                           [sTk, "wb16"], ["psf4"])
                        for ec in range(2):
                            pe(lambda e, pd=pd, h=h, ec=ec, tok=tok: e.matmul(pd, lhsT=qT[:, h * 2 + ec, tok], rhs=nb[:, h, ec:ec + 1], start=False, stop=(ec == 1)),
                               [rk(h * 2 + ec), "nb"], ["psf4"])
                    for h in range(4):
                        pu, puk = psf[2 + h % 2], "psf%d" % (2 + h % 2)
                        for dk in range(2):
                            pe(lambda e, pu=pu, sub=sub, h=h, dk=dk: e.matmul(pu[:, dk * 256:(dk + 1) * 256], lhsT=k_tm[:, sub, h * 256 + dk * 128:h * 256 + (dk + 1) * 128],
                                                                              rhs=vp[:, sub, h, :], start=True, stop=True),
                               [rk(16 + sub * 2 + h // 2)] + VP, [puk])
                            pe(lambda e, sub=sub, h=h, dk=dk: e.matmul(psf[4][:, 136 + h * 2 + dk:137 + h * 2 + dk], lhsT=k_tm[:, sub, h * 256 + dk * 128:h * 256 + (dk + 1) * 128],
                                                                       rhs=wb16[:, sub, h:h + 1], start=True, stop=True),
                               [rk(16 + sub * 2 + h // 2), "wb16"], ["psf4"])
                        dve(lambda e, pu=pu, h=h, sub=sub: e.scalar_tensor_tensor(C32[:, h, :, :].rearrange("p a b -> p (a b)"), C32[:, h, :, :].rearrange("p a b -> p (a b)"),
                                                                                  dcol[:, sub, h:h + 1], pu[:, :], ALU.mult, ALU.add),
                            [puk, "dcol", "C32_%d" % h], ["C32_%d" % h])
                    dve(lambda e, sub=sub: e.tensor_tensor(n32[:], n32[:], dcol[:, sub, :].rearrange("p (h a) -> p h a", a=1).to_broadcast([128, 4, 2]), ALU.mult),
                        ["n32", "dcol", "nb"], ["n32"])
                    dve(lambda e: e.tensor_tensor(n32[:], n32[:], psf[4][:, 136:144].rearrange("p (h a) -> p h a", a=2), ALU.add), ["n32", "psf4"], ["n32"])
                    for h in range(4):
                        pn, pnk = psf[h // 2][:, (h % 2) * 256:(h % 2 + 1) * 256], "psf%d" % (h // 2)
                        dve(lambda e, pn=pn, h=h: e.bn_stats(st6[:, h, :], pn), [pnk], ["st6"])
                    for h in range(4):
                        dve(lambda e, h=h: e.bn_aggr(mv[:, h, :], st6[:, h, :]), ["st6"], ["mv"])
                    dve(lambda e: e.tensor_copy(dd, psf[4][:, 128:132]), ["psf4"], ["dd"])
                    dve(lambda e: e.scalar_tensor_tensor(mx, dd, -1.0, dd, ALU.mult, ALU.max), ["dd"], ["mx"])
                    dve(lambda e, sub=sub: e.tensor_tensor(mx, mx, e2c[:, sub, :], ALU.max), ["mx", "e2c"], ["mx"])
                    dve(lambda e: e.tensor_tensor(mx, mx, mx, ALU.mult), ["mx"], ["mx"])
                    dve(lambda e: e.scalar_tensor_tensor(rr, mx, EPS, mv[:, :, 1], ALU.mult, ALU.add), ["mx", "mv"], ["rr"])
                    act(lambda e: e.activation(rr, rr, AF.Ln), ["rr"], ["rr"])
                    act(lambda e: e.activation(rr, rr, AF.Exp, scale=-0.5), ["rr"], ["rr"])
                    for h in range(4):
                        pn, pnk = psf[h // 2][:, (h % 2) * 256:(h % 2 + 1) * 256], "psf%d" % (h // 2)
                        dve(lambda e, pn=pn, sub=sub, h=h: e.tensor_scalar(hn_tm[:, sub, h * 256:(h + 1) * 256], pn, mv[:, h, 0:1], rr[:, h:h + 1], ALU.subtract, ALU.mult),
                            [pnk, "mv", "rr"], XM)
                for fc in range(8):
                    pb = psb[fc % 2]
                    for sub in range(4):
                        pe(lambda e, fc=fc, sub=sub, pb=pb: e.transpose(pb[:, sub * 128:(sub + 1) * 128], hn_tm[:, sub, fc * 128:(fc + 1) * 128], ident[:]),
                           XM, ["psb%d" % (fc % 2)])
                    hb, hk = bfbuf()
                    act(lambda e, fc=fc, pb=pb, hb=hb: e.activation(hb, pb[:, 0:512], AF.Identity, scale=cc(C_HG, fc)), ["psb%d" % (fc % 2)], [hk])
                    dve(lambda e, fc=fc, hb=hb: e.tensor_tensor(hb, hb, som[:, fc, :], ALU.mult), [hk, "som%d" % fc], [hk])
                    dve(lambda e, fc=fc, hb=hb: e.scalar_tensor_tensor(aoutT[:, fc, :], xcT[:, fc, :], cc(C_SK, fc), hb, ALU.mult, ALU.add),
                        [hk, "xcT%d" % fc], ["xcT%d" % fc])
                if debug == "aout":
                    dump_T(aoutT, 8, ["xcT%d" % c for c in range(8)], stg)
                US = ["usT%d" % c for c in range(4)]
                def s5_half(half, stg=stg):
                    for gl_ in range(8):
                        gp = half * 8 + gl_
                        chunk, win = gp // 4, gp % 4
                        cl = gl_ // 4
                        for ri in range(2):
                            for j in range(8):
                                pe(lambda e, ri=ri, j=j, cl=cl, chunk=chunk, win=win: e.matmul(
                                    psf[win][:, (cl * 2 + ri) * 64:(cl * 2 + ri + 1) * 64], lhsT=WZt[32 * win:32 * win + 32, chunk, ri, j, :],
                                    rhs=usT[32 * win:32 * win + 32, chunk, :].rearrange("p (b j) -> p b j", j=8)[:, :, j],
                                    start=(j == 0), stop=(j == 7), tile_position=(32 * win, 0)), US, ["psf%d" % win])
                    gs = slice(half * 8, half * 8 + 8)
                    EcH = Ec[:, gs, :].rearrange("p g k -> p (g k)")
                    EsH = Es[:, gs, :].rearrange("p g k -> p (g k)")
                    RtH = Rtab[:, gs, :].rearrange("p g k -> p (g k)")
                    (bre, kbre), (bim, kbim), (ore, kore), (oim, koim), (tt, ktt) = f32buf(), f32buf(), f32buf(), f32buf(), f32buf()
                    w4 = lambda a, w: a.rearrange("p (c w k) -> p c w k", c=2, w=4)[:, :, w, :]
                    for w in range(4):
                        Zw = psf[w][:, 0:256].rearrange("p (c r k) -> p c r k", c=2, r=2)
                        Zre_w, Zim_w = Zw[:, :, 0, :], Zw[:, :, 1, :]
                        pk_ = "psf%d" % w
                        dve(lambda e, w=w, Zre_w=Zre_w: e.tensor_tensor(w4(bre, w), Zre_w, w4(EcH, w), ALU.mult), [pk_], [kbre])
                        dve(lambda e, w=w, Zim_w=Zim_w: e.tensor_tensor(w4(tt, w), Zim_w, w4(EsH, w), ALU.mult), [pk_], [ktt])
                        dve(lambda e, w=w, Zim_w=Zim_w: e.tensor_tensor(w4(bim, w), Zim_w, w4(EcH, w), ALU.mult), [pk_], [kbim])
                        dve(lambda e, w=w, Zre_w=Zre_w: e.tensor_tensor(w4(ore, w), Zre_w, w4(EsH, w), ALU.mult), [pk_], [kore])
                    dve(lambda e: e.tensor_tensor(bre, bre, tt, ALU.add), [kbre, ktt], [kbre])
                    dve(lambda e: e.tensor_tensor(bim, bim, ore, ALU.subtract), [kbim, kore], [kbim])
                    c0 = small[:, 128:136]
                    for (bb, SS, sk, bk) in ((bre, Sre, "Sre", kbre), (bim, Sim, "Sim", kbim)):
                        dve(lambda e, SS=SS: e.tensor_tensor(c0.rearrange("p (g a) -> p g a", a=1), SS[:, gs, 0:1], Rl[:, gs].rearrange("p (g a) -> p g a", a=1), ALU.mult),
                            [sk], ["c0"])
                        dve(lambda e, bb=bb: e.tensor_tensor(bb.rearrange("p (g k) -> p g k", g=8)[:, :, 0:1], bb.rearrange("p (g k) -> p g k", g=8)[:, :, 0:1],
                                                            c0.rearrange("p (g a) -> p g a", a=1), ALU.add), ["c0", bk], [bk])
                    dve(lambda e: e.tensor_tensor_scan(ore, RtH, bre, 0.0, ALU.mult, ALU.add), [kbre, kbim, kore], [kore])
                    dve(lambda e: e.tensor_tensor_scan(oim, RtH, bim, 0.0, ALU.mult, ALU.add), [kbim], [koim])
                    v3 = lambda a: a.rearrange("p (g k) -> p g k", g=8)
                    dve(lambda e: e.tensor_tensor(tt, oim, EsH, ALU.mult), [koim], [ktt])
                    dve(lambda e: e.tensor_tensor(bre, ore, EcH, ALU.mult), [kore], [kbre])
                    dve(lambda e: e.tensor_tensor(Sre[:, gs, 1:65], v3(bre), v3(tt), ALU.subtract), [kbre, ktt, "Sbre"], ["Sre"])
                    dve(lambda e: e.tensor_tensor(tt, ore, EsH, ALU.mult), [kore], [ktt])
                    dve(lambda e: e.tensor_tensor(bim, oim, EcH, ALU.mult), [koim], [kbim])
                    dve(lambda e: e.tensor_tensor(Sim[:, gs, 1:65], v3(bim), v3(tt), ALU.add), [kbim, ktt, "Sbim"], ["Sim"])
                    pool(lambda e: e.tensor_copy(Sbre[:, gs, :], Sre[:, gs, 0:64]), ["Sre"], ["Sbre"])
                    pool(lambda e: e.tensor_copy(Sbim[:, gs, :], Sim[:, gs, 0:64]), ["Sim"], ["Sbim"])
                    pool(lambda e: e.tensor_copy(Sre[:, gs, 0:1], Sre[:, gs, 64:65]), ["Sre", "Sbre"], ["Sre"])
                    pool(lambda e: e.tensor_copy(Sim[:, gs, 0:1], Sim[:, gs, 64:65]), ["Sim", "Sbim"], ["Sim"])
                    for cl in range(2):
                        chunk = half * 2 + cl
                        Y, yk = psf[4 + cl], "psf%d" % (4 + cl)
                        Y3 = Y[:, :].rearrange("p (b j) -> p b j", j=8)
                        U3 = usT[:, chunk, :].rearrange("p (b j) -> p b j", j=8)
                        for tau in range(8):
                            pe(lambda e, tau=tau, chunk=chunk, Y3=Y3, U3=U3: e.matmul(Y3[:, :, tau:8], lhsT=Kt[:, chunk, tau, :], rhs=U3[:, :, 0:8 - tau],
                                                                                      start=(tau == 0), stop=False), US, [yk])
                        for win in range(4):
                            gp = chunk * 4 + win
                            for j in range(8):
                                for ri in range(2):
                                    last = (ri == 1)
                                    SB = (Sbre, Sbim)[ri]
                                    pe(lambda e, win=win, gp=gp, j=j, ri=ri, SB=SB, Y3=Y3, last=last: e.matmul(
                                        Y3[32 * win:32 * win + 32, :, j], lhsT=WI[:, gp, ri, j, :], rhs=SB[:, gp, :],
                                        start=False, stop=last, tile_position=(0, 32 * win)), ["Sbre", "Sbim"], [yk])
                        (ysb, yk2), (z2, zk2) = f32buf(), f32buf()
                        sgg, sgk = bfbuf()
                        act(lambda e, Y=Y, ysb=ysb: e.activation(ysb, Y[:, :], AF.Identity), [yk], [yk2])
                        act(lambda e, Y=Y, z2=z2: e.activation(z2, Y[:, :], AF.Square), [yk], [zk2])
                        pool(lambda e, z2=z2: e.tensor_scalar(z2, z2, 0.044715, 1.0, ALU.mult, ALU.add), [zk2], [zk2])
                        pool(lambda e, z2=z2, ysb=ysb: e.tensor_tensor(z2, z2, ysb, ALU.mult), [zk2, yk2], [zk2])
                        sig(sgg, z2, [zk2], [sgk], scale=2.0 * C1G)
                        dve(lambda e, chunk=chunk, ysb=ysb, sgg=sgg: e.tensor_tensor(glT[:, chunk, :], ysb, sgg, ALU.mult), [yk2, sgk], ["glT%d" % chunk])
                for half in range(2):
                    s5_half(half)
                if debug == "gl":
                    dump_T(glT, 4, ["glT%d" % c for c in range(4)], stg)
                slot, skey = wnext()
                for jc in range(4):
                    pi, ps, pk = nextps()
                    for kt in range(4):
                        pe(lambda e, ps=ps, jc=jc, kt=kt, slot=slot: e.matmul(ps[:, :], lhsT=slot[:, (jc * 4 + kt) * 128:(jc * 4 + kt + 1) * 128], rhs=glT[:, kt, :],
                                                                             start=(kt == 0), stop=(kt == 3)), [skey, "glT%d" % kt], [pk])
                    sgg, sgk = bfbuf()
                    sig(sgg, ps[:, :], [pk], [sgk], bias=ch(C_BGLU, jc))
                    dve(lambda e, jc=jc, sgg=sgg: e.tensor_tensor(boutT[:, jc, :], glT[:, jc, :], sgg, ALU.mult), [sgk, "glT%d" % jc], ["usT%d" % jc])
                if debug == "bout":
                    dump_T(boutT, 4, US, stg)
                for mc in range(20, 36):
                    if mc % 2 == 0:
                        slot, skey = wnext()
                    ps, pk = inproj_chunk(mc, slot, skey)
                    sig(qT[:, mc - 20, :] if mc < 28 else kT[:, mc - 28, :], ps[:, :], [pk], [rk(mc - 20)], bias=ch(C_BIN, mc))
                for mc in range(8):
                    if mc % 2 == 0:
                        slot, skey = wnext()
                    pi, ps, pk = nextps()
                    for kt in range(8):
                        pe(lambda e, ps=ps, mc=mc, kt=kt, slot=slot: e.matmul(ps[:, :], lhsT=slot[:, ((mc % 2) * 8 + kt) * 128:((mc % 2) * 8 + kt + 1) * 128], rhs=aoutT[:, kt, :],
                                                                             start=(kt == 0), stop=(kt == 7)), [skey, "xcT%d" % kt], [pk])
                    dve(lambda e, ps=ps, mc=mc: e.tensor_tensor(mT[:, mc, :], ps[:, :], sga[:, mc, :], ALU.mult), [pk, rk(mc)], VP)
                for mc in range(8):
                    if mc % 4 == 0:
                        slot, skey = wnext()
                    pi, ps, pk = nextps()
                    for kt in range(4):
                        pe(lambda e, ps=ps, mc=mc, kt=kt, slot=slot: e.matmul(ps[:, :], lhsT=slot[:, ((mc % 4) * 4 + kt) * 128:((mc % 4) * 4 + kt + 1) * 128], rhs=boutT[:, kt, :],
                                                                             start=(kt == 0), stop=(kt == 3)), [skey, "usT%d" % kt], [pk])
                    tb_, tk_ = bfbuf()
                    dve(lambda e, ps=ps, mc=mc, tb_=tb_: e.tensor_tensor(tb_, ps[:, :], sgb[:, mc, :], ALU.mult), [pk, rk(8 + mc)], [tk_])
                    pool(lambda e, mc=mc, tb_=tb_: e.tensor_tensor(mT[:, mc, :], mT[:, mc, :], tb_, ALU.add), [tk_] + VP, VP)
                if debug == "merged":
                    dump_T(mT, 8, VP, stg)
                for nh in range(2):
                    pss = [nextps() for _ in range(4)]
                    for kq in range(2):
                        slot, skey = wnext()
                        for k4 in range(4):
                            kt = kq * 4 + k4
                            for sub in range(4):
                                pi, ps, pk = pss[sub]
                                pe(lambda e, ps=ps, kt=kt, k4=k4, sub=sub, slot=slot: e.matmul(ps[:, :], lhsT=mT[:, kt, sub * 128:(sub + 1) * 128], rhs=slot[:, k4 * 512:(k4 + 1) * 512],
                                                                                              start=(kt == 0), stop=(kt == 7)), [skey] + VP, [pk])
                    for sub in range(4):
                        pi, ps, pk = pss[sub]
                        dve(lambda e, ps=ps, sub=sub, nh=nh: e.tensor_tensor(x_sb[:, sub, nh * 512:(nh + 1) * 512], x_sb[:, sub, nh * 512:(nh + 1) * 512], ps[:, :], ALU.add),
                            [pk, "x_sb%d" % sub], ["x_sb%d" % sub])
                if debug == "x1":
                    for sub in range(4):
                        S.dma("pool", y_d[t0 + sub * 128:t0 + (sub + 1) * 128, :], x_sb[:, sub, :], reads=["x_sb%d" % sub], writes=["y"])
                norm_T(C_G2)
                fcw3 = ccol[:, C_FCW:C_FCW + 132].rearrange("p (m k) -> p m k", k=3)
                bt = small[:, 192:236]
                dve(lambda e: e.tensor_tensor(bt, fcar[:, :, 1], fcw3[:, :, 1], ALU.mult), ["fcar"], ["bt"])
                dve(lambda e: e.tensor_tensor(bnd[:, :, 1], fcar[:, :, 1], fcw3[:, :, 0], ALU.mult), ["fcar"], ["bnd"])
                dve(lambda e: e.tensor_tensor(bnd[:, :, 0], fcar[:, :, 0], fcw3[:, :, 0], ALU.mult), ["fcar", "bnd"], ["bnd"])
                dve(lambda e: e.tensor_tensor(bnd[:, :, 0], bnd[:, :, 0], bt, ALU.add), ["bt", "bnd"], ["bnd"])
                for i in range(22):
                    slot, skey = wnext()
                    accs = (f32buf(), f32buf())
                    pss2 = []
                    for vg in range(2):
                        pi, ps, pk = nextps6()
                        pss2.append((ps, pk))
                        for kt in range(8):
                            pe(lambda e, ps=ps, vg=vg, kt=kt, slot=slot: e.matmul(ps[:, :], lhsT=slot[:, (vg * 8 + kt) * 128:(vg * 8 + kt + 1) * 128], rhs=hT[:, kt, :],
                                                                                 start=(kt == 0), stop=(kt == 7)), [skey, "hT%d" % kt], [pk])
                    for vg in range(2):
                        mc = i + 22 * vg
                        (acc, akey), (ps, pk) = accs[vg], pss2[vg]
                        act(lambda e, ps=ps, mc=mc, acc=acc: e.activation(acc, ps[:, :], AF.Identity, bias=cc(C_FCB, mc), scale=cc(C_FCW, mc * 3 + 2)),
                            [pk], [akey])
                    for tap, sh in ((1, 1), (0, 2)):
                        for vg in range(2):
                            mc = i + 22 * vg
                            (acc, akey), (ps, pk) = accs[vg], pss2[vg]
                            dve(lambda e, ps=ps, mc=mc, acc=acc, tap=tap, sh=sh: e.scalar_tensor_tensor(acc[:, sh:512], ps[:, 0:512 - sh], cc(C_FCW, mc * 3 + tap),
                                                                                                    acc[:, sh:512], ALU.mult, ALU.add),
                                [pk, akey], [akey])
                    for vg in range(2):
                        mc = i + 22 * vg
                        (acc, akey), (ps, pk) = accs[vg], pss2[vg]
                        dve(lambda e, mc=mc, acc=acc: e.tensor_tensor(acc[:, 0:2], acc[:, 0:2], bnd[:, mc, :], ALU.add), ["bnd", akey], [akey])
                        act(lambda e, ps=ps, mc=mc: e.copy(fcar[:, mc, 0:2], ps[:, 510:512]), [pk, akey, "bnd"], ["fcar"])
                    (av, avk), (ag, agk) = accs
                    sgg, sgk = bfbuf()
                    sig(sgg, ag, [agk], [sgk])
                    pool(lambda e, ag=ag, sgg=sgg: e.tensor_tensor(ag, ag, sgg, ALU.mult), [agk, sgk], [agk])
                    pool(lambda e, i=i, ag=ag, av=av: e.tensor_tensor(gT[:, i, :], ag, av, ALU.mult), [avk, agk], [rk(i)])
                for nh in range(2):
                    pss = [nextps() for _ in range(4)]
                    for kq in range(6):
                        slot, skey = wnext()
                        for k4 in range(4):
                            kt = kq * 4 + k4
                            if kt >= 22:
                                continue
                            for sub in range(4):
                                pi, ps, pk = pss[sub]
                                pe(lambda e, ps=ps, kt=kt, k4=k4, sub=sub, slot=slot: e.matmul(ps[:, :], lhsT=gT[:, kt, sub * 128:(sub + 1) * 128], rhs=slot[:, k4 * 512:(k4 + 1) * 512],
                                                                                              start=(kt == 0), stop=(kt == 21)), [skey, rk(kt)], [pk])
                    for sub in range(4):
                        pi, ps, pk = pss[sub]
                        dve(lambda e, ps=ps, sub=sub, nh=nh: e.tensor_tensor(x_sb[:, sub, nh * 512:(nh + 1) * 512], x_sb[:, sub, nh * 512:(nh + 1) * 512], ps[:, :], ALU.add),
                            [pk, "x_sb%d" % sub], ["x_sb%d" % sub])
                if debug is None:
                    rs = rms_rstd()
                    for sub in range(4):
                        for nh in range(2):
                            ob, okk = f32buf()
                            dve(lambda e, sub=sub, ob=ob, nh=nh: e.scalar_tensor_tensor(ob, x_sb[:, sub, nh * 512:(nh + 1) * 512], rs[:, sub:sub + 1],
                                                                                       crow[:, nh * 512:(nh + 1) * 512], ALU.mult, ALU.mult),
                                ["x_sb%d" % sub, "rs%d" % sub], [okk])
                            S.dma("pool", y_d[t0 + sub * 128:t0 + (sub + 1) * 128, nh * 512:(nh + 1) * 512], ob, reads=[okk], writes=["y"])
            S.emit()
    return nc


_CACHE = {}


def kernel(**inputs):
    prep = _prep(inputs)
    x = np.ascontiguousarray(np.asarray(inputs["x"], dtype=np.float32))
    if "nc" not in _CACHE:
        _CACHE["nc"] = build_nc()
    nc = _CACHE["nc"]
    in_maps = []
    for c in range(8):
        m = {"x": x[c]}
        m.update(prep)
        in_maps.append(m)
    res = run_bass_kernel_spmd(nc, in_maps, core_ids=list(range(8)))
    return np.stack([np.asarray(r["y"], dtype=np.float32) for r in res.results], axis=0)
```

```python
import contextlib
import math
import numpy as np
import concourse.bass as bass
import concourse.mybir as mybir
from concourse.bass_utils import run_bass_kernel_spmd

F32 = mybir.dt.float32
BF16 = mybir.dt.bfloat16
ALU = mybir.AluOpType
AF = mybir.ActivationFunctionType

SEQ = 4096
DM = 1024
TT = 512
NST = SEQ // TT
NS = 67
RING = 4
EPS = 1e-6


class _Op:
    __slots__ = ("eng", "fn", "is_dma", "deps", "inc", "sem", "semval")

    def __init__(self, eng, fn, is_dma):
        self.eng = eng
        self.fn = fn
        self.is_dma = is_dma
        self.deps = []
        self.inc = False
        self.sem = None
        self.semval = 0


class Sched:
    ENGS = ("pe", "act", "dve", "pool", "sp")
    N_DMA_SEMS = 24

    _uid = [0]

    @staticmethod
    def make_sems(nc, st):
        csem = {e: st.enter_context(nc.semaphore("cs_" + e)) for e in ("pe", "act", "dve", "pool")}
        dsem = {e: [st.enter_context(nc.semaphore("ds_%s%d" % (e, i))) for i in range(Sched.N_DMA_SEMS)]
                for e in ("sp", "pool")}
        return dict(csem=csem, dsem=dsem, ccount={e: 0 for e in csem}, dcount={e: [0] * Sched.N_DMA_SEMS for e in dsem})

    def __init__(self, nc, sems=None):
        self.nc = nc
        self.sems = sems
        Sched._uid[0] += 1
        self.uid = Sched._uid[0]
        self.ops = {e: [] for e in self.ENGS}
        self.last_w = {}
        self.readers = {}

    def _add(self, eng, fn, is_dma, reads, writes):
        op = _Op(eng, fn, is_dma)
        deps = []
        raw = set()
        for k in reads:
            w = self.last_w.get(k)
            if w is not None:
                deps.append(w)
                raw.add(id(w))
        for k in writes:
            w = self.last_w.get(k)
            if w is not None:
                deps.append(w)
            deps.extend(self.readers.get(k, ()))
        for k in writes:
            self.last_w[k] = op
            self.readers[k] = []
        for k in reads:
            if k not in writes:
                self.readers.setdefault(k, []).append(op)
        seen = set()
        for d in deps:
            if d is op or id(d) in seen:
                continue
            seen.add(id(d))
            if d.eng == eng and not d.is_dma and not is_dma:
                if eng == "pe" or id(d) not in raw:
                    continue
            op.deps.append(d)
            d.inc = True
        self.ops[eng].append(op)
        return op

    def op(self, eng, fn, reads=(), writes=()):
        return self._add(eng, fn, False, tuple(reads), tuple(writes))

    def dma(self, eng, out, in_, reads=(), writes=()):
        return self._add(eng, lambda e: e.dma_start(out=out, in_=in_), True, tuple(reads), tuple(writes))

    def emit(self):
        nc = self.nc
        with contextlib.ExitStack() as st:
            pool_ = self.sems
            csem, dsem, ccount, dcount = pool_["csem"], pool_["dsem"], pool_["ccount"], pool_["dcount"]
            for e in self.ENGS:
                nd = 0
                for op in self.ops[e]:
                    if op.is_dma:
                        i = nd % self.N_DMA_SEMS
                        nd += 1
                        dcount[e][i] += 16
                        op.sem = dsem[e][i]
                        op.semval = dcount[e][i]
                    elif op.inc:
                        ccount[e] += 1
                        op.sem = csem[e]
                        op.semval = ccount[e]
            block = st.enter_context(nc.Block())

            def run(ename, eng):
                waited = {}
                last = {}
                for op in self.ops[ename]:
                    need = {}
                    for d in op.deps:
                        key = id(d.sem)
                        if waited.get(key, 0) >= d.semval:
                            continue
                        if key not in need or need[key][1] < d.semval:
                            need[key] = (d.sem, d.semval)
                    for key, (sem, val) in need.items():
                        eng.wait_ge(sem, val)
                        waited[key] = val
                    ins = op.fn(eng)
                    if op.is_dma:
                        ins.then_inc(op.sem, 16)
                        last[id(op.sem)] = (op.sem, op.semval)
                    elif op.inc:
                        ins.then_inc(op.sem, 1)
                for key, (sem, val) in last.items():
                    if waited.get(key, 0) < val:
                        eng.wait_ge(sem, val)

            block.tensor(lambda e: run("pe", e))
            block.scalar(lambda e: run("act", e))
            block.vector(lambda e: run("dve", e))
            block.gpsimd(lambda e: run("pool", e))
            block.sync(lambda e: run("sp", e))


C_BIN, C_G1, C_G2, C_MCW, C_MCB, C_HG, C_SK, C_FCW, C_FCB, C_BGLU, C_SD = 0, 36, 44, 52, 84, 92, 100, 108, 240, 284, 288
NCOL = 292
IN_PERM = np.concatenate([np.arange(0, 2048), np.arange(2056, 4616)])
SL_A, SL_B, SL_KT, SL_VT, SL_GLU, SL_BRA, SL_BRB, SL_WO, SL_UP, SL_DN = 0, 18, 20, 21, 22, 23, 27, 29, 33, 55
ORDER = (list(range(0, 10)) + [18, 19, 20, 21, 22] + list(range(10, 18)) + list(range(23, 67)))


def _prep(inp):
    f = lambda a: np.ascontiguousarray(np.asarray(a, dtype=np.float32))
    w_in = f(inp["w_in"][0])
    w_in_p = w_in[:, IN_PERM]
    b_in = f(inp["b_in"][0])
    slabs = np.zeros((NS, 128, 2048), np.float32)

    def put(s, col, blk):
        slabs[s, :, col:col + blk.shape[1]] = blk

    for mc in range(36):
        for kt in range(8):
            put(SL_A + mc // 2, ((mc % 2) * 8 + kt) * 128, w_in_p[kt * 128:(kt + 1) * 128, mc * 128:(mc + 1) * 128])
    wq, wk, wv = f(inp["m_wq"][0]), f(inp["m_wk"][0]), f(inp["m_wv"][0])
    for h in range(4):
        for qk, W in enumerate((wq, wk)):
            for ec in range(2):
                for kt in range(2):
                    col = ((((h % 2) * 2 + qk) * 2 + ec) * 2 + kt) * 128
                    put(SL_B + h // 2, col, W[h, kt * 128:(kt + 1) * 128, ec * 128:(ec + 1) * 128])
        for kt in range(2):
            put(SL_KT, (h * 2 + kt) * 256, wk[h, kt * 128:(kt + 1) * 128, :])
            put(SL_VT, (h * 2 + kt) * 256, wv[h, kt * 128:(kt + 1) * 128, :])
    wglu = f(inp["s_w_glu"][0])
    for jc in range(4):
        for kt in range(4):
            put(SL_GLU, (jc * 4 + kt) * 128, wglu[kt * 128:(kt + 1) * 128, jc * 128:(jc + 1) * 128])
    wa, wb, wo = f(inp["w_branch_a"][0]), f(inp["w_branch_b"][0]), f(inp["w_out"][0])
    for mc in range(8):
        for kt in range(8):
            put(SL_BRA + mc // 2, ((mc % 2) * 8 + kt) * 128, wa[kt * 128:(kt + 1) * 128, mc * 128:(mc + 1) * 128])
        for kt in range(4):
            put(SL_BRB + mc // 4, ((mc % 4) * 4 + kt) * 128, wb[kt * 128:(kt + 1) * 128, mc * 128:(mc + 1) * 128])
    for nh in range(2):
        for kt in range(8):
            put(SL_WO + nh * 2 + kt // 4, (kt % 4) * 512, wo[kt * 128:(kt + 1) * 128, nh * 512:(nh + 1) * 512])
    wup, wdn = f(inp["w_up"][0]), f(inp["w_down"][0])
    for i in range(22):
        for vg in range(2):
            mc = i + 22 * vg
            for kt in range(8):
                put(SL_UP + i, (vg * 8 + kt) * 128, wup[kt * 128:(kt + 1) * 128, mc * 128:(mc + 1) * 128])
    for nh in range(2):
        for kt in range(22):
            put(SL_DN + nh * 6 + kt // 4, (kt % 4) * 512, wdn[kt * 128:(kt + 1) * 128, nh * 512:(nh + 1) * 512])

    ccol = np.zeros((128, NCOL), np.float32)
    col = lambda v, n: f(v).reshape(n, 128).T
    ccol[:, C_BIN:C_BIN + 36] = col(b_in[IN_PERM], 36)
    ccol[:, C_G1:C_G1 + 8] = col(inp["mix_norm_g"][0], 8)
    ccol[:, C_G2:C_G2 + 8] = col(inp["ffn_norm_g"][0], 8)
    mcw = f(inp["m_conv_w"][0])
    ccol[:, C_MCW:C_MCW + 32] = mcw.T.reshape(8, 128, 4).transpose(1, 0, 2).reshape(128, 32)
    ccol[:, C_MCB:C_MCB + 8] = col(inp["m_conv_b"][0], 8)
    ccol[:, C_HG:C_HG + 8] = col(inp["m_head_g"][0], 8)
    ccol[:, C_SK:C_SK + 8] = col(inp["m_skip"][0], 8)
    fcw = f(inp["ffn_conv_w"][0])
    ccol[:, C_FCW:C_FCW + 132] = fcw.T.reshape(44, 128, 3).transpose(1, 0, 2).reshape(128, 132)
    ccol[:, C_FCB:C_FCB + 44] = col(inp["ffn_conv_b"][0], 44)
    ccol[:, C_BGLU:C_BGLU + 4] = col(inp["s_b_glu"][0], 4)
    ccol[:, C_SD:C_SD + 4] = col(f(inp["s_d"][0]).reshape(-1), 4)
    crow = np.zeros((128, 1032), np.float32)
    crow[:, 0:1024] = f(inp["final_norm_g"])[None, :]
    crow[:, 1024:1032] = b_in[2048:2056][None, :]
    wg = np.ascontiguousarray(w_in[:, 2048:2056].reshape(8, 128, 8).transpose(1, 0, 2).reshape(128, 64))

    are, aim, ldt = f(inp["s_a_re"][0]), f(inp["s_a_im"][0]), f(inp["s_log_dt"][0])
    bre, bim = f(inp["s_b_re"][0]), f(inp["s_b_im"][0])
    cre, cim = f(inp["s_c_re"][0]), f(inp["s_c_im"][0])
    toL = lambda a: a.reshape(16, 2, 64).transpose(1, 2, 0).reshape(128, 16)
    rep = lambda a: np.broadcast_to(toL(a)[:, :, None], (128, 16, 32)).reshape(128, 512)
    s5X = np.stack([rep(are), rep(aim), rep(np.broadcast_to(ldt[:, None], (32, 64)))], axis=1)

    def toT(a):
        t = a.reshape(4, 4, 2, 64)
        t = np.broadcast_to(t[:, :, None, None, :, :], (4, 4, 2, 16, 2, 64))
        return t.transpose(1, 2, 3, 0, 4, 5).reshape(128, 4, 128)

    s5T = np.stack([toT(are), toT(aim), toT(np.broadcast_to(ldt[:, None], (32, 64)))], axis=1)

    def bT(b):
        t = b.reshape(4, 4, 2, 64, 16)
        o = np.zeros((4, 2, 16, 4, 2, 64), np.float32)
        for g2 in range(2):
            o[:, g2, :, :, g2, :] = t[:, :, g2].transpose(1, 3, 0, 2)
        return o.reshape(128, 4, 128)

    def bX(b):
        t = b.reshape(16, 2, 64, 16)
        o = np.zeros((2, 64, 16, 2, 16), np.float32)
        for g2 in range(2):
            o[g2, :, :, g2, :] = t[:, g2].transpose(1, 0, 2)
        return o.reshape(128, 16, 32)

    BtD = np.stack([bT(bre), bT(bim)], axis=1)
    BxD = np.stack([bX(bre), bX(bim)], axis=1)
    CxD = np.stack([bX(cre.transpose(0, 2, 1)), bX(cim.transpose(0, 2, 1))], axis=1)
    return dict(wall=slabs, ccol=ccol, crow=crow, wg=wg,
                s5X=np.ascontiguousarray(s5X), s5T=np.ascontiguousarray(s5T.reshape(128, 3, 512)),
                BtD=np.ascontiguousarray(BtD.reshape(128, 2, 512)), BxD=np.ascontiguousarray(BxD.reshape(128, 2, 512)),
                CxD=np.ascontiguousarray(CxD.reshape(128, 2, 512)))


C1G = math.sqrt(2.0 / math.pi)


def _cmul(dve, o_re, o_im, a_re, a_im, b_re, b_im, t1, t2):
    dve(lambda e: e.tensor_tensor(t1, a_re, b_re, ALU.mult))
    dve(lambda e: e.tensor_tensor(t2, a_im, b_im, ALU.mult))
    dve(lambda e: e.tensor_tensor(t1, t1, t2, ALU.subtract))
    dve(lambda e: e.tensor_tensor(t2, a_re, b_im, ALU.mult))
    dve(lambda e: e.tensor_tensor(o_im, a_im, b_re, ALU.mult))
    dve(lambda e: e.tensor_tensor(o_im, o_im, t2, ALU.add))
    dve(lambda e: e.tensor_copy(o_re, t1))


class _Rec:
    def __init__(self):
        self.ops = []

    def op(self, eng, fn, r=(), w=()):
        self.ops.append((eng, fn))


def _interleave(S, recs):
    n = max(len(r.ops) for r, _ in recs)
    for i in range(n):
        for r, key in recs:
            if i < len(r.ops):
                eng, fn = r.ops[i]
                S.op(eng, fn, [key], [key])


def _s5_params(S, par, W):
    k = ["s5"]
    dve = lambda fn: S.op("dve", fn, k, k)
    act = lambda fn: S.op("act", fn, k, k)
    are, aim, ldt = par[:, 0, :], par[:, 1, :], par[:, 2, :]
    dt, dre, dim, mag, c, s, t1, t2 = (W["w%d" % i][:] for i in range(8))
    act(lambda e: e.activation(dt, ldt, AF.Exp))
    dve(lambda e: e.tensor_tensor(dre, dt, are, ALU.mult))
    dve(lambda e: e.tensor_tensor(dim, dt, aim, ALU.mult))
    act(lambda e: e.activation(mag, dre, AF.Exp))
    act(lambda e: e.activation(W["R8"][:], dre, AF.Exp, scale=8.0))
    act(lambda e: e.activation(s, dim, AF.Sin, scale=1.0 / 16.0))
    act(lambda e: e.activation(c, dim, AF.Sin, bias=W["hpi"], scale=1.0 / 16.0))

    def square():
        dve(lambda e: e.tensor_tensor(t1, c, c, ALU.mult))
        dve(lambda e: e.tensor_tensor(t2, s, s, ALU.mult))
        dve(lambda e: e.scalar_tensor_tensor(s, c, 2.0, s, ALU.mult, ALU.mult))
        dve(lambda e: e.tensor_tensor(c, t1, t2, ALU.subtract))

    for _ in range(4):
        square()
    dve(lambda e: e.tensor_tensor(W["ab_re"][:], mag, c, ALU.mult))
    dve(lambda e: e.tensor_tensor(W["ab_im"][:], mag, s, ALU.mult))
    for _ in range(3):
        square()
    dve(lambda e: e.tensor_copy(W["c8"][:], c))
    dve(lambda e: e.tensor_copy(W["s8"][:], s))
    xr, den = dt, dre
    dve(lambda e: e.tensor_scalar_add(xr, W["ab_re"][:], -1.0))
    dve(lambda e: e.tensor_tensor(t1, are, are, ALU.mult))
    dve(lambda e: e.tensor_tensor(t2, aim, aim, ALU.mult))
    dve(lambda e: e.tensor_tensor(den, t1, t2, ALU.add))
    dve(lambda e: e.reciprocal(den, den))
    dve(lambda e: e.tensor_tensor(t1, xr, are, ALU.mult))
    dve(lambda e: e.tensor_tensor(t2, W["ab_im"][:], aim, ALU.mult))
    dve(lambda e: e.tensor_tensor(t1, t1, t2, ALU.add))
    dve(lambda e: e.tensor_tensor(W["r_re"][:], t1, den, ALU.mult))
    dve(lambda e: e.tensor_tensor(t1, W["ab_im"][:], are, ALU.mult))
    dve(lambda e: e.tensor_tensor(t2, xr, aim, ALU.mult))
    dve(lambda e: e.tensor_tensor(t1, t1, t2, ALU.subtract))
    dve(lambda e: e.tensor_tensor(W["r_im"][:], t1, den, ALU.mult))


def build_nc(debug=None, nst=NST):
    nc = bass.Bass("TRN2", target_bir_lowering=False)
    din = lambda n, s: nc.dram_tensor(n, s, F32, kind="ExternalInput").ap()
    x_d = din("x", [SEQ, DM])
    wall_d = din("wall", [NS, 128, 2048])
    ccol_d = din("ccol", [128, NCOL])
    crow_d = din("crow", [128, 1032])
    wg_d = din("wg", [128, 64])
    s5X_d = din("s5X", [128, 3, 512])
    s5T_d = din("s5T", [128, 3, 512])
    Bt_d = din("BtD", [128, 2, 512])
    Bx_d = din("BxD", [128, 2, 512])
    Cx_d = din("CxD", [128, 2, 512])
    y_d = nc.dram_tensor("y", [SEQ, DM], F32, kind="ExternalOutput").ap()
    wsl_d = nc.dram_tensor("wsl", [NS, 128, 2048], BF16, kind="Internal").ap()

    with contextlib.ExitStack() as st0:
        def T0(name, shape, dt=F32):
            return st0.enter_context(nc.sbuf_tensor("s_" + name, shape, dt))

        SEMS = Sched.make_sems(nc, st0)
        WZt = T0("WZt", [128, 4, 2, 8, 128], BF16)
        WI = T0("WI", [128, 16, 2, 8, 32], BF16)
        Kt = T0("Kt", [128, 4, 8, 128], BF16)
        Ec = T0("Ec", [128, 16, 64])
        Es = T0("Es", [128, 16, 64])
        Rtab = T0("Rtab", [128, 16, 64])
        Rl = T0("Rl", [128, 16])
        ccol = T0("ccol", [128, NCOL])
        chalf = T0("chalf", [128, NCOL])
        crow = T0("crow", [128, 1032])
        wgb = T0("wgb", [128, 64], BF16)
        ident = T0("ident", [128, 128], BF16)
        cmask = T0("cmask", [128, 128], BF16)
        cmask4 = T0("cmask4", [128, 512], BF16)
        LT = T0("LT", [128, 128])
        ONES = T0("ONES", [128, 128])
        small = T0("small", [128, 256])
        psf = [st0.enter_context(nc.psum_tensor("psf%d" % i, [128, 512], F32)) for i in range(6)]
        psb = [st0.enter_context(nc.psum_tensor("psb%d" % i, [128, 1024], BF16)) for i in range(2)]

        def cc(off, i=0):
            return ccol[:, off + i:off + i + 1]

        def ch(off, i=0):
            return chalf[:, off + i:off + i + 1]

        with contextlib.ExitStack() as st1:
            def T1(name, shape, dt=F32):
                return st1.enter_context(nc.sbuf_tensor("a_" + name, shape, dt))

            S = Sched(nc, SEMS)
            dve = lambda fn, r=(), w=(): S.op("dve", fn, r, w)
            act = lambda fn, r=(), w=(): S.op("act", fn, r, w)
            pool = lambda fn, r=(), w=(): S.op("pool", fn, r, w)
            pe = lambda fn, r=(), w=(): S.op("pe", fn, r, w)
            w32 = [T1("w32_0", [128, 64])]
            S.dma("sp", ccol[:], ccol_d, writes=["ccol"])
            S.dma("sp", crow[:], crow_d, writes=["crow"])
            S.dma("sp", w32[0][:, 0:64], wg_d, writes=["w32_0"])
            act(lambda e: e.copy(wgb[:], w32[0][:, 0:64]), ["w32_0"], ["wgb"])
            act(lambda e: e.mul(chalf[:], ccol[:], 0.5), ["ccol"], ["chalf"])
            pool(lambda e: e.memset(ident[:], 0.0), [], ["ident"])
            pool(lambda e: e.affine_select(out=ident[:], in_=ident[:], compare_op=ALU.not_equal, fill=1.0, base=0,
                                           pattern=[[-1, 128]], channel_multiplier=1), ["ident"], ["ident"])
            pool(lambda e: e.memset(cmask[:], 1.0), [], ["cmask"])
            pool(lambda e: e.affine_select(out=cmask[:], in_=cmask[:], compare_op=ALU.is_ge, fill=0.0, base=0,
                                           pattern=[[1, 128]], channel_multiplier=-1), ["cmask"], ["cmask"])
            pool(lambda e: e.memset(cmask4[:], 1.0), [], ["cmask4"])
            pool(lambda e: e.affine_select(out=cmask4[:, :].rearrange("p (h q) -> p h q", h=4), in_=cmask4[:, :].rearrange("p (h q) -> p h q", h=4),
                                           compare_op=ALU.is_ge, fill=0.0, base=0, pattern=[[0, 4], [1, 128]], channel_multiplier=-1), ["cmask4"], ["cmask4"])
            pool(lambda e: e.memset(LT[:], 1.0), [], ["LT"])
            pool(lambda e: e.affine_select(out=LT[:], in_=LT[:], compare_op=ALU.is_ge, fill=0.0, base=-1,
                                           pattern=[[-1, 128]], channel_multiplier=1), ["LT"], ["LT"])
            pool(lambda e: e.memset(ONES[:], 1.0), [], ["ONES"])
            pool(lambda e: e.memset(small[:], 0.0), [], ["small"])
            pool(lambda e: e.memset(small[:, 250:251], EPS), ["small"], ["small"])
            pool(lambda e: e.memset(small[:, 251:252], 1.0), ["small"], ["small"])
            pool(lambda e: e.memset(small[:, 252:253], math.pi / 2.0), ["small"], ["small"])
            k5 = ["s5"]
            d5 = lambda fn: S.op("dve", fn, k5, k5)
            a5 = lambda fn: S.op("act", fn, k5, k5)
            parT = T1("parT", [128, 3, 512])
            parX = T1("parX", [128, 3, 512])
            BtS = T1("BtS", [128, 2, 512])
            BxS = T1("BxS", [128, 2, 512])
            CxS = T1("CxS", [128, 2, 512])
            S.dma("sp", parT[:], s5T_d, writes=["ld0"])
            S.dma("sp", parX[:], s5X_d, writes=["ld1"])
            S.dma("sp", BtS[:], Bt_d, writes=["ld2"])
            S.dma("sp", BxS[:], Bx_d, writes=["ld3"])
            S.dma("sp", CxS[:], Cx_d, writes=["ld4"])
            S.op("dve", lambda e: e.memset(small[:, 253:254], 0.0), ["ld0", "ld1", "ld2", "ld3", "ld4", "small"], ["s5", "s5T", "s5X"])
            for s_ in range(NS):
                S.dma("pool", wsl_d[s_], wall_d[s_], reads=[], writes=["wsl%d" % s_])
            names = ["w%d" % i for i in range(8)] + ["ab_re", "ab_im", "r_re", "r_im", "c8", "s8", "R8"]
            WT = {n: T1("T_" + n, [128, 512]) for n in names}
            WX = {n: T1("X_" + n, [128, 512]) for n in names}
            WT["hpi"] = small[:, 252:253]
            WX["hpi"] = small[:, 252:253]
            recT, recX = _Rec(), _Rec()
            _s5_params(recT, parT, WT)
            _s5_params(recX, parX, WX)
            _interleave(S, [(recT, "s5T"), (recX, "s5X")])
            cur_re, cur_im, t1, t2 = T1("cur_re", [128, 512]), T1("cur_im", [128, 512]), T1("t1", [128, 512]), T1("t2", [128, 512])
            cuT_re, cuT_im, t1T, t2T = T1("cuT_re", [128, 512]), T1("cuT_im", [128, 512]), T1("t1T", [128, 512]), T1("t2T", [128, 512])
            recT, recX = _Rec(), _Rec()
            dT = lambda fn: recT.op("dve", fn)
            aT = lambda fn: recT.op("act", fn)
            dX = lambda fn: recX.op("dve", fn)
            aX = lambda fn: recX.op("act", fn)
            _cmul(dT, cuT_re[:], cuT_im[:], WT["r_re"][:], WT["r_im"][:], BtS[:, 0, :], BtS[:, 1, :], t1T[:], t2T[:])
            for kk in range(8):
                j = 7 - kk
                aT(lambda e, j=j: e.copy(WZt[:, :, 0, j, :], cuT_re[:, :].rearrange("p (c m) -> p c m", c=4)))
                aT(lambda e, j=j: e.copy(WZt[:, :, 1, j, :], cuT_im[:, :].rearrange("p (c m) -> p c m", c=4)))
                if kk < 7:
                    _cmul(dT, cuT_re[:], cuT_im[:], cuT_re[:], cuT_im[:], WT["ab_re"][:], WT["ab_im"][:], t1T[:], t2T[:])
            _cmul(dX, cur_re[:], cur_im[:], CxS[:, 0, :], CxS[:, 1, :], WX["ab_re"][:], WX["ab_im"][:], t1[:], t2[:])
            for j in range(8):
                aX(lambda e, j=j: e.copy(WI[:, :, 0, j, :], cur_re[:, :].rearrange("p (g m) -> p g m", g=16)))
                aX(lambda e, j=j: e.mul(WI[:, :, 1, j, :], cur_im[:, :].rearrange("p (g m) -> p g m", g=16), -1.0))
                if j < 7:
                    _cmul(dX, cur_re[:], cur_im[:], cur_re[:], cur_im[:], WX["ab_re"][:], WX["ab_im"][:], t1[:], t2[:])
            _interleave(S, [(recT, "s5T"), (recX, "s5X")])
            S.op("dve", lambda e: e.memset(small[:, 254:255], 0.0), ["s5T", "s5X", "s5"], k5)
            Cb_re = T1("Cb_re", [128, 512], BF16)
            nCb_im = T1("nCb_im", [128, 512], BF16)
            Xb_re = T1("Xb_re", [128, 512], BF16)
            Xb_im = T1("Xb_im", [128, 512], BF16)
            a5(lambda e: e.copy(Cb_re[:], CxS[:, 0, :]))
            a5(lambda e: e.mul(nCb_im[:], CxS[:, 1, :], -1.0))
            _cmul(d5, cur_re[:], cur_im[:], WX["r_re"][:], WX["r_im"][:], BxS[:, 0, :], BxS[:, 1, :], t1[:], t2[:])
            for tau in range(8):
                a5(lambda e: e.copy(Xb_re[:], cur_re[:]))
                a5(lambda e: e.copy(Xb_im[:], cur_im[:]))
                d5(lambda e: e.memset(psf[0][:, :], 0.0))
                for gp in range(16):
                    chunk, win = gp // 4, gp % 4
                    o = psf[0][32 * win:32 * win + 32, chunk * 128 + 32 * win:chunk * 128 + 32 * win + 32]
                    S.op("pe", lambda e, o=o, gp=gp, win=win: e.matmul(o, lhsT=Xb_re[:, gp * 32:(gp + 1) * 32], rhs=Cb_re[:, gp * 32:(gp + 1) * 32],
                                                                     start=True, stop=False, tile_position=(0, 32 * win)), k5, k5)
                    S.op("pe", lambda e, o=o, gp=gp, win=win: e.matmul(o, lhsT=Xb_im[:, gp * 32:(gp + 1) * 32], rhs=nCb_im[:, gp * 32:(gp + 1) * 32],
                                                                     start=False, stop=True, tile_position=(0, 32 * win)), k5, k5)
                if tau == 0:
                    for chunk in range(4):
                        S.op("dve", lambda e, chunk=chunk: e.scalar_tensor_tensor(Kt[:, chunk, 0, :], ident[:], cc(C_SD, chunk), psf[0][:, chunk * 128:(chunk + 1) * 128],
                                                                               ALU.mult, ALU.add), k5 + ["ident", "ccol"], k5)
                else:
                    d5(lambda e, tau=tau: e.tensor_copy(Kt[:, :, tau, :], psf[0][:, :].rearrange("p (c m) -> p c m", c=4)))
                if tau < 7:
                    _cmul(d5, cur_re[:], cur_im[:], cur_re[:], cur_im[:], WX["ab_re"][:], WX["ab_im"][:], t1[:], t2[:])
            c8 = WX["c8"][:, :].rearrange("p (g m) -> p g m", g=16)
            s8 = WX["s8"][:, :].rearrange("p (g m) -> p g m", g=16)
            R8 = WX["R8"][:, :].rearrange("p (g m) -> p g m", g=16)
            d5(lambda e: e.tensor_copy(Ec[:, :, 0:1], c8[:, :, 0:1]))
            d5(lambda e: e.tensor_copy(Es[:, :, 0:1], s8[:, :, 0:1]))
            d5(lambda e: e.tensor_copy(Rl[:, :].rearrange("p (g a) -> p g a", a=1), R8[:, :, 0:1]))
            d5(lambda e: e.memset(Rtab[:], 0.0))
            m = 1
            while m < 64:
                bre_b = Ec[:, :, m - 1:m].to_broadcast([128, 16, m])
                bim_b = Es[:, :, m - 1:m].to_broadcast([128, 16, m])
                u1 = t1[:, 0:16 * m].rearrange("p (g k) -> p g k", g=16)
                u2 = t2[:, 0:16 * m].rearrange("p (g k) -> p g k", g=16)
                d5(lambda e, m=m, bre_b=bre_b, u1=u1: e.tensor_tensor(u1, Ec[:, :, 0:m], bre_b, ALU.mult))
                d5(lambda e, m=m, bim_b=bim_b, u2=u2: e.tensor_tensor(u2, Es[:, :, 0:m], bim_b, ALU.mult))
                d5(lambda e, m=m, u1=u1, u2=u2: e.tensor_tensor(u1, u1, u2, ALU.subtract))
                d5(lambda e, m=m, bre_b=bre_b, u2=u2: e.tensor_tensor(u2, Es[:, :, 0:m], bre_b, ALU.mult))
                d5(lambda e, m=m, u1=u1: e.tensor_copy(Ec[:, :, m:2 * m], u1))
                d5(lambda e, m=m, bim_b=bim_b, u1=u1: e.tensor_tensor(u1, Ec[:, :, 0:m], bim_b, ALU.mult))
                d5(lambda e, m=m, u1=u1, u2=u2: e.tensor_tensor(Es[:, :, m:2 * m], u1, u2, ALU.add))
                m *= 2
            d5(lambda e: e.memset(Rtab[:, :, 1:64], 1.0))
            d5(lambda e: e.tensor_tensor(Rtab[:, :, 1:64], Rtab[:, :, 1:64], Rl[:, :].rearrange("p (g a) -> p g a", a=1).to_broadcast([128, 16, 63]), ALU.mult))
            S.emit()

        with contextlib.ExitStack() as st:
            def T(name, shape, dt=F32):
                return st.enter_context(nc.sbuf_tensor("m_" + name, shape, dt))

            S = Sched(nc, SEMS)
            dve = lambda fn, r=(), w=(): S.op("dve", fn, r, w)
            act = lambda fn, r=(), w=(): S.op("act", fn, r, w)
            pool = lambda fn, r=(), w=(): S.op("pool", fn, r, w)
            pe = lambda fn, r=(), w=(): S.op("pe", fn, r, w)
            x_sb = T("x_sb", [128, 4, DM])
            hT = T("hT", [128, 8, TT], BF16)
            xmT = T("xmT", [128, 8, TT + 4], BF16)
            xcar = T("xcar", [128, 8, 4], BF16)
            som = T("som", [128, 8, TT], BF16)
            usT = T("usT", [128, 4, TT], BF16)
            xcT = T("xcT", [128, 8, TT], BF16)
            R1 = T("R1", [128, 12288], BF16)
            vp = T("vp", [128, 4, 4, 256], BF16)
            C32 = T("C32", [128, 4, 2, 256])
            Cb = T("Cb", [128, 4, 2, 256], BF16)
            n32 = T("n32", [128, 4, 2])
            nb = T("nb", [128, 4, 2], BF16)
            wb16 = T("wb16", [128, 4, 4], BF16)
            glT = T("glT", [128, 4, TT], BF16)
            ring = T("ring", [128, RING, 2048], BF16)
            Sre = T("Sre", [128, 16, 65])
            Sim = T("Sim", [128, 16, 65])
            Sbre = T("Sbre", [128, 16, 64], BF16)
            Sbim = T("Sbim", [128, 16, 64], BF16)
            NF, NB = 6, 3
            fring = T("fring", [128, NF, 512])
            bring = T("bring", [128, NB, 512], BF16)
            rstate = {"f": 0, "b": 0}

            def f32buf():
                rstate["f"] = (rstate["f"] + 1) % NF
                return fring[:, rstate["f"], :], "fr%d" % rstate["f"]

            def bfbuf():
                rstate["b"] = (rstate["b"] + 1) % NB
                return bring[:, rstate["b"], :], "br%d" % rstate["b"]
            fcar = T("fcar", [128, 44, 2])
            bnd = T("bnd", [128, 44, 2])
            XM = ["xmT%d" % c for c in range(8)]
            VP = ["vp%d" % s_ for s_ in range(4)]
            rk = lambda b: "R1_%d" % b
            hn_tm = xmT[:, :, :].rearrange("p a b -> p (a b)")[:, 0:4096].rearrange("p (s f) -> p s f", s=4)
            qT = R1[:, 0:4096].rearrange("p (c t) -> p c t", c=8)
            kT = R1[:, 4096:8192].rearrange("p (c t) -> p c t", c=8)
            k_tm = R1[:, 8192:12288].rearrange("p (s f) -> p s f", s=4)
            sga, sgb = qT, kT
            gT = R1[:, 0:11264].rearrange("p (c t) -> p c t", c=22)
            hnb_t = R1[:, 0:4096].rearrange("p (s f) -> p s f", s=4)
            mT = vp[:, :, :, :].rearrange("p a b c -> p (a b c)").rearrange("p (c t) -> p c t", c=8)
            aoutT, boutT = xcT, usT
            epsc, onec = small[:, 250:251], small[:, 251:252]

            pool(lambda e: e.memset(C32[:], 0.0), [], ["C32_%d" % h for h in range(4)])
            pool(lambda e: e.memset(xcar[:], 0.0), [], ["xcar"])
            pool(lambda e: e.memset(fcar[:], 0.0), [], ["fcar"])
            pool(lambda e: e.memset(Sre[:], 0.0), [], ["Sre"])
            pool(lambda e: e.memset(Sim[:], 0.0), [], ["Sim"])
            pool(lambda e: e.memset(vp[:], 0.0), [], VP)
            pool(lambda e: e.memset(Cb[:], 0.0), [], ["Cb%d" % h for h in range(4)])
            pool(lambda e: e.memset(n32[:], 0.0), [], ["n32"])
            pool(lambda e: e.memset(nb[:], 0.0), [], ["nb"])

            seq = [sid for _ in range(nst) for sid in ORDER]
            wstate = {"issued": 0, "cur": -1}

            def wnext():
                wstate["cur"] += 1
                i = wstate["cur"]
                while wstate["issued"] < min(len(seq), i + RING):
                    n = wstate["issued"]
                    S.dma("sp", ring[:, n % RING, :], wsl_d[seq[n]], reads=[], writes=["ring%d" % (n % RING)])
                    wstate["issued"] += 1
                return ring[:, i % RING, :], "ring%d" % (i % RING)

            def sig(out, in_, rkeys, wkeys, bias=None, scale=1.0, eng2="pool"):
                if bias is None:
                    act(lambda e: e.activation(out, in_, AF.Tanh, scale=0.5 * scale), rkeys, wkeys)
                else:
                    act(lambda e: e.activation(out, in_, AF.Tanh, bias=bias, scale=0.5 * scale), list(rkeys) + ["chalf"], wkeys)
                S.op(eng2, lambda e: e.tensor_scalar(out, out, 0.5, 0.5, ALU.mult, ALU.add), wkeys, wkeys)

            psrr = {"i": 0}

            def nextps():
                psrr["i"] = (psrr["i"] + 1) % 4
                return psrr["i"], psf[psrr["i"]], "psf%d" % psrr["i"]

            def nextps6():
                psrr["i"] = (psrr["i"] + 1) % 6
                return psrr["i"], psf[psrr["i"]], "psf%d" % psrr["i"]

            def rms_rstd():
                ssq, rs = small[:, 0:4], small[:, 4:8]
                pool(lambda e: e.memset(ssq, 0.0), [], ["ssq%d" % i for i in range(4)])
                for sub in range(4):
                    act(lambda e, sub=sub: e.activation(hnb_t[:, sub, :], x_sb[:, sub, :], AF.Square, accum_out=ssq[:, sub:sub + 1]),
                        ["x_sb%d" % sub, "ssq%d" % sub], [rk(2 * sub), rk(2 * sub + 1), "ssq%d" % sub])
                    act(lambda e, sub=sub: e.activation(rs[:, sub:sub + 1], ssq[:, sub:sub + 1], AF.Ln, bias=epsc, scale=1.0 / DM), ["ssq%d" % sub], ["rs%d" % sub])
                    act(lambda e, sub=sub: e.activation(rs[:, sub:sub + 1], rs[:, sub:sub + 1], AF.Exp, scale=-0.5), ["rs%d" % sub], ["rs%d" % sub])
                return rs

            def norm_T(gcol_off):
                rs = rms_rstd()
                for sub in range(4):
                    dve(lambda e, sub=sub: e.tensor_scalar_mul(hnb_t[:, sub, :], x_sb[:, sub, :], rs[:, sub:sub + 1]),
                        ["x_sb%d" % sub, "rs%d" % sub], [rk(2 * sub), rk(2 * sub + 1)])
                for fc in range(8):
                    pb = psb[fc % 2]
                    for sub in range(4):
                        pe(lambda e, fc=fc, sub=sub, pb=pb: e.transpose(pb[:, sub * 128:(sub + 1) * 128], hnb_t[:, sub, fc * 128:(fc + 1) * 128], ident[:]),
                           [rk(2 * sub), rk(2 * sub + 1)], ["psb%d" % (fc % 2)])
                    dve(lambda e, fc=fc, pb=pb: e.tensor_scalar_mul(hT[:, fc, :], pb[:, 0:512], cc(gcol_off, fc)),
                        ["psb%d" % (fc % 2)], ["hT%d" % fc])

            def dump_T(src, nchunk, keys, stg):
                for c in range(nchunk):
                    fb, fk = f32buf()
                    dve(lambda e, c=c, fb=fb: e.tensor_copy(fb, src[:, c, :]), list(keys), [fk])
                    S.dma("pool", y_d[c * 128:(c + 1) * 128, stg * 512:(stg + 1) * 512], fb, reads=[fk], writes=["y"])

            def inproj_chunk(mc, slot, skey):
                pi, ps, pk = nextps()
                base = ((mc % 2) * 8) * 128
                for kt in range(8):
                    pe(lambda e, kt=kt, ps=ps: e.matmul(ps[:, :], lhsT=slot[:, base + kt * 128: base + (kt + 1) * 128], rhs=hT[:, kt, :],
                                                          start=(kt == 0), stop=(kt == 7)), [skey, "hT%d" % kt], [pk])
                return ps, pk

            for stg in range(nst):
                t0 = stg * TT
                for sub in range(4):
                    S.dma("pool", x_sb[:, sub, :], x_d[t0 + sub * 128:t0 + (sub + 1) * 128, :], writes=["x_sb%d" % sub])
                norm_T(C_G1)
                pool(lambda e: e.tensor_copy(xmT[:, :, 1:4], xcar[:, :, 1:4]), ["xcar"], XM)
                def conv_chunk(c):
                    z, zk = f32buf()
                    sg, sk = bfbuf()
                    dve(lambda e: e.tensor_scalar(z, xmT[:, c, 1:TT + 1], cc(C_MCW, c * 4 + 0), cc(C_MCB, c), ALU.mult, ALU.add),
                        ["xmT%d" % c], [zk])
                    for k in range(1, 4):
                        dve(lambda e, k=k: e.scalar_tensor_tensor(z, xmT[:, c, 1 + k:TT + 1 + k], cc(C_MCW, c * 4 + k), z, ALU.mult, ALU.add),
                            ["xmT%d" % c, zk], [zk])
                    sig(sg, z, [zk], [sk])
                    dve(lambda e: e.tensor_tensor(xcT[:, c, :], z, sg, ALU.mult), [zk, sk], ["xcT%d" % c])

                for mc in range(20):
                    if mc % 2 == 0:
                        slot, skey = wnext()
                    ps, pk = inproj_chunk(mc, slot, skey)
                    if mc < 8:
                        act(lambda e, mc=mc, ps=ps: e.activation(xmT[:, mc, 4:TT + 4], ps[:, :], AF.Identity, bias=cc(C_BIN, mc)),
                            [pk], ["xmT%d" % mc])
                        conv_chunk(mc)
                    elif mc < 16:
                        sig(som[:, mc - 8, :], ps[:, :], [pk], ["som%d" % (mc - 8)], bias=ch(C_BIN, mc))
                    else:
                        act(lambda e, mc=mc, ps=ps: e.activation(usT[:, mc - 16, :], ps[:, :], AF.Identity, bias=cc(C_BIN, mc)),
                            [pk], ["usT%d" % (mc - 16)])
                if debug == "h":
                    dump_T(hT, 8, ["hT%d" % c for c in range(8)], stg)
                if debug == "xm":
                    dump_T(xmT[:, :, 4:TT + 4], 8, XM, stg)
                gsb = small[:, 16:48].rearrange("p (s g) -> p s g", s=4)
                for sub in range(4):
                    for kt in range(8):
                        pe(lambda e, sub=sub, kt=kt: e.matmul(psf[4][:, sub * 8:(sub + 1) * 8], lhsT=hT[:, kt, sub * 128:(sub + 1) * 128],
                                                              rhs=wgb[:, kt * 8:(kt + 1) * 8], start=(kt == 0), stop=(kt == 7)),
                           ["hT%d" % kt], ["psf4"])
                for sub in range(4):
                    dve(lambda e, sub=sub: e.tensor_tensor(gsb[:, sub, :], psf[4][:, sub * 8:(sub + 1) * 8], crow[:, 1024:1032], ALU.add),
                        ["psf4"], ["gsb"])
                pool(lambda e: e.tensor_copy(xcar[:, :, 1:4], xmT[:, :, TT + 1:TT + 4]), XM, ["xcar"])
                if debug == "xc":
                    dump_T(xcT, 8, ["xcT%d" % c for c in range(8)], stg)
                lfn = small[:, 48:64].rearrange("p (s h) -> p s h", s=4)
                act(lambda e: e.activation(lfn, gsb[:, :, 4:8], AF.Exp, scale=-1.0), ["gsb"], ["lfn"])
                act(lambda e: e.activation(lfn, lfn, AF.Ln, bias=onec, scale=1.0), ["lfn"], ["lfn"])
                for sub in range(4):
                    pe(lambda e, sub=sub: e.matmul(psf[4][:, 64 + sub * 8:64 + sub * 8 + 4], lhsT=LT[:], rhs=lfn[:, sub, :], start=True, stop=True),
                       ["lfn"], ["psf4"])
                    pe(lambda e, sub=sub: e.matmul(psf[4][:, 64 + sub * 8 + 4:64 + sub * 8 + 8], lhsT=ONES[:], rhs=lfn[:, sub, :], start=True, stop=True),
                       ["lfn"], ["psf4"])
                Pm = psf[4][:, 64:96].rearrange("p (s g) -> p s g", s=4)
                wcol = small[:, 64:80].rearrange("p (s h) -> p s h", s=4)
                e2c = small[:, 80:96].rearrange("p (s h) -> p s h", s=4)
                dcol = small[:, 96:112].rearrange("p (s h) -> p s h", s=4)
                dve(lambda e: e.tensor_tensor(wcol, gsb[:, :, 0:4], Pm[:, :, 0:4], ALU.subtract), ["gsb", "psf4"], ["wcol"])
                act(lambda e: e.activation(wcol, wcol, AF.Exp), ["wcol"], ["wcol"])
                act(lambda e: e.copy(wb16[:], wcol), ["wcol"], ["wb16"])
                act(lambda e: e.activation(e2c, Pm[:, :, 0:4], AF.Exp, scale=-1.0), ["psf4"], ["e2c"])
                act(lambda e: e.activation(dcol, Pm[:, :, 4:8], AF.Exp, scale=-1.0), ["psf4"], ["dcol"])
                for hh in range(2):
                    slot, skey = wnext()
                    for hl in range(2):
                        h = hh * 2 + hl
                        for qk in range(2):
                            for ec in range(2):
                                pi, ps, pk = nextps()
                                for kt in range(2):
                                    col = ((((hl * 2 + qk) * 2 + ec) * 2 + kt)) * 128
                                    pe(lambda e, ps=ps, col=col, h=h, kt=kt, slot=slot: e.matmul(ps[:, :], lhsT=slot[:, col:col + 128], rhs=xcT[:, h * 2 + kt, :],
                                                                                                start=(kt == 0), stop=(kt == 1)),
                                       [skey, "xcT%d" % (h * 2 + kt)], [pk])
                                dst = (qT, kT)[qk]
                                act(lambda e, ps=ps, dst=dst, h=h, ec=ec, qk=qk: e.activation(dst[:, h * 2 + ec, :], ps[:, :], AF.Identity, scale=(1.0, 1.0 / 16.0)[qk]),
                                    [pk], [rk(qk * 8 + h * 2 + ec)])
                slot, skey = wnext()
                for sub in range(4):
                    for hp in range(2):
                        pi, ps, pk = nextps()
                        for hl in range(2):
                            h = hp * 2 + hl
                            for kt in range(2):
                                pe(lambda e, ps=ps, h=h, hl=hl, kt=kt, sub=sub, slot=slot: e.matmul(ps[:, hl * 256:(hl + 1) * 256], lhsT=xcT[:, h * 2 + kt, sub * 128:(sub + 1) * 128],
                                                                                                   rhs=slot[:, (h * 2 + kt) * 256:(h * 2 + kt + 1) * 256], start=(kt == 0), stop=(kt == 1)),
                                   [skey, "xcT%d" % (h * 2 + kt)], [pk])
                        act(lambda e, ps=ps, sub=sub, hp=hp: e.activation(k_tm[:, sub, hp * 512:(hp + 1) * 512], ps[:, :], AF.Identity, scale=1.0 / 16.0),
                            [pk], [rk(16 + sub * 2 + hp)])
                slot, skey = wnext()
                for sub in range(4):
                    for hp in range(2):
                        pi, ps, pk = nextps()
                        for hl in range(2):
                            h = hp * 2 + hl
                            for kt in range(2):
                                pe(lambda e, ps=ps, h=h, hl=hl, kt=kt, sub=sub, slot=slot: e.matmul(ps[:, hl * 256:(hl + 1) * 256], lhsT=xmT[:, h * 2 + kt, 4 + sub * 128:4 + (sub + 1) * 128],
                                                                                                   rhs=slot[:, (h * 2 + kt) * 256:(h * 2 + kt + 1) * 256], start=(kt == 0), stop=(kt == 1)),
                                   [skey, "xmT%d" % (h * 2 + kt)], [pk])
                        for hl in range(2):
                            h = hp * 2 + hl
                            dve(lambda e, ps=ps, sub=sub, h=h, hl=hl: e.tensor_scalar_mul(vp[:, sub, h, 0:256], ps[:, hl * 256:(hl + 1) * 256], wcol[:, sub, h:h + 1]),
                                [pk, "wcol"], VP)
                st6 = small[:, 144:168].rearrange("p (h s) -> p h s", h=4)
                mv = small[:, 168:176].rearrange("p (h s) -> p h s", h=4)
                dd, mx, rr = small[:, 176:180], small[:, 180:184], small[:, 184:188]
                for sub in range(4):
                    tok = slice(sub * 128, (sub + 1) * 128)
                    CBK = ["Cb%d" % h for h in range(4)]
                    for h in range(4):
                        act(lambda e, h=h, sub=sub: e.activation(Cb[:, h, :, :], C32[:, h, :, :], AF.Identity, scale=dcol[:, sub, h:h + 1]),
                            ["C32_%d" % h, "dcol"], ["Cb%d" % h])
                    dve(lambda e, sub=sub: e.tensor_tensor(nb[:], n32[:], dcol[:, sub, :].rearrange("p (h a) -> p h a", a=1).to_broadcast([128, 4, 2]), ALU.mult),
                        ["n32", "dcol"], ["nb"])
                    for h in range(4):
                        for ec in range(2):
                            pe(lambda e, h=h, ec=ec, tok=tok: e.matmul(psf[5][:, h * 128:(h + 1) * 128], lhsT=kT[:, h * 2 + ec, tok], rhs=qT[:, h * 2 + ec, tok],
                                                                       start=(ec == 0), stop=(ec == 1)),
                               [rk(8 + h * 2 + ec), rk(h * 2 + ec)], ["psf5"])
                    sTb, sTk = bfbuf()
                    dve(lambda e, sTb=sTb: e.tensor_tensor(sTb, psf[5][:, :], cmask4[:], ALU.mult), ["psf5"], [sTk])
                    for h in range(4):
                        pn, pnk = psf[h // 2][:, (h % 2) * 256:(h % 2 + 1) * 256], "psf%d" % (h // 2)
                        pe(lambda e, pn=pn, sub=sub, h=h, sTb=sTb: e.matmul(pn, lhsT=sTb[:, h * 128:(h + 1) * 128], rhs=vp[:, sub, h, :], start=True, stop=False),
                           [sTk] + VP, [pnk])
                        for ec in range(2):
                            pe(lambda e, pn=pn, h=h, ec=ec, tok=tok: e.matmul(pn, lhsT=qT[:, h * 2 + ec, tok], rhs=Cb[:, h, ec, :], start=False, stop=(ec == 1)),
                               [rk(h * 2 + ec), "Cb%d" % h], [pnk])
                        pd = psf[4][:, 128 + h:129 + h]
                        pe(lambda e, pd=pd, sub=sub, h=h, sTb=sTb: e.matmul(pd, lhsT=sTb[:, h * 128:(h + 1) * 128], rhs=wb16[:, sub, h:h + 1], start=True, stop=False),
                           [sTk, "wb16"], ["psf4"])
                        for ec in range(2):
                            pe(lambda e, pd=pd, h=h, ec=ec, tok=tok: e.matmul(pd, lhsT=qT[:, h * 2 + ec, tok], rhs=nb[:, h, ec:ec + 1], start=False, stop=(ec == 1)),
                               [rk(h * 2 + ec), "nb"], ["psf4"])
                    for h in range(4):
                        pu, puk = psf[2 + h % 2], "psf%d" % (2 + h % 2)
                        for dk in range(2):
                            pe(lambda e, pu=pu, sub=sub, h=h, dk=dk: e.matmul(pu[:, dk * 256:(dk + 1) * 256], lhsT=k_tm[:, sub, h * 256 + dk * 128:h * 256 + (dk + 1) * 128],
                                                                              rhs=vp[:, sub, h, :], start=True, stop=True),
                               [rk(16 + sub * 2 + h // 2)] + VP, [puk])
                            pe(lambda e, sub=sub, h=h, dk=dk: e.matmul(psf[4][:, 136 + h * 2 + dk:137 + h * 2 + dk], lhsT=k_tm[:, sub, h * 256 + dk * 128:h * 256 + (dk + 1) * 128],
                                                                       rhs=wb16[:, sub, h:h + 1], start=True, stop=True),
                               [rk(16 + sub * 2 + h // 2), "wb16"], ["psf4"])
                        dve(lambda e, pu=pu, h=h, sub=sub: e.scalar_tensor_tensor(C32[:, h, :, :].rearrange("p a b -> p (a b)"), C32[:, h, :, :].rearrange("p a b -> p (a b)"),
                                                                                  dcol[:, sub, h:h + 1], pu[:, :], ALU.mult, ALU.add),
                            [puk, "dcol", "C32_%d" % h], ["C32_%d" % h])
                    dve(lambda e, sub=sub: e.tensor_tensor(n32[:], n32[:], dcol[:, sub, :].rearrange("p (h a) -> p h a", a=1).to_broadcast([128, 4, 2]), ALU.mult),
                        ["n32", "dcol", "nb"], ["n32"])
                    dve(lambda e: e.tensor_tensor(n32[:], n32[:], psf[4][:, 136:144].rearrange("p (h a) -> p h a", a=2), ALU.add), ["n32", "psf4"], ["n32"])
                    for h in range(4):
                        pn, pnk = psf[h // 2][:, (h % 2) * 256:(h % 2 + 1) * 256], "psf%d" % (h // 2)
                        dve(lambda e, pn=pn, h=h: e.bn_stats(st6[:, h, :], pn), [pnk], ["st6"])
                    for h in range(4):
                        dve(lambda e, h=h: e.bn_aggr(mv[:, h, :], st6[:, h, :]), ["st6"], ["mv"])
                    dve(lambda e: e.tensor_copy(dd, psf[4][:, 128:132]), ["psf4"], ["dd"])
                    dve(lambda e: e.scalar_tensor_tensor(mx, dd, -1.0, dd, ALU.mult, ALU.max), ["dd"], ["mx"])
                    dve(lambda e, sub=sub: e.tensor_tensor(mx, mx, e2c[:, sub, :], ALU.max), ["mx", "e2c"], ["mx"])
                    dve(lambda e: e.tensor_tensor(mx, mx, mx, ALU.mult), ["mx"], ["mx"])
                    dve(lambda e: e.scalar_tensor_tensor(rr, mx, EPS, mv[:, :, 1], ALU.mult, ALU.add), ["mx", "mv"], ["rr"])
                    act(lambda e: e.activation(rr, rr, AF.Ln), ["rr"], ["rr"])
                    act(lambda e: e.activation(rr, rr, AF.Exp, scale=-0.5), ["rr"], ["rr"])
                    for h in range(4):
                        pn, pnk = psf[h // 2][:, (h % 2) * 256:(h % 2 + 1) * 256], "psf%d" % (h // 2)
                        dve(lambda e, pn=pn, sub=sub, h=h: e.tensor_scalar(hn_tm[:, sub, h * 256:(h + 1) * 256], pn, mv[:, h, 0:1], rr[:, h:h + 1], ALU.subtract, ALU.mult),
                            [pnk, "mv", "rr"], XM)
                for fc in range(8):
                    pb = psb[fc % 2]
                    for sub in range(4):
                        pe(lambda e, fc=fc, sub=sub, pb=pb: e.transpose(pb[:, sub * 128:(sub + 1) * 128], hn_tm[:, sub, fc * 128:(fc + 1) * 128], ident[:]),
                           XM, ["psb%d" % (fc % 2)])
                    hb, hk = bfbuf()
                    act(lambda e, fc=fc, pb=pb, hb=hb: e.activation(hb, pb[:, 0:512], AF.Identity, scale=cc(C_HG, fc)), ["psb%d" % (fc % 2)], [hk])
                    dve(lambda e, fc=fc, hb=hb: e.tensor_tensor(hb, hb, som[:, fc, :], ALU.mult), [hk, "som%d" % fc], [hk])
                    dve(lambda e, fc=fc, hb=hb: e.scalar_tensor_tensor(aoutT[:, fc, :], xcT[:, fc, :], cc(C_SK, fc), hb, ALU.mult, ALU.add),
                        [hk, "xcT%d" % fc], ["xcT%d" % fc])
                if debug == "aout":
                    dump_T(aoutT, 8, ["xcT%d" % c for c in range(8)], stg)
                US = ["usT%d" % c for c in range(4)]
                def s5_half(half, stg=stg):
                    for gl_ in range(8):
                        gp = half * 8 + gl_
                        chunk, win = gp // 4, gp % 4
                        cl = gl_ // 4
                        for ri in range(2):
                            for j in range(8):
                                pe(lambda e, ri=ri, j=j, cl=cl, chunk=chunk, win=win: e.matmul(
                                    psf[win][:, (cl * 2 + ri) * 64:(cl * 2 + ri + 1) * 64], lhsT=WZt[32 * win:32 * win + 32, chunk, ri, j, :],
                                    rhs=usT[32 * win:32 * win + 32, chunk, :].rearrange("p (b j) -> p b j", j=8)[:, :, j],
                                    start=(j == 0), stop=(j == 7), tile_position=(32 * win, 0)), US, ["psf%d" % win])
                    gs = slice(half * 8, half * 8 + 8)
                    EcH = Ec[:, gs, :].rearrange("p g k -> p (g k)")
                    EsH = Es[:, gs, :].rearrange("p g k -> p (g k)")
                    RtH = Rtab[:, gs, :].rearrange("p g k -> p (g k)")
                    (bre, kbre), (bim, kbim), (ore, kore), (oim, koim), (tt, ktt) = f32buf(), f32buf(), f32buf(), f32buf(), f32buf()
                    w4 = lambda a, w: a.rearrange("p (c w k) -> p c w k", c=2, w=4)[:, :, w, :]
                    for w in range(4):
                        Zw = psf[w][:, 0:256].rearrange("p (c r k) -> p c r k", c=2, r=2)
                        Zre_w, Zim_w = Zw[:, :, 0, :], Zw[:, :, 1, :]
                        pk_ = "psf%d" % w
                        dve(lambda e, w=w, Zre_w=Zre_w: e.tensor_tensor(w4(bre, w), Zre_w, w4(EcH, w), ALU.mult), [pk_], [kbre])
                        dve(lambda e, w=w, Zim_w=Zim_w: e.tensor_tensor(w4(tt, w), Zim_w, w4(EsH, w), ALU.mult), [pk_], [ktt])
                        dve(lambda e, w=w, Zim_w=Zim_w: e.tensor_tensor(w4(bim, w), Zim_w, w4(EcH, w), ALU.mult), [pk_], [kbim])
                        dve(lambda e, w=w, Zre_w=Zre_w: e.tensor_tensor(w4(ore, w), Zre_w, w4(EsH, w), ALU.mult), [pk_], [kore])
                    dve(lambda e: e.tensor_tensor(bre, bre, tt, ALU.add), [kbre, ktt], [kbre])
                    dve(lambda e: e.tensor_tensor(bim, bim, ore, ALU.subtract), [kbim, kore], [kbim])
                    c0 = small[:, 128:136]
                    for (bb, SS, sk, bk) in ((bre, Sre, "Sre", kbre), (bim, Sim, "Sim", kbim)):
                        dve(lambda e, SS=SS: e.tensor_tensor(c0.rearrange("p (g a) -> p g a", a=1), SS[:, gs, 0:1], Rl[:, gs].rearrange("p (g a) -> p g a", a=1), ALU.mult),
                            [sk], ["c0"])
                        dve(lambda e, bb=bb: e.tensor_tensor(bb.rearrange("p (g k) -> p g k", g=8)[:, :, 0:1], bb.rearrange("p (g k) -> p g k", g=8)[:, :, 0:1],
                                                            c0.rearrange("p (g a) -> p g a", a=1), ALU.add), ["c0", bk], [bk])
                    dve(lambda e: e.tensor_tensor_scan(ore, RtH, bre, 0.0, ALU.mult, ALU.add), [kbre, kbim, kore], [kore])
                    dve(lambda e: e.tensor_tensor_scan(oim, RtH, bim, 0.0, ALU.mult, ALU.add), [kbim], [koim])
                    v3 = lambda a: a.rearrange("p (g k) -> p g k", g=8)
                    dve(lambda e: e.tensor_tensor(tt, oim, EsH, ALU.mult), [koim], [ktt])
                    dve(lambda e: e.tensor_tensor(bre, ore, EcH, ALU.mult), [kore], [kbre])
                    dve(lambda e: e.tensor_tensor(Sre[:, gs, 1:65], v3(bre), v3(tt), ALU.subtract), [kbre, ktt, "Sbre"], ["Sre"])
                    dve(lambda e: e.tensor_tensor(tt, ore, EsH, ALU.mult), [kore], [ktt])
                    dve(lambda e: e.tensor_tensor(bim, oim, EcH, ALU.mult), [koim], [kbim])
                    dve(lambda e: e.tensor_tensor(Sim[:, gs, 1:65], v3(bim), v3(tt), ALU.add), [kbim, ktt, "Sbim"], ["Sim"])
                    pool(lambda e: e.tensor_copy(Sbre[:, gs, :], Sre[:, gs, 0:64]), ["Sre"], ["Sbre"])
                    pool(lambda e: e.tensor_copy(Sbim[:, gs, :], Sim[:, gs, 0:64]), ["Sim"], ["Sbim"])
                    pool(lambda e: e.tensor_copy(Sre[:, gs, 0:1], Sre[:, gs, 64:65]), ["Sre", "Sbre"], ["Sre"])
                    pool(lambda e: e.tensor_copy(Sim[:, gs, 0:1], Sim[:, gs, 64:65]), ["Sim", "Sbim"], ["Sim"])
                    for cl in range(2):
                        chunk = half * 2 + cl
                        Y, yk = psf[4 + cl], "psf%d" % (4 + cl)
                        Y3 = Y[:, :].rearrange("p (b j) -> p b j", j=8)
                        U3 = usT[:, chunk, :].rearrange("p (b j) -> p b j", j=8)
                        for tau in range(8):
                            pe(lambda e, tau=tau, chunk=chunk, Y3=Y3, U3=U3: e.matmul(Y3[:, :, tau:8], lhsT=Kt[:, chunk, tau, :], rhs=U3[:, :, 0:8 - tau],
                                                                                      start=(tau == 0), stop=False), US, [yk])
                        for win in range(4):
                            gp = chunk * 4 + win
                            for j in range(8):
                                for ri in range(2):
                                    last = (ri == 1)
                                    SB = (Sbre, Sbim)[ri]
                                    pe(lambda e, win=win, gp=gp, j=j, ri=ri, SB=SB, Y3=Y3, last=last: e.matmul(
                                        Y3[32 * win:32 * win + 32, :, j], lhsT=WI[:, gp, ri, j, :], rhs=SB[:, gp, :],
                                        start=False, stop=last, tile_position=(0, 32 * win)), ["Sbre", "Sbim"], [yk])
                        (ysb, yk2), (z2, zk2) = f32buf(), f32buf()
                        sgg, sgk = bfbuf()
                        act(lambda e, Y=Y, ysb=ysb: e.activation(ysb, Y[:, :], AF.Identity), [yk], [yk2])
                        act(lambda e, Y=Y, z2=z2: e.activation(z2, Y[:, :], AF.Square), [yk], [zk2])
                        pool(lambda e, z2=z2: e.tensor_scalar(z2, z2, 0.044715, 1.0, ALU.mult, ALU.add), [zk2], [zk2])
                        pool(lambda e, z2=z2, ysb=ysb: e.tensor_tensor(z2, z2, ysb, ALU.mult), [zk2, yk2], [zk2])
                        sig(sgg, z2, [zk2], [sgk], scale=2.0 * C1G)
                        dve(lambda e, chunk=chunk, ysb=ysb, sgg=sgg: e.tensor_tensor(glT[:, chunk, :], ysb, sgg, ALU.mult), [yk2, sgk], ["glT%d" % chunk])
                for half in range(2):
                    s5_half(half)
                if debug == "gl":
                    dump_T(glT, 4, ["glT%d" % c for c in range(4)], stg)
                slot, skey = wnext()
                for jc in range(4):
                    pi, ps, pk = nextps()
                    for kt in range(4):
                        pe(lambda e, ps=ps, jc=jc, kt=kt, slot=slot: e.matmul(ps[:, :], lhsT=slot[:, (jc * 4 + kt) * 128:(jc * 4 + kt + 1) * 128], rhs=glT[:, kt, :],
                                                                             start=(kt == 0), stop=(kt == 3)), [skey, "glT%d" % kt], [pk])
                    sgg, sgk = bfbuf()
                    sig(sgg, ps[:, :], [pk], [sgk], bias=ch(C_BGLU, jc))
                    dve(lambda e, jc=jc, sgg=sgg: e.tensor_tensor(boutT[:, jc, :], glT[:, jc, :], sgg, ALU.mult), [sgk, "glT%d" % jc], ["usT%d" % jc])
                if debug == "bout":
                    dump_T(boutT, 4, US, stg)
                for mc in range(20, 36):
                    if mc % 2 == 0:
                        slot, skey = wnext()
                    ps, pk = inproj_chunk(mc, slot, skey)
                    sig(qT[:, mc - 20, :] if mc < 28 else kT[:, mc - 28, :], ps[:, :], [pk], [rk(mc - 20)], bias=ch(C_BIN, mc))
                for mc in range(8):
                    if mc % 2 == 0:
                        slot, skey = wnext()
                    pi, ps, pk = nextps()
                    for kt in range(8):
                        pe(lambda e, ps=ps, mc=mc, kt=kt, slot=slot: e.matmul(ps[:, :], lhsT=slot[:, ((mc % 2) * 8 + kt) * 128:((mc % 2) * 8 + kt + 1) * 128], rhs=aoutT[:, kt, :],
                                                                             start=(kt == 0), stop=(kt == 7)), [skey, "xcT%d" % kt], [pk])
                    dve(lambda e, ps=ps, mc=mc: e.tensor_tensor(mT[:, mc, :], ps[:, :], sga[:, mc, :], ALU.mult), [pk, rk(mc)], VP)
                for mc in range(8):
                    if mc % 4 == 0:
                        slot, skey = wnext()
                    pi, ps, pk = nextps()
                    for kt in range(4):
                        pe(lambda e, ps=ps, mc=mc, kt=kt, slot=slot: e.matmul(ps[:, :], lhsT=slot[:, ((mc % 4) * 4 + kt) * 128:((mc % 4) * 4 + kt + 1) * 128], rhs=boutT[:, kt, :],
                                                                             start=(kt == 0), stop=(kt == 3)), [skey, "usT%d" % kt], [pk])
                    tb_, tk_ = bfbuf()
                    dve(lambda e, ps=ps, mc=mc, tb_=tb_: e.tensor_tensor(tb_, ps[:, :], sgb[:, mc, :], ALU.mult), [pk, rk(8 + mc)], [tk_])
                    pool(lambda e, mc=mc, tb_=tb_: e.tensor_tensor(mT[:, mc, :], mT[:, mc, :], tb_, ALU.add), [tk_] + VP, VP)
                if debug == "merged":
                    dump_T(mT, 8, VP, stg)
                for nh in range(2):
                    pss = [nextps() for _ in range(4)]
                    for kq in range(2):
                        slot, skey = wnext()
                        for k4 in range(4):
                            kt = kq * 4 + k4
                            for sub in range(4):
                                pi, ps, pk = pss[sub]
                                pe(lambda e, ps=ps, kt=kt, k4=k4, sub=sub, slot=slot: e.matmul(ps[:, :], lhsT=mT[:, kt, sub * 128:(sub + 1) * 128], rhs=slot[:, k4 * 512:(k4 + 1) * 512],
                                                                                              start=(kt == 0), stop=(kt == 7)), [skey] + VP, [pk])
                    for sub in range(4):
                        pi, ps, pk = pss[sub]
                        dve(lambda e, ps=ps, sub=sub, nh=nh: e.tensor_tensor(x_sb[:, sub, nh * 512:(nh + 1) * 512], x_sb[:, sub, nh * 512:(nh + 1) * 512], ps[:, :], ALU.add),
                            [pk, "x_sb%d" % sub], ["x_sb%d" % sub])
                if debug == "x1":
                    for sub in range(4):
                        S.dma("pool", y_d[t0 + sub * 128:t0 + (sub + 1) * 128, :], x_sb[:, sub, :], reads=["x_sb%d" % sub], writes=["y"])
                norm_T(C_G2)
                fcw3 = ccol[:, C_FCW:C_FCW + 132].rearrange("p (m k) -> p m k", k=3)
                bt = small[:, 192:236]
                dve(lambda e: e.tensor_tensor(bt, fcar[:, :, 1], fcw3[:, :, 1], ALU.mult), ["fcar"], ["bt"])
                dve(lambda e: e.tensor_tensor(bnd[:, :, 1], fcar[:, :, 1], fcw3[:, :, 0], ALU.mult), ["fcar"], ["bnd"])
                dve(lambda e: e.tensor_tensor(bnd[:, :, 0], fcar[:, :, 0], fcw3[:, :, 0], ALU.mult), ["fcar", "bnd"], ["bnd"])
                dve(lambda e: e.tensor_tensor(bnd[:, :, 0], bnd[:, :, 0], bt, ALU.add), ["bt", "bnd"], ["bnd"])
                for i in range(22):
                    slot, skey = wnext()
                    accs = (f32buf(), f32buf())
                    pss2 = []
                    for vg in range(2):
                        pi, ps, pk = nextps6()
                        pss2.append((ps, pk))
                        for kt in range(8):
                            pe(lambda e, ps=ps, vg=vg, kt=kt, slot=slot: e.matmul(ps[:, :], lhsT=slot[:, (vg * 8 + kt) * 128:(vg * 8 + kt + 1) * 128], rhs=hT[:, kt, :],
                                                                                 start=(kt == 0), stop=(kt == 7)), [skey, "hT%d" % kt], [pk])
                    for vg in range(2):
                        mc = i + 22 * vg
                        (acc, akey), (ps, pk) = accs[vg], pss2[vg]
                        act(lambda e, ps=ps, mc=mc, acc=acc: e.activation(acc, ps[:, :], AF.Identity, bias=cc(C_FCB, mc), scale=cc(C_FCW, mc * 3 + 2)),
                            [pk], [akey])
                    for tap, sh in ((1, 1), (0, 2)):
                        for vg in range(2):
                            mc = i + 22 * vg
                            (acc, akey), (ps, pk) = accs[vg], pss2[vg]
                            dve(lambda e, ps=ps, mc=mc, acc=acc, tap=tap, sh=sh: e.scalar_tensor_tensor(acc[:, sh:512], ps[:, 0:512 - sh], cc(C_FCW, mc * 3 + tap),
                                                                                                    acc[:, sh:512], ALU.mult, ALU.add),
                                [pk, akey], [akey])
                    for vg in range(2):
                        mc = i + 22 * vg
                        (acc, akey), (ps, pk) = accs[vg], pss2[vg]
                        dve(lambda e, mc=mc, acc=acc: e.tensor_tensor(acc[:, 0:2], acc[:, 0:2], bnd[:, mc, :], ALU.add), ["bnd", akey], [akey])
                        act(lambda e, ps=ps, mc=mc: e.copy(fcar[:, mc, 0:2], ps[:, 510:512]), [pk, akey, "bnd"], ["fcar"])
                    (av, avk), (ag, agk) = accs
                    sgg, sgk = bfbuf()
                    sig(sgg, ag, [agk], [sgk])
                    pool(lambda e, ag=ag, sgg=sgg: e.tensor_tensor(ag, ag, sgg, ALU.mult), [agk, sgk], [agk])
                    pool(lambda e, i=i, ag=ag, av=av: e.tensor_tensor(gT[:, i, :], ag, av, ALU.mult), [avk, agk], [rk(i)])
                for nh in range(2):
                    pss = [nextps() for _ in range(4)]
                    for kq in range(6):
                        slot, skey = wnext()
                        for k4 in range(4):
                            kt = kq * 4 + k4
                            if kt >= 22:
                                continue
                            for sub in range(4):
                                pi, ps, pk = pss[sub]
                                pe(lambda e, ps=ps, kt=kt, k4=k4, sub=sub, slot=slot: e.matmul(ps[:, :], lhsT=gT[:, kt, sub * 128:(sub + 1) * 128], rhs=slot[:, k4 * 512:(k4 + 1) * 512],
                                                                                              start=(kt == 0), stop=(kt == 21)), [skey, rk(kt)], [pk])
                    for sub in range(4):
                        pi, ps, pk = pss[sub]
                        dve(lambda e, ps=ps, sub=sub, nh=nh: e.tensor_tensor(x_sb[:, sub, nh * 512:(nh + 1) * 512], x_sb[:, sub, nh * 512:(nh + 1) * 512], ps[:, :], ALU.add),
                            [pk, "x_sb%d" % sub], ["x_sb%d" % sub])
                if debug is None:
                    rs = rms_rstd()
                    for sub in range(4):
                        for nh in range(2):
                            ob, okk = f32buf()
                            dve(lambda e, sub=sub, ob=ob, nh=nh: e.scalar_tensor_tensor(ob, x_sb[:, sub, nh * 512:(nh + 1) * 512], rs[:, sub:sub + 1],
                                                                                       crow[:, nh * 512:(nh + 1) * 512], ALU.mult, ALU.mult),
                                ["x_sb%d" % sub, "rs%d" % sub], [okk])
                            S.dma("pool", y_d[t0 + sub * 128:t0 + (sub + 1) * 128, nh * 512:(nh + 1) * 512], ob, reads=[okk], writes=["y"])
            S.emit()
    return nc


_CACHE = {}


def kernel(**inputs):
    prep = _prep(inputs)
    x = np.ascontiguousarray(np.asarray(inputs["x"], dtype=np.float32))
    if "nc" not in _CACHE:
        _CACHE["nc"] = build_nc()
    nc = _CACHE["nc"]
    in_maps = []
    for c in range(8):
        m = {"x": x[c]}
        m.update(prep)
        in_maps.append(m)
    res = run_bass_kernel_spmd(nc, in_maps, core_ids=list(range(8)))
    return np.stack([np.asarray(r["y"], dtype=np.float32) for r in res.results], axis=0)
```
